# Optimizing a Trainium2 kernel written in Bass

```python
import jax, jax.numpy as jnp
from jax import lax
import numpy as np

D_MODEL = 1024
BATCH = 8
SEQ = 4096
DEPTH = 1

CTX_LEN = 256
GRID_W = 64
HEAD_DIM = 128
N_Q_HEADS = D_MODEL // HEAD_DIM
N_KV_HEADS = N_Q_HEADS // 4
GQA_GROUP = N_Q_HEADS // N_KV_HEADS
ROPE_AXIS_DIM = HEAD_DIM // 2
ROPE_THETA = 10000.0
Q_BLOCK = 128
CONV_DIM = D_MODEL
CONV_WIDTH = 31
PEER_HEADS = 8
PEER_N_KEYS = 128
PEER_EXPERTS = PEER_N_KEYS * PEER_N_KEYS
PEER_TOPK = 16
PEER_KEY_DIM = 256
PEER_HALF = PEER_KEY_DIM // 2
PEER_CHUNK = 128
N_MOD = 6
EPS = 1e-6
Q_W = N_Q_HEADS * HEAD_DIM
KV_W = N_KV_HEADS * HEAD_DIM
IN_SPLITS = (Q_W, Q_W + KV_W, Q_W + 2 * KV_W, Q_W + 2 * KV_W + 2 * CONV_DIM, Q_W + 2 * KV_W + 2 * CONV_DIM + D_MODEL)
IN_W = IN_SPLITS[-1] + D_MODEL

kernel_name = 'hybrid_gqa_conformer_peer_dit_layer'


def rmsnorm(x, g):
    xf = x.astype(jnp.float32)
    y = xf * lax.rsqrt(jnp.mean(xf * xf, axis=-1, keepdims=True) + EPS)
    return (y * g.astype(jnp.float32)).astype(x.dtype)


def layernorm(x, g, b):
    xf = x.astype(jnp.float32)
    mu = jnp.mean(xf, axis=-1, keepdims=True)
    var = jnp.mean(jnp.square(xf - mu), axis=-1, keepdims=True)
    y = (xf - mu) * lax.rsqrt(var + EPS) * g.astype(jnp.float32) + b.astype(jnp.float32)
    return y.astype(x.dtype)


def modulate(h, shift, scale):
    return h * (1 + scale) + shift


def axial_rope_tables(n_tokens, dtype):
    rows = n_tokens // GRID_W
    row = jnp.repeat(jnp.arange(rows, dtype=jnp.float32), GRID_W)
    col = jnp.tile(jnp.arange(GRID_W, dtype=jnp.float32), rows)
    inv = ROPE_THETA ** (-jnp.arange(0, ROPE_AXIS_DIM, 2, dtype=jnp.float32) / ROPE_AXIS_DIM)
    ang_r = row[:, None] * inv
    ang_c = col[:, None] * inv
    return (jnp.cos(ang_r)[:, None, :].astype(dtype), jnp.sin(ang_r)[:, None, :].astype(dtype),
            jnp.cos(ang_c)[:, None, :].astype(dtype), jnp.sin(ang_c)[:, None, :].astype(dtype))


def rotate(xa, cos, sin):
    x1, x2 = jnp.split(xa, 2, axis=-1)
    return jnp.concatenate([x1 * cos - x2 * sin, x1 * sin + x2 * cos], axis=-1)


def apply_rope(x, rope):
    cos_r, sin_r, cos_c, sin_c = rope
    xr, xc = jnp.split(x, 2, axis=-1)
    return jnp.concatenate([rotate(xr, cos_r, sin_r), rotate(xc, cos_c, sin_c)], axis=-1)


def attend(q, k, v):
    B, T = q.shape[0], q.shape[1]
    nb = T // Q_BLOCK
    scale = HEAD_DIM ** -0.5
    qb = q.reshape(B, nb, Q_BLOCK, N_KV_HEADS, GQA_GROUP, HEAD_DIM).transpose(1, 0, 2, 3, 4, 5)

    def block(qi):
        s = jnp.einsum('bqkgd,bskd->bkgqs', qi, k).astype(jnp.float32) * scale
        p = jax.nn.softmax(s, axis=-1).astype(v.dtype)
        return jnp.einsum('bkgqs,bskd->bqkgd', p, v)

    o = lax.map(block, qb)
    return o.transpose(1, 0, 2, 3, 4, 5).reshape(B, T, Q_W)


def conformer_conv(u, lw):
    a, b = jnp.split(u, 2, axis=-1)
    g = a * jax.nn.sigmoid(b)
    pad = CONV_WIDTH // 2
    g = lax.conv_general_dilated(g, lw['conv_dw'][:, None, :], window_strides=(1,), padding=[(pad, pad)],
                                 dimension_numbers=('NWC', 'WIO', 'NWC'), feature_group_count=CONV_DIM)
    g = layernorm(g + lw['conv_b'], lw['conv_ln_g'], lw['conv_ln_b'])
    return jax.nn.silu(g) @ lw['w_conv_o']


def context_kv(h, lw):
    B, T, _ = h.shape
    kv = h @ lw['w_in'][:, Q_W:Q_W + 2 * KV_W]
    k, v = jnp.split(kv, 2, axis=-1)
    k = rmsnorm(k.reshape(B, T, N_KV_HEADS, HEAD_DIM), lw['k_norm_g'])
    return k, v.reshape(B, T, N_KV_HEADS, HEAD_DIM)


def token_mixer(h, lw, rope, k_ctx, v_ctx):
    B, T, _ = h.shape
    q, k, v, u, ga, gc = jnp.split(h @ lw['w_in'], IN_SPLITS, axis=-1)
    q = rmsnorm(q.reshape(B, T, N_Q_HEADS, HEAD_DIM), lw['q_norm_g'])
    k = rmsnorm(k.reshape(B, T, N_KV_HEADS, HEAD_DIM), lw['k_norm_g'])
    v = v.reshape(B, T, N_KV_HEADS, HEAD_DIM)
    if rope is not None:
        q = apply_rope(q, rope)
        k = jnp.concatenate([apply_rope(k, rope), k_ctx], axis=1)
        v = jnp.concatenate([v, v_ctx], axis=1)
    y_att = attend(q, k, v) @ lw['w_attn_o']
    y_conv = conformer_conv(u, lw)
    merged = jax.nn.sigmoid(ga) * y_att + jax.nn.sigmoid(gc) * y_conv
    return merged @ lw['w_out']


def peer_ffn(h, lw):
    B, T, D = h.shape
    wq, keys, u_tab, v_tab = lw['peer_wq'], lw['peer_keys'], lw['peer_u'], lw['peer_v']

    def chunk(xc):
        q = (xc @ wq).reshape(PEER_CHUNK, PEER_HEADS, 2, PEER_HALF)
        s = jnp.einsum('thpd,hpnd->thpn', q, keys)
        s_top, i_top = lax.top_k(s, PEER_TOPK)
        n_cand = PEER_TOPK * PEER_TOPK
        cand_s = (s_top[:, :, 0, :, None] + s_top[:, :, 1, None, :]).reshape(PEER_CHUNK, PEER_HEADS, n_cand)
        cand_i = (i_top[:, :, 0, :, None] * PEER_N_KEYS + i_top[:, :, 1, None, :]).reshape(PEER_CHUNK, PEER_HEADS, n_cand)
        best_s, best_pos = lax.top_k(cand_s, PEER_TOPK)
        eid = jnp.take_along_axis(cand_i, best_pos, axis=-1)
        w = jax.nn.softmax(best_s.astype(jnp.float32), axis=-1).astype(xc.dtype)
        act = jax.nn.gelu(jnp.einsum('thkd,td->thk', jnp.take(u_tab, eid, axis=0), xc), approximate=False)
        return jnp.einsum('thk,thkd->td', w * act, jnp.take(v_tab, eid, axis=0))

    y = lax.map(chunk, h.reshape(-1, PEER_CHUNK, D))
    return y.reshape(B, T, D)


def setup_inputs(seed: int = 0) -> dict:
    key = jax.random.key(seed)
    ks = jax.random.split(key, 24)
    L, D = DEPTH, D_MODEL

    def nrm(k, shape, scale):
        return jax.random.normal(k, shape, jnp.float32) * scale

    return {
        'x': nrm(ks[0], (BATCH, SEQ, D), 1.0),
        'c': nrm(ks[1], (BATCH, D), 1.0),
        'ctx': nrm(ks[2], (BATCH, CTX_LEN, D), 1.0),
        'c_ctx': nrm(ks[3], (D,), 1.0),
        'w_mod': nrm(ks[4], (L, D, N_MOD * D), 0.5 * D ** -0.5),
        'b_mod': nrm(ks[5], (L, N_MOD * D), 0.01),
        'norm1_g': 1.0 + nrm(ks[6], (L, D), 0.1),
        'norm2_g': 1.0 + nrm(ks[7], (L, D), 0.1),
        'w_in': nrm(ks[8], (L, D, IN_W), D ** -0.5),
        'q_norm_g': 1.0 + nrm(ks[9], (L, HEAD_DIM), 0.1),
        'k_norm_g': 1.0 + nrm(ks[10], (L, HEAD_DIM), 0.1),
        'w_attn_o': nrm(ks[11], (L, Q_W, D), Q_W ** -0.5),
        'conv_dw': nrm(ks[12], (L, CONV_WIDTH, CONV_DIM), CONV_WIDTH ** -0.5),
        'conv_b': nrm(ks[13], (L, CONV_DIM), 0.01),
        'conv_ln_g': 1.0 + nrm(ks[14], (L, CONV_DIM), 0.1),
        'conv_ln_b': nrm(ks[15], (L, CONV_DIM), 0.01),
        'w_conv_o': nrm(ks[16], (L, CONV_DIM, D), CONV_DIM ** -0.5),
        'w_out': nrm(ks[17], (L, D, D), D ** -0.5),
        'peer_wq': nrm(ks[18], (L, D, PEER_HEADS * PEER_KEY_DIM), D ** -0.5),
        'peer_keys': nrm(ks[19], (L, PEER_HEADS, 2, PEER_N_KEYS, PEER_HALF), PEER_HALF ** -0.5),
        'peer_u': nrm(ks[20], (L, PEER_EXPERTS, D), D ** -0.5),
        'peer_v': nrm(ks[21], (L, PEER_EXPERTS, D), 0.5),
        'final_norm_g': 1.0 + nrm(ks[22], (D,), 0.1),
    }


def reference(x, c, ctx, c_ctx, w_mod, b_mod, norm1_g, norm2_g, w_in, q_norm_g, k_norm_g,
              w_attn_o, conv_dw, conv_b, conv_ln_g, conv_ln_b, w_conv_o, w_out,
              peer_wq, peer_keys, peer_u, peer_v, final_norm_g):
    S = x.shape[1]
    rope = axial_rope_tables(S, x.dtype)
    xs, cs = x, ctx
    for l in range(DEPTH):
        lw = {'w_in': w_in[l], 'q_norm_g': q_norm_g[l], 'k_norm_g': k_norm_g[l], 'w_attn_o': w_attn_o[l],
              'conv_dw': conv_dw[l], 'conv_b': conv_b[l], 'conv_ln_g': conv_ln_g[l], 'conv_ln_b': conv_ln_b[l],
              'w_conv_o': w_conv_o[l], 'w_out': w_out[l], 'peer_wq': peer_wq[l], 'peer_keys': peer_keys[l],
              'peer_u': peer_u[l], 'peer_v': peer_v[l]}
        last = l == DEPTH - 1
        sh1, sc1, g1, sh2, sc2, g2 = [m[:, None, :] for m in
                                      jnp.split(jax.nn.silu(c) @ w_mod[l] + b_mod[l], N_MOD, axis=-1)]
        csh1, csc1, cg1, csh2, csc2, cg2 = jnp.split(jax.nn.silu(c_ctx) @ w_mod[l] + b_mod[l], N_MOD, axis=-1)
        hc = modulate(rmsnorm(cs, norm1_g[l]), csh1, csc1)
        k_c, v_c = context_kv(hc, lw)
        hx = modulate(rmsnorm(xs, norm1_g[l]), sh1, sc1)
        xs = xs + g1 * token_mixer(hx, lw, rope, k_c, v_c)
        if not last:
            cs = cs + cg1 * token_mixer(hc, lw, None, None, None)
        hx = modulate(rmsnorm(xs, norm2_g[l]), sh2, sc2)
        xs = xs + g2 * peer_ffn(hx, lw)
        if not last:
            hc = modulate(rmsnorm(cs, norm2_g[l]), csh2, csc2)
            cs = cs + cg2 * peer_ffn(hc, lw)
    return rmsnorm(xs, final_norm_g)
```

```python
import numpy as np
from contextlib import ExitStack
import concourse.bass as bass
import concourse.mybir as mybir
from concourse.bass_utils import run_bass_kernel_spmd

F32 = mybir.dt.float32
BF16 = mybir.dt.bfloat16
I32 = mybir.dt.int32
U32 = mybir.dt.uint32
AF = mybir.ActivationFunctionType
ALU = mybir.AluOpType
AX = mybir.AxisListType

T = 4096
NT = 32
CTXN = 256
NKEY = T + CTXN
NKT = NKEY // 128
D = 1024
KC = 8
INW = 5632
EPS = 1e-6
GP = 256
ENGS = ("pe", "act", "dve", "pool", "sp")


class Buf:
    __slots__ = ("name", "lw", "rd")

    def __init__(self, name=""):
        self.name = name
        self.lw = None
        self.rd = {}


class Sched:
    def __init__(self, nc, n_dma_sems=(("sp", 16), ("pool", 40), ("act", 4))):
        self.nc = nc
        self.q = {e: [] for e in ENGS}
        self.sems = {}
        self.cnt = {}
        for e in ENGS:
            self.sems[e] = nc.alloc_semaphore(name="s_" + e)
            self.cnt[e] = 0
        self.dsem = {}
        self.drr = {}
        for e, n in n_dma_sems:
            self.dsem[e] = []
            for i in range(n):
                k = "d_%s_%d" % (e, i)
                self.sems[k] = nc.alloc_semaphore(name=k)
                self.cnt[k] = 0
                self.dsem[e].append(k)
            self.drr[e] = 0
        self.seen = {e: {} for e in ENGS}
        self.nops = 0

    def _deps(self, reads, writes):
        deps = {}

        def need(tok):
            if tok is None:
                return
            k, v = tok
            if deps.get(k, 0) < v:
                deps[k] = v
        for r in reads:
            need(r.lw)
        for w in writes:
            need(w.lw)
            for k, v in w.rd.items():
                need((k, v))
        return deps

    def _push(self, eng, deps, fn, sk, inc, reads, writes, silent=False):
        waits = []
        seen = self.seen[eng]
        for k, v in deps.items():
            if k == eng and v > self.cnt[k]:
                continue
            if seen.get(k, 0) < v:
                seen[k] = v
                waits.append((k, v))
        if silent:
            val = self.cnt[sk] + inc
            self.q[eng].append((waits, fn, None, 0))
        else:
            self.cnt[sk] += inc
            val = self.cnt[sk]
            self.q[eng].append((waits, fn, sk, inc))
        for r in reads:
            if r.rd.get(sk, 0) < val:
                r.rd[sk] = val
        for w in writes:
            w.lw = (sk, val)
            w.rd = {}
        self.nops += 1

    def op(self, eng, fn, reads=(), writes=(), silent=False):
        self._push(eng, self._deps(reads, writes), fn, eng, 1, reads, writes, silent)

    def dma(self, eng, fn, reads=(), writes=()):
        deps = self._deps(reads, writes)
        k = self.dsem[eng][self.drr[eng] % len(self.dsem[eng])]
        self.drr[eng] += 1
        if self.cnt[k] > 0 and deps.get(k, 0) < self.cnt[k]:
            deps[k] = self.cnt[k]
        self._push(eng, deps, fn, k, 16, reads, writes)

    def barrier(self, exclude=()):
        for e in ENGS:
            waits = []
            for k, v in self.cnt.items():
                if k in exclude or v == 0:
                    continue
                if self.seen[e].get(k, 0) < v:
                    self.seen[e][k] = v
                    waits.append((k, v))
            if waits:
                self.q[e].append((waits, None, None, 0))

    def emit(self):
        nc = self.nc
        sems = self.sems
        q = self.q
        cnt = self.cnt
        with nc.Block() as block:
            def mk(ename):
                def body(e):
                    for waits, fn, sk, inc in q[ename]:
                        for k, v in waits:
                            e.wait_ge(sems[k], v)
                        if fn is not None:
                            if sk is None:
                                fn(e)
                            else:
                                fn(e).then_inc(sems[sk], inc)
                    if ename == "sp":
                        for k, v in cnt.items():
                            if v > 0:
                                e.wait_ge(sems[k], v)
                return body
            block.tensor(mk("pe"))
            block.scalar(mk("act"))
            block.vector(mk("dve"))
            block.gpsimd(mk("pool"))
            block.sync(mk("sp"))


def XAP(base, dims, off=0):
    return bass.AP(tensor=base.tensor, offset=base.offset + off,
                   ap=[list(base.ap[0])] + [list(d) for d in dims])


def build(upto=9, dbg=()):
    nc = bass.Bass("TRN2", target_bir_lowering=False)
    dt_in = lambda n, s, d=F32: nc.dram_tensor(n, list(s), d, kind="ExternalInput").ap()
    x_d = dt_in("x", [T, D])
    ctx_d = dt_in("ctx", [CTXN, D])
    ccol_d = dt_in("ccol", [128, 16])
    wmod_d = dt_in("w_mod", [D, 6 * D])
    bmod_d = dt_in("b_mod", [1, 6 * D])
    ncol_d = dt_in("ncol", [128, 40])
    win_d = dt_in("w_in", [D, INW])
    qkg_d = dt_in("qkg", [1, 256])
    wao_d = dt_in("w_attn_o", [D, D])
    cdw_d = dt_in("cdw", [128, 8 * 31])
    wco_d = dt_in("w_conv_o", [D, D])
    wout_d = dt_in("w_out", [D, D])
    wq_d = dt_in("peer_wq", [D, 2048])
    keyT_d = dt_in("keyT", [128, 16 * 128])
    ut_d = dt_in("ut_l", [16384, 1024])
    vl_d = dt_in("v_l", [16384, 1024])
    fg_d = dt_in("fg", [1, D])
    ropec_d = dt_in("rope_c", [T, 128])
    ropes_d = dt_in("rope_s", [T, 128])
    out_d = nc.dram_tensor("out", [T, D], F32, kind="ExternalOutput").ap()
    dbg_d = {}
    for n, s, d in dbg:
        dbg_d[n] = nc.dram_tensor("dbg_" + n, list(s), d, kind="ExternalOutput").ap()

    scr = lambda n, s, d: nc.dram_tensor(n, list(s), d, kind="Internal").ap()
    utbf_d = scr("utbf", [16384, 1024], BF16)
    vbf_d = scr("vbf", [16384, 1024], BF16)
    qt_d = scr("qt_scr", [8, 128, T], BF16)
    gt_d = scr("gt_scr", [8, 128, T + 30], BF16)
    sg_d = scr("sg_scr", [16, 128, T], BF16)
    xs_d = scr("xs_scr", [T, D], F32)
    h2_d = scr("h2_scr", [8, 128, T], BF16)
    rt_d = scr("rt_scr", [3, 128, T], F32)
    g2_d = scr("g2_scr", [1, D], F32)
    waobf_d = scr("wao_bf", [D, D], BF16); b_waobf = Buf()
    wcobf_d = scr("wco_bf", [D, D], BF16); b_wcobf = Buf()
    woutbf_d = scr("wout_bf", [D, D], BF16); b_woutbf = Buf()
    wqbf_d = scr("wq_bf", [D, 2048], BF16); b_wqbf = Buf()
    keyTbf_d = scr("keyT_bf", [128, 2048], BF16); b_keyTbf = Buf()
    b_g2d = Buf()
    b_qt = [Buf() for _ in range(8)]
    b_gt = [Buf() for _ in range(8)]
    b_gtpad = Buf()
    b_sg = [Buf() for _ in range(8)]
    b_xs = [Buf() for _ in range(NT)]
    b_h2 = [Buf() for _ in range(8)]
    b_rt = [Buf() for _ in range(8)]

    S = Sched(nc)

    def mm(out, lhsT, rhs, start, stop, r, w, silent=False):
        S.op("pe", lambda e: e.matmul(out, lhsT=lhsT, rhs=rhs, start=start, stop=stop), r, w, silent=silent)

    def tr(out, in_, ident, r, w):
        S.op("pe", lambda e: e.transpose(out=out, in_=in_, identity=ident), r, w)

    def act(out, in_, func, r, w, bias=None, scale=None, accum=None, eng="act"):
        kw = {}
        if bias is not None:
            kw["bias"] = bias
        if scale is not None:
            kw["scale"] = scale
        if accum is not None:
            kw["accum_out"] = accum
        S.op(eng, lambda e: e.activation(out=out, in_=in_, func=func, **kw), r, w)

    def tt(eng, out, in0, in1, op, r, w):
        S.op(eng, lambda e: e.tensor_tensor(out=out, in0=in0, in1=in1, op=op), r, w)

    def ts(eng, out, in0, s1, s2, op0, op1, r, w):
        if op1 is None:
            S.op(eng, lambda e: e.tensor_scalar(out=out, in0=in0, scalar1=s1, scalar2=None, op0=op0), r, w)
        else:
            S.op(eng, lambda e: e.tensor_scalar(out=out, in0=in0, scalar1=s1, scalar2=s2, op0=op0, op1=op1), r, w)

    def stt(out, in0, scalar, in1, op0, op1, r, w):
        S.op("dve", lambda e: e.scalar_tensor_tensor(out=out, in0=in0, scalar=scalar, in1=in1, op0=op0, op1=op1), r, w)

    def cp(eng, out, in_, r, w):
        if eng == "act":
            S.op("act", lambda e: e.activation(out=out, in_=in_, func=AF.Copy), r, w)
        else:
            S.op(eng, lambda e: e.tensor_copy(out=out, in_=in_), r, w)

    def rcp(out, in_, r, w):
        S.op("dve", lambda e: e.reciprocal(out=out, in_=in_), r, w)

    def dma(q, out, in_, r, w):
        S.dma(q, lambda e: e.dma_start(out=out, in_=in_), r, w)

    def dump(name, ap, r):
        if name in dbg_d:
            dma("sp", dbg_d[name], ap, r, [])

    top = ExitStack()
    with top:
        sbt = lambda es, n, s, d: es.enter_context(nc.sbuf_tensor("sb_" + n, list(s), d))
        PS = top.enter_context(nc.psum_tensor("PS", [128, 4096], F32))
        PSB = PS.bitcast(BF16)
        pb = [Buf("bank%d" % i) for i in range(8)]
        bank = lambda i, a=0, b=512: PS[:, i * 512 + a:i * 512 + b]
        bankb = lambda i, a=0, b=1024: PSB[:, i * 1024 + a:i * 1024 + b]

        ident_f = sbt(top, "ident_f", [128, 128], F32); b_idf = Buf()
        ident_b = sbt(top, "ident_b", [128, 128], BF16); b_idb = Buf()
        ones_f = sbt(top, "ones_f", [128, 128], F32); b_1f = Buf()
        ones_b = sbt(top, "ones_b", [128, 128], BF16); b_1b = Buf()
        modc = sbt(top, "modc", [128, 48], F32); b_modc = Buf()
        ncol = sbt(top, "ncol", [128, 40], F32); b_ncol = Buf()
        mid = ExitStack()
        g1b = sbt(mid, "g1b", [128, D], F32); b_g1b = Buf()
        KT = sbt(mid, "KT", [128, 2 * NKEY], BF16); b_KT = [Buf() for _ in range(NKT)]
        Vs = sbt(mid, "Vs", [128, NKT * 256], BF16); b_V = [Buf() for _ in range(NKT)]

        S.op("pool", lambda e: e.memset(ident_f[:], 0.0), [], [b_idf])
        S.op("pool", lambda e: e.affine_select(out=ident_f[:], in_=ident_f[:], pattern=[[-1, 128]],
                                               compare_op=ALU.not_equal, fill=1.0, base=0, channel_multiplier=1),
             [b_idf], [b_idf])
        cp("dve", ident_b[:], ident_f[:], [b_idf], [b_idb])
        S.op("pool", lambda e: e.memset(ones_f[:], 1.0), [], [b_1f])
        cp("dve", ones_b[:], ones_f[:], [b_1f], [b_1b])
        dma("sp", ncol[:], ncol_d, [], [b_ncol])

        b_ut = [Buf() for _ in range(8)]
        b_vt = [Buf() for _ in range(8)]
        def cast_tables(after=()):
            if upto >= 4:
                for a in range(8):
                    dma("pool", utbf_d[a * 2048:(a + 1) * 2048, :], ut_d[a * 2048:(a + 1) * 2048, :], list(after), [b_ut[a]])
                    dma("pool", vbf_d[a * 2048:(a + 1) * 2048, :], vl_d[a * 2048:(a + 1) * 2048, :], list(after), [b_vt[a]])

        w01 = ExitStack()
        win = sbt(w01, "win", [128, 8 * INW], BF16); b_win = [Buf() for _ in range(11)]
        winv = win_d.rearrange("(k p) n -> p k n", p=128)
        win3 = win[:].rearrange("p (k n) -> p k n", k=8)

        with ExitStack() as es:
            ccol = sbt(es, "ccol", [128, 16], F32); b_ccol = Buf()
            cs = sbt(es, "cs", [128, 16], F32); b_cs = Buf()
            wm = [sbt(es, "wm%d" % i, [128, 8 * 512], F32) for i in range(2)]; b_wm = [Buf(), Buf()]
            bmj = [sbt(es, "bm%d" % i, [1, 512], F32) for i in range(2)]; b_bmj = [Buf(), Buf()]
            mrow = sbt(es, "mrow", [1, 6 * D], F32); b_mrow = Buf()
            mrowc = sbt(es, "mrowc", [1, 2 * D], F32); b_mrowc = Buf()
            tmpc = sbt(es, "tmpc", [128, 48], F32); b_tmpc = Buf()
            dma("sp", ccol[:], ccol_d, [], [b_ccol])
            act(cs[:], ccol[:], AF.Silu, [b_ccol], [b_cs])
            wmv = wmod_d.rearrange("(k p) n -> p k n", p=128)
            for j in range(12):
                w_ = wm[j % 2]
                dma("sp", w_[:].rearrange("p (k n) -> p k n", k=8), wmv[:, :, j * 512:(j + 1) * 512], [], [b_wm[j % 2]])
                bm = bmj[j % 2]; b_bm = b_bmj[j % 2]
                dma("sp", bm[:], bmod_d[0:1, j * 512:(j + 1) * 512], [], [b_bm])
                if upto >= 1 and j >= 1:
                    wi_ = wm[(j - 1) % 2]
                    dma("sp", wi_[:].rearrange("p (k n) -> p k n", k=8), winv[:, :, (j - 1) * 512:j * 512], [], [b_wm[(j - 1) % 2]])
                    cp("act", win3[:, :, (j - 1) * 512:j * 512], wi_[:].rearrange("p (k n) -> p k n", k=8),
                       [b_wm[(j - 1) % 2]], [b_win[j - 1]])
                for k in range(8):
                    mm(bank(0)[0:1, :], cs[:, k:k + 1], w_[:, k * 512:(k + 1) * 512], k == 0, k == 7,
                       [b_cs, b_wm[j % 2]], [pb[0]])
                tt("dve", mrow[0:1, j * 512:(j + 1) * 512], bank(0)[0:1, :], bm[0:1, :], ALU.add,
                   [pb[0], b_bm], [b_mrow])
                if j < 4:
                    for k in range(8):
                        mm(bank(1)[0:1, :], cs[:, 8 + k:9 + k], w_[:, k * 512:(k + 1) * 512], k == 0, k == 7,
                           [b_cs, b_wm[j % 2]], [pb[1]])
                    tt("dve", mrowc[0:1, j * 512:(j + 1) * 512], bank(1)[0:1, :], bm[0:1, :], ALU.add,
                       [pb[1], b_bm], [b_mrowc])
            segs = [(mrow, 0, b_mrow), (mrow, 1, b_mrow), (mrowc, 0, b_mrowc), (mrowc, 1, b_mrowc),
                    (mrow, 3, b_mrow), (mrow, 4, b_mrow)]
            for si, (row, seg, bb) in enumerate(segs):
                for k in range(8):
                    mm(bank(2)[:, si * 8 + k:si * 8 + k + 1], row[0:1, seg * D + k * 128:seg * D + (k + 1) * 128],
                       ones_f[0:1, 0:1], True, True, [bb, b_1f], [pb[2]])
            cp("dve", tmpc[:], bank(2)[:, 0:48], [pb[2]], [b_tmpc])
            for (dst, sc_i, sh_i, ng_off) in ((0, 1, 0, 0), (2, 3, 2, 0), (4, 5, 4, 8)):
                stt(modc[:, dst * 8:dst * 8 + 8], tmpc[:, sc_i * 8:sc_i * 8 + 8], 1.0, ncol[:, ng_off:ng_off + 8],
                    ALU.add, ALU.mult, [b_tmpc, b_ncol], [b_modc])
                cp("dve", modc[:, (dst + 1) * 8:(dst + 1) * 8 + 8], tmpc[:, sh_i * 8:sh_i * 8 + 8], [b_tmpc], [b_modc])
            for (dstt, bdst, seg) in ((g1b, b_g1b, 2),):
                for nb in range(2):
                    mm(bank(3 + nb), ones_f[0:1, :], mrow[0:1, seg * D + nb * 512:seg * D + (nb + 1) * 512], True, True,
                       [b_mrow, b_1f], [pb[3 + nb]])
                    cp("dve", dstt[:, nb * 512:(nb + 1) * 512], bank(3 + nb), [pb[3 + nb]], [bdst])
            dma("sp", g2_d, mrow[0:1, 5 * D:6 * D], [b_mrow], [b_g2d])
            dump("modc", modc[:], [b_modc])
            dump("g1b", g1b[:], [b_g1b])
            S.barrier(exclude=S.dsem["pool"])

        if upto >= 1:
            with ExitStack() as es:
                bw = lambda c0, c1: [b_win[i] for i in range(c0 // 512, (c1 - 1) // 512 + 1)]
                dma("pool", waobf_d, wao_d, [], [b_waobf])
                dma("pool", wcobf_d, wco_d, [], [b_wcobf])
                dma("pool", woutbf_d, wout_d, [], [b_woutbf])
                xt = [sbt(es, "xt%d" % i, [128, D], F32) for i in range(2)]; b_xt = [Buf(), Buf()]
                junk = sbt(es, "junk", [128, D], BF16); b_junk = Buf()
                st4 = sbt(es, "st4", [128, 4], F32); b_st4 = Buf()
                xn = sbt(es, "xn", [128, D], F32); b_xn = Buf()
                hT = sbt(es, "hT", [128, 8 * 512], BF16); b_hT = [Buf() for _ in range(4)]
                hT3 = hT[:].rearrange("p (k n) -> p k n", k=8)
                sq = sbt(es, "sq", [128, 1280], F32); b_sq = Buf()
                qn = sbt(es, "qn", [128, 1280], F32); b_qn = Buf()
                t2 = sbt(es, "t2", [128, 1280], F32); b_t2 = Buf()
                qr = sbt(es, "qr", [128, 1280], BF16); b_qr = Buf()
                s10 = sbt(es, "s10", [128, 32], F32); b_s10 = Buf()
                Gt = sbt(es, "Gt", [128, 1280], F32); b_Gt = Buf()
                gq = sbt(es, "gq", [128, 256], F32); b_gq = Buf()
                rc = [sbt(es, "rc%d" % i, [128, 128], F32) for i in range(2)]; b_rc = [Buf(), Buf()]
                rs = [sbt(es, "rs%d" % i, [128, 128], F32) for i in range(2)]; b_rs = [Buf(), Buf()]
                QTb = sbt(es, "QTb", [128, 8 * 512], BF16); b_QTb = Buf()
                QTb3 = QTb[:].rearrange("p (h n) -> p h n", h=8)
                sgt = sbt(es, "sgt", [128, 512], F32); b_sgt = Buf()
                ob = [sbt(es, "ob%d" % i, [128, 512], BF16) for i in range(2)]; b_ob = [Buf(), Buf()]
                zpad = sbt(es, "zpad", [128, 8 * 15], BF16); b_zpad = Buf()

                dma("sp", gq[:], bass.AP(tensor=qkg_d.tensor, offset=0, ap=[[0, 128], [1, 256]]), [], [b_gq])
                ts("dve", Gt[:, 0:1024].rearrange("p (h d) -> p h d", h=8), XAP(gq[:, 0:128], [[0, 8], [1, 128]]),
                   float(128 ** -0.5), None, ALU.mult, None, [b_gq], [b_Gt])
                cp("dve", Gt[:, 1024:1280].rearrange("p (h d) -> p h d", h=2), XAP(gq[:, 128:256], [[0, 2], [1, 128]]), [b_gq], [b_Gt])
                S.op("pool", lambda e: e.memset(zpad[:], 0.0), [], [b_zpad])
                gtv = gt_d.rearrange("k p n -> p k n")
                dma("sp", gtv[:, :, 0:15], zpad[:].rearrange("p (k n) -> p k n", k=8), [b_zpad], [b_gtpad])
                dma("sp", gtv[:, :, T + 15:T + 30], zpad[:].rearrange("p (k n) -> p k n", k=8), [b_zpad], [b_gtpad])

                t1r = sbt(es, "t1r", [128, 1280], F32); b_t1r = Buf()

                def p1_tile(src_ap, ti_in_blk, key_tile, is_ctx, pos0, cnt):
                    i2 = cnt % 2
                    a_off, b_off = (16, 24) if is_ctx else (0, 8)
                    if is_ctx:
                        c0, nh, banks = 1024, 2, [pb[4]]
                        src_qk = bank(4, 0, 256)
                    else:
                        c0, nh, banks = 0, 10, [pb[2], pb[3], pb[4]]
                        src_qk = PS[:, 2 * 512:2 * 512 + 1280]
                    w_ = nh * 128

                    def front():
                        dma("sp", xt[i2][:], src_ap, [], [b_xt[i2]])
                        if not is_ctx:
                            dma("sp", rc[i2][:], ropec_d[pos0:pos0 + 128, :], [], [b_rc[i2]])
                            dma("sp", rs[i2][:], ropes_d[pos0:pos0 + 128, :], [], [b_rs[i2]])
                        act(junk[:], xt[i2][:], AF.Square, [b_xt[i2]], [b_junk, b_st4], accum=st4[:, 0:1])
                        ts("dve", st4[:, 1:2], st4[:, 0:1], 1.0 / D, EPS, ALU.mult, ALU.add, [b_st4], [b_st4])
                        act(st4[:, 2:3], st4[:, 1:2], AF.Sqrt, [b_st4], [b_st4])
                        rcp(st4[:, 3:4], st4[:, 2:3], [b_st4], [b_st4])
                        ts("dve", xn[:], xt[i2][:], st4[:, 3:4], None, ALU.mult, None, [b_xt[i2], b_st4], [b_xn])
                        for k in range(8):
                            tr(bank(k // 4, (k % 4) * 128, (k % 4) * 128 + 128), xn[:, k * 128:(k + 1) * 128], ident_f[:],
                               [b_xn, b_idf], [pb[k // 4]])
                        for k in range(8):
                            dst = hT3[:, k, ti_in_blk * 128:(ti_in_blk + 1) * 128]
                            src = bank(k // 4, (k % 4) * 128, (k % 4) * 128 + 128)
                            if k % 2 == 0:
                                act(dst, src, AF.Identity, [pb[k // 4], b_modc], [b_hT[ti_in_blk]],
                                    bias=modc[:, b_off + k:b_off + k + 1], scale=modc[:, a_off + k:a_off + k + 1])
                            else:
                                ts("dve", dst, src, modc[:, a_off + k:a_off + k + 1], modc[:, b_off + k:b_off + k + 1],
                                   ALU.mult, ALU.add, [pb[k // 4], b_modc], [b_hT[ti_in_blk]])
                        for cb in ((2,) if is_ctx else (0, 1, 2)):
                            for k in range(8):
                                mm(bank(2 + cb), hT3[:, k, ti_in_blk * 128:(ti_in_blk + 1) * 128],
                                   win3[:, k, cb * 512:(cb + 1) * 512], k == 0, k == 7,
                                   [b_hT[ti_in_blk]] + bw(cb * 512, cb * 512 + 512), [pb[2 + cb]], silent=(k < 7))

                    def backA():
                        cp("act", Vs[:, key_tile * 256:(key_tile + 1) * 256], bank(4, 256, 512), [pb[4]], [b_V[key_tile]])
                        act(sq[:, c0:c0 + w_], src_qk, AF.Square, banks, [b_sq])
                        S.op("dve", lambda e: e.tensor_reduce(out=s10[:, 0:nh], in_=sq[:, c0:c0 + w_].rearrange("p (h d) -> p h d", h=nh),
                                                              axis=AX.X, op=ALU.add), [b_sq], [b_s10])
                        ts("dve", s10[:, 10:10 + nh], s10[:, 0:nh], 1.0 / 128, EPS, ALU.mult, ALU.add, [b_s10], [b_s10])
                        act(s10[:, 20:20 + nh], s10[:, 10:10 + nh], AF.Sqrt, [b_s10], [b_s10])
                        rcp(s10[:, 0:nh], s10[:, 20:20 + nh], [b_s10], [b_s10])
                        tt("dve", qn[:, c0:c0 + w_].rearrange("p (h d) -> p h d", h=nh), src_qk.rearrange("p (h d) -> p h d", h=nh),
                           XAP(s10[:, 0:nh], [[1, nh], [0, 128]]), ALU.mult, banks + [b_s10], [b_qn])

                    def backB():
                        tt("pool", qn[:, c0:c0 + w_], qn[:, c0:c0 + w_], Gt[:, c0:c0 + w_], ALU.mult, [b_qn, b_Gt], [b_qn])
                        if is_ctx:
                            cp("dve", qr[:, 1024:1280], qn[:, 1024:1280], [b_qn], [b_qr])
                        else:
                            tt("pool", t1r[:].rearrange("p (h d) -> p h d", h=10), qn[:].rearrange("p (h d) -> p h d", h=10),
                               XAP(rc[i2][:], [[0, 10], [1, 128]]), ALU.mult, [b_qn, b_rc[i2]], [b_t1r])
                            for hf in range(2):
                                o_off, i_off = hf * 32, (1 - hf) * 32
                                tt("dve", XAP(t2[:], [[128, 10], [64, 2], [1, 32]], o_off),
                                   XAP(qn[:], [[128, 10], [64, 2], [1, 32]], i_off),
                                   XAP(rs[i2][:], [[0, 10], [64, 2], [1, 32]], o_off), ALU.mult, [b_qn, b_rs[i2]], [b_t2])
                            tt("dve", qr[:], t1r[:], t2[:], ALU.add, [b_t1r, b_t2], [b_qr])
                        if not is_ctx:
                            for h in range(8):
                                tr(bankb(5, h * 128, (h + 1) * 128), qr[:, h * 128:(h + 1) * 128], ident_b[:], [b_qr, b_idb], [pb[5]])
                            cp("act", QTb3[:, :, ti_in_blk * 128:(ti_in_blk + 1) * 128],
                               bankb(5).rearrange("p (h n) -> p h n", h=8), [pb[5]], [b_QTb])
                        for h in range(2):
                            tr(bankb(6, h * 128, (h + 1) * 128), qr[:, 1024 + h * 128:1024 + (h + 1) * 128], ident_b[:],
                               [b_qr, b_idb], [pb[6]])
                        cp("dve", XAP(KT[:, key_tile * 128:(key_tile + 1) * 128], [[NKEY, 2], [1, 128]]),
                           bankb(6, 0, 256).rearrange("p (h n) -> p h n", h=2), [pb[6]], [b_KT[key_tile]])
                    return front, backA, backB

                def run_tiles(tiles, mid_fn=None):
                    n_ = len(tiles)
                    tiles[0][0]()
                    for i_ in range(n_):
                        tiles[i_][1]()
                        if i_ + 1 < n_:
                            tiles[i_ + 1][0]()
                        elif mid_fn is not None:
                            mid_fn()
                        tiles[i_][2]()

                cnt = 0
                ctl = []
                for ci in range(2):
                    ctl.append(p1_tile(ctx_d[ci * 128:(ci + 1) * 128, :], ci, NT + ci, True, 0, cnt))
                    cnt += 1
                run_tiles(ctl)
                for blk in range(8):
                    tl_ = []
                    for ti in range(4):
                        tg = blk * 4 + ti
                        tl_.append(p1_tile(x_d[tg * 128:(tg + 1) * 128, :], ti, tg, False, tg * 128, cnt))
                        cnt += 1
                    obi = [0]

                    def glu_part():
                        for j in range(8):
                            ca = 1536 + j * 128
                            cb_ = 1536 + 1024 + j * 128
                            ba, bb_ = (0, 1) if j % 2 == 0 else (2, 3)
                            for k in range(8):
                                mm(bank(ba), win3[:, k, ca:ca + 128], hT3[:, k, :], k == 0, k == 7, b_hT + bw(ca, ca + 128), [pb[ba]], silent=(k < 7))
                            for k in range(8):
                                mm(bank(bb_), win3[:, k, cb_:cb_ + 128], hT3[:, k, :], k == 0, k == 7, b_hT + bw(cb_, cb_ + 128), [pb[bb_]], silent=(k < 7))
                            act(sgt[:], bank(bb_), AF.Sigmoid, [pb[bb_]], [b_sgt])
                            o = ob[obi[0] % 2]; bo = b_ob[obi[0] % 2]; obi[0] += 1
                            tt("dve", o[:], bank(ba), sgt[:], ALU.mult, [pb[ba], b_sgt], [bo])
                            dma("sp", gt_d[j, :, 15 + blk * 512:15 + (blk + 1) * 512], o[:], [bo], [b_gt[blk]])
                    run_tiles(tl_, glu_part)
                    dma("sp", qt_d.rearrange("h p n -> p h n")[:, :, blk * 512:(blk + 1) * 512], QTb3, [b_QTb], [b_qt[blk]])
                    for j in range(16):
                        cg = 3584 + j * 128
                        bg = 4 + (j % 2)
                        for k in range(8):
                            mm(bank(bg), win3[:, k, cg:cg + 128], hT3[:, k, :], k == 0, k == 7, b_hT + bw(cg, cg + 128), [pb[bg]], silent=(k < 7))
                        o = ob[obi[0] % 2]; bo = b_ob[obi[0] % 2]; obi[0] += 1
                        act(o[:], bank(bg), AF.Sigmoid, [pb[bg]], [bo])
                        dma("sp", sg_d[j, :, blk * 512:(blk + 1) * 512], o[:], [bo], [b_sg[blk]])
                    if upto == 1 and blk == 0:
                        break
                dump("KT", KT[:], b_KT)
                dump("Vs", Vs[:], b_V)
                dump("QTb", QTb[:], [b_QTb])
                S.barrier(exclude=S.dsem["pool"])
                dump("gt", gt_d.rearrange("k p n -> p k n")[:, :, 0:542], [])
                dump("sg", sg_d.rearrange("k p n -> p k n")[:, :, 0:512], [])

        if upto >= 1:
            S.barrier(exclude=S.dsem["pool"])
        w01.close()

        if upto >= 2:
            with ExitStack() as es:
                wao = sbt(es, "wao", [128, 8 * D], BF16); b_wao = Buf()
                wco = sbt(es, "wco", [128, 8 * D], BF16); b_wco = Buf()
                wout = sbt(es, "wout", [128, 8 * D], BF16); b_wout = Buf()
                wao3 = wao[:].rearrange("p (k n) -> p k n", k=8)
                wco3 = wco[:].rearrange("p (k n) -> p k n", k=8)
                wout3 = wout[:].rearrange("p (k n) -> p k n", k=8)
                dma("sp", wao3, waobf_d.rearrange("(k p) n -> p k n", p=128), [b_waobf], [b_wao])
                dma("sp", wco3, wcobf_d.rearrange("(k p) n -> p k n", p=128), [b_wcobf], [b_wco])
                dma("sp", wout3, woutbf_d.rearrange("(k p) n -> p k n", p=128), [b_woutbf], [b_wout])
                dma("pool", wqbf_d, wq_d, [], [b_wqbf])
                dma("pool", keyTbf_d, keyT_d, [], [b_keyTbf])
                cdw = sbt(es, "cdw", [128, 248], F32); b_cdw = Buf()
                dma("sp", cdw[:], cdw_d, [], [b_cdw])
                QTb = sbt(es, "QTb2", [128, 8 * 512], BF16); b_QTb = Buf()
                QTb3 = QTb[:].rearrange("p (h n) -> p h n", h=8)
                gth = sbt(es, "gth", [128, 8 * 542], BF16); b_gth = Buf()
                gth3 = gth[:].rearrange("p (k n) -> p k n", k=8)
                sgl = [sbt(es, "sgl%d" % i, [128, 512], BF16) for i in range(4)]; b_sgl = [Buf() for _ in range(4)]
                psb = [sbt(es, "psb%d" % i, [128, 1024], BF16) for i in range(3)]; b_psb = [Buf() for _ in range(3)]
                rz = sbt(es, "rz", [128, 512], F32); b_rz = Buf()
                OTb = sbt(es, "OTb", [128, 8 * 512], BF16); b_OT = [Buf() for _ in range(8)]
                OTb3 = OTb[:].rearrange("p (h n) -> p h n", h=8)
                ycv = sbt(es, "ycv", [128, 8 * 512], F32); b_ycv = [Buf() for _ in range(8)]
                ycv3 = ycv[:].rearrange("p (k n) -> p k n", k=8)
                sqy = sbt(es, "sqy", [128, 512], F32); b_sqy = Buf()
                cacc = [sbt(es, "cacc%d" % i, [128, 512], F32) for i in range(3)]; b_cacc = [Buf() for _ in range(3)]
                mean = sbt(es, "mean", [128, 512], F32); b_mean = Buf()
                msq = sbt(es, "msq", [128, 512], F32); b_msq = Buf()
                var = sbt(es, "var", [128, 512], F32); b_var = Buf()
                rstd = sbt(es, "rstd", [128, 512], F32); b_rstd = Buf()
                yns = [sbt(es, "yn%d" % i, [128, 512], F32) for i in range(2)]; b_yns = [Buf(), Buf()]
                zT = sbt(es, "zT", [128, 8 * 512], BF16); b_zT = [Buf() for _ in range(8)]
                zT3 = zT[:].rearrange("p (k n) -> p k n", k=8)
                m2 = sbt(es, "m2", [128, 512], F32); b_m2 = Buf()
                tm2 = sbt(es, "tm2", [128, 512], F32); b_tm2 = Buf()
                mgT = sbt(es, "mgT", [128, 8 * 512], BF16); b_mg = [Buf() for _ in range(8)]
                mgT3 = mgT[:].rearrange("p (k n) -> p k n", k=8)
                xt = [sbt(es, "xt2_%d" % i, [128, D], F32) for i in range(2)]; b_xt = [Buf(), Buf()]
                t1 = sbt(es, "t1", [128, D], F32); b_t1 = Buf()
                xs = sbt(es, "xs", [128, D], F32); b_xs_ = Buf()
                junk = sbt(es, "junk2", [128, D], BF16); b_junk = Buf()
                xn = sbt(es, "xn2", [128, D], F32); b_xn = Buf()
                st4 = sbt(es, "st4b", [128, 4], F32); b_st4 = Buf()

                STG = 9
                pcnt = 0
                sgcnt = 0
                xcnt = 0
                nblk = 1 if upto == 2 else 8
                for blk in range(nblk):
                    dma("sp", QTb3, qt_d.rearrange("h p n -> p h n")[:, :, blk * 512:(blk + 1) * 512], [], [b_QTb])
                    dma("sp", gth3, gt_d.rearrange("k p n -> p k n")[:, :, blk * 512:blk * 512 + 542], [], [b_gth])
                    NK_ = NKT
                    steps = [(h_, kt_) for h_ in range(8) for kt_ in range(NK_)]
                    SB = [0, 1, 6, 7]

                    def emitS(si):
                        h_, kt_ = steps[si]
                        kvh_ = h_ // 4
                        bk_ = SB[si % 4]
                        mm(bank(bk_), KT[:, kvh_ * NKEY + kt_ * 128:kvh_ * NKEY + (kt_ + 1) * 128], QTb3[:, h_, :], True, True,
                           [b_QTb], [pb[bk_]])

                    def conv_tap(j, tp):
                        ch = tp % 4
                        acc = ycv3[:, j, :] if ch == 0 else cacc[ch - 1][:]
                        bacc = b_ycv[j] if ch == 0 else b_cacc[ch - 1]
                        if tp == 0:
                            ts("dve", acc, gth3[:, j, 0:512], cdw[:, j * 31:j * 31 + 1], ncol[:, 16 + j:17 + j],
                               ALU.mult, ALU.add, [b_gth, b_cdw, b_ncol], [bacc])
                        elif tp < 4:
                            ts("dve", acc, gth3[:, j, tp:tp + 512], cdw[:, j * 31 + tp:j * 31 + tp + 1], None,
                               ALU.mult, None, [b_gth, b_cdw], [bacc])
                        else:
                            stt(acc, gth3[:, j, tp:tp + 512], cdw[:, j * 31 + tp:j * 31 + tp + 1], acc,
                                ALU.mult, ALU.add, [b_gth, b_cdw, bacc], [bacc])
                    emitS(0)
                    emitS(1)
                    for h in range(8):
                        kvh = h // 4
                        bo, bz = (2, 3) if h % 2 == 0 else (4, 5)
                        j = h
                        for kp in range(NK_ // 2):
                            si = h * NK_ + 2 * kp
                            for d_ in (2, 3):
                                if si + d_ < len(steps):
                                    emitS(si + d_)
                            for tp in (2 * kp, 2 * kp + 1):
                                if tp < 31:
                                    conv_tap(j, tp)
                            b0 = SB[si % 4]
                            p_ = psb[pcnt % 3]; bp = b_psb[pcnt % 3]; pcnt += 1
                            act(p_[:], PS[:, b0 * 512:(b0 + 2) * 512], AF.Exp, [pb[b0], pb[b0 + 1]], [bp])
                            for hf in range(2):
                                kt = 2 * kp + hf
                                mm(bank(bo), Vs[:, kt * 256 + kvh * 128:kt * 256 + (kvh + 1) * 128], p_[:, hf * 512:(hf + 1) * 512],
                                   kt == 0, kt == NK_ - 1, [bp], [pb[bo]])
                                mm(bank(bz), ones_b[:], p_[:, hf * 512:(hf + 1) * 512], kt == 0, kt == NK_ - 1, [bp], [pb[bz]])
                        rcp(rz[:], bank(bz), [pb[bz]], [b_rz])
                        tt("dve", OTb3[:, h, :], bank(bo), rz[:], ALU.mult, [pb[bo], b_rz], [b_OT[h]])
                        tt("pool", cacc[1][:], cacc[1][:], cacc[2][:], ALU.add, [b_cacc[1], b_cacc[2]], [b_cacc[1]])
                        tt("pool", ycv3[:, j, :], ycv3[:, j, :], cacc[0][:], ALU.add, [b_ycv[j], b_cacc[0]], [b_ycv[j]])
                        tt("pool", ycv3[:, j, :], ycv3[:, j, :], cacc[1][:], ALU.add, [b_ycv[j], b_cacc[1]], [b_ycv[j]])
                    for j in range(8):
                        act(sqy[:], ycv3[:, j, :], AF.Square, [b_ycv[j]], [b_sqy])
                        mm(bank(6), ones_f[:], ycv3[:, j, :], j == 0, j == 7, [b_ycv[j], b_1f], [pb[6]])
                        mm(bank(7), ones_f[:], sqy[:], j == 0, j == 7, [b_sqy, b_1f], [pb[7]])
                    if STG < 3:
                        continue
                    ts("dve", mean[:], bank(6), 1.0 / D, None, ALU.mult, None, [pb[6]], [b_mean])
                    tt("dve", msq[:], mean[:], mean[:], ALU.mult, [b_mean], [b_msq])
                    stt(var[:], bank(7), 1.0 / D, msq[:], ALU.mult, ALU.subtract, [pb[7], b_msq], [b_var])
                    ts("dve", var[:], var[:], EPS, None, ALU.add, None, [b_var], [b_var])
                    act(var[:], var[:], AF.Sqrt, [b_var], [b_var])
                    rcp(rstd[:], var[:], [b_var], [b_rstd])
                    for j in range(8):
                        yn = yns[j % 2]; b_yn = b_yns[j % 2]
                        tt("dve", yn[:], ycv3[:, j, :], mean[:], ALU.subtract, [b_ycv[j], b_mean], [b_yn])
                        tt("dve", yn[:], yn[:], rstd[:], ALU.mult, [b_yn, b_rstd], [b_yn])
                        act(zT3[:, j, :], yn[:], AF.Silu, [b_yn, b_ncol], [b_zT[j]],
                            bias=ncol[:, 32 + j:33 + j], scale=ncol[:, 24 + j:25 + j])
                    if STG < 4:
                        continue
                    for j in range(8):
                        sgc = sgl[sgcnt % 4]; bsgc = b_sgl[sgcnt % 4]; sgcnt += 1
                        dma("sp", sgc[:], sg_d[8 + j, :, blk * 512:(blk + 1) * 512], [], [bsgc])
                        sga = sgl[sgcnt % 4]; bsga = b_sgl[sgcnt % 4]; sgcnt += 1
                        dma("sp", sga[:], sg_d[j, :, blk * 512:(blk + 1) * 512], [], [bsga])
                        bc_, ba_ = (6, 7) if j % 2 == 0 else (0, 1)
                        for k in range(8):
                            mm(bank(bc_), wco3[:, k, j * 128:(j + 1) * 128], zT3[:, k, :], k == 0, k == 7, [b_wco, b_zT[k]], [pb[bc_]], silent=(k < 7))
                        tt("dve", m2[:], bank(bc_), sgc[:], ALU.mult, [pb[bc_], bsgc], [b_m2])
                        for k in range(8):
                            mm(bank(ba_), wao3[:, k, j * 128:(j + 1) * 128], OTb3[:, k, :], k == 0, k == 7, [b_wao, b_OT[k]], [pb[ba_]], silent=(k < 7))
                        tt("dve", tm2[:], bank(ba_), sga[:], ALU.mult, [pb[ba_], bsga], [b_tm2])
                        tt("dve", mgT3[:, j, :], tm2[:], m2[:], ALU.add, [b_tm2, b_m2], [b_mg[j]])
                    if blk == 0:
                        dump("mgT", mgT[:], b_mg)
                        dump("OTb", OTb[:], b_OT)
                        dump("zT", zT[:], b_zT)
                    if STG < 5:
                        continue
                    xbufs = []
                    for ti in range(4):
                        tg = blk * 4 + ti
                        x_ = xt[xcnt % 2]; bx = b_xt[xcnt % 2]; xcnt += 1
                        xbufs.append((x_, bx))

                    def t_mm(ti):
                        tg = blk * 4 + ti
                        x_, bx = xbufs[ti]
                        dma("sp", x_[:], x_d[tg * 128:(tg + 1) * 128, :], [], [bx])
                        ob_ = 6 if ti % 2 == 0 else 2
                        for nb in range(2):
                            for k in range(8):
                                mm(bank(ob_ + nb), mgT3[:, k, ti * 128:(ti + 1) * 128], wout3[:, k, nb * 512:(nb + 1) * 512],
                                   k == 0, k == 7, [b_mg[k], b_wout], [pb[ob_ + nb]], silent=(k < 7))

                    def t_chain(ti):
                        tg = blk * 4 + ti
                        x_, bx = xbufs[ti]
                        ob_ = 6 if ti % 2 == 0 else 2
                        tt("dve", t1[:], PS[:, ob_ * 512:(ob_ + 2) * 512], g1b[:], ALU.mult, [pb[ob_], pb[ob_ + 1], b_g1b], [b_t1])
                        tt("dve", xs[:], t1[:], x_[:], ALU.add, [b_t1, bx], [b_xs_])
                        dma("sp", xs_d[tg * 128:(tg + 1) * 128, :], xs[:], [b_xs_], [b_xs[tg]])
                        act(junk[:], xs[:], AF.Square, [b_xs_], [b_junk, b_st4], accum=st4[:, 0:1])
                        ts("dve", st4[:, 1:2], st4[:, 0:1], 1.0 / D, EPS, ALU.mult, ALU.add, [b_st4], [b_st4])
                        act(st4[:, 2:3], st4[:, 1:2], AF.Sqrt, [b_st4], [b_st4])
                        rcp(st4[:, 3:4], st4[:, 2:3], [b_st4], [b_st4])
                        ts("dve", xn[:], xs[:], st4[:, 3:4], None, ALU.mult, None, [b_xs_, b_st4], [b_xn])

                    def t_trev(ti):
                        tb_ = 0 if ti % 2 == 0 else 4
                        for k in range(8):
                            tr(bank(tb_ + k // 4, (k % 4) * 128, (k % 4) * 128 + 128), xn[:, k * 128:(k + 1) * 128], ident_f[:],
                               [b_xn, b_idf], [pb[tb_ + k // 4]])
                        for k in range(8):
                            dst = zT3[:, k, ti * 128:(ti + 1) * 128]
                            src = bank(tb_ + k // 4, (k % 4) * 128, (k % 4) * 128 + 128)
                            ts("dve", dst, src, modc[:, 32 + k:33 + k], modc[:, 40 + k:41 + k],
                               ALU.mult, ALU.add, [pb[tb_ + k // 4], b_modc], [b_zT[k]])
                    t_mm(0)
                    t_mm(1)
                    for ti in range(4):
                        t_chain(ti)
                        if ti + 2 < 4:
                            t_mm(ti + 2)
                        t_trev(ti)
                    if STG >= 8:
                        dma("sp", h2_d.rearrange("k p n -> p k n")[:, :, blk * 512:(blk + 1) * 512], zT3, b_zT, [b_h2[blk]])
                S.barrier(exclude=S.dsem["pool"])
                dump("xs0", xs_d[0:512, :], [])
                dump("h2", h2_d.rearrange("k p n -> p k n")[:, :, 0:512], [])

        mid.close()

        if upto >= 3:
            with ExitStack() as es:
                wq = sbt(es, "wq", [128, 8 * 2048], BF16); b_wq = Buf()
                wq3 = wq[:].rearrange("p (k n) -> p k n", k=8)
                dma("sp", wq3, wqbf_d.rearrange("(k p) n -> p k n", p=128), [b_wqbf], [b_wq])
                keyT = sbt(es, "keyT", [128, 2048], BF16); b_keyT = Buf()
                dma("sp", keyT[:], keyTbf_d, [b_keyTbf], [b_keyT])
                h2Ts = [sbt(es, "h2T%d" % i, [128, 8 * 512], BF16) for i in range(2)]; b_h2Ts = [Buf(), Buf()]
                qT = sbt(es, "qT", [128, 16 * 512], BF16); b_qT = [Buf() for _ in range(16)]
                qT3 = qT[:].rearrange("p (c n) -> p c n", c=16)
                thr = sbt(es, "thr", [128, 16], F32); b_thr = Buf()
                io16 = sbt(es, "io16", [128, 16], F32); b_io16 = Buf()
                rtb = sbt(es, "rtb", [128, 3 * 512], F32); b_rtb = Buf()
                rtb3 = rtb[:].rearrange("p (a n) -> p a n", a=3)

                class _NS:
                    pass
                sets = []
                for s_ in range(2):
                    n_ = _NS()
                    n_.sc = sbt(es, "sc%d" % s_, [128, 2048], F32); n_.b_sc = Buf()
                    n_.scr2 = sbt(es, "scr2%d" % s_, [128, 2048], F32)
                    n_.m16 = sbt(es, "m16%d" % s_, [128, 256], F32)
                    n_.b_m16a = [Buf() for _ in range(16)]; n_.b_m16b = [Buf() for _ in range(16)]
                    n_.b_i16a = [Buf() for _ in range(16)]; n_.b_i16b = [Buf() for _ in range(16)]
                    n_.b_scr2g = [Buf() for _ in range(16)]
                    n_.b_b16a = [Buf() for _ in range(8)]; n_.b_b16b = [Buf() for _ in range(8)]
                    n_.b_p16a = [Buf() for _ in range(8)]; n_.b_p16b = [Buf() for _ in range(8)]
                    n_.b_cs2h = [Buf() for _ in range(8)]
                    n_.i16 = sbt(es, "i16%d" % s_, [128, 256], U32)
                    n_.i16f = sbt(es, "i16f%d" % s_, [128, 256], F32); n_.b_i16f = Buf()
                    n_.cs_ = sbt(es, "cs_%d" % s_, [128, 2048], F32); n_.b_cs_ = Buf()
                    n_.cs2 = sbt(es, "cs2%d" % s_, [128, 2048], F32)
                    n_.b16 = sbt(es, "b16%d" % s_, [128, 128], F32)
                    n_.p16 = sbt(es, "p16%d" % s_, [128, 128], U32)
                    n_.p16f = sbt(es, "p16f%d" % s_, [128, 128], F32); n_.b_p16f = Buf()
                    n_.af = sbt(es, "af%d" % s_, [128, 128], F32); n_.b_af = Buf()
                    n_.bf_ = sbt(es, "bf_%d" % s_, [128, 128], F32); n_.b_bf = Buf()
                    n_.E = sbt(es, "E%d" % s_, [128, 2048], F32); n_.b_E = Buf()
                    n_.sel = sbt(es, "sel%d" % s_, [128, 3 * 128], F32); n_.b_sel = Buf()
                    n_.z8 = sbt(es, "z8%d" % s_, [128, 16], F32); n_.b_z8 = Buf()
                    n_.pb0 = 4 * s_
                    sets.append(n_)
                S.op("pool", lambda e: e.iota(out=io16[:], pattern=[[1, 16]], base=0, channel_multiplier=0,
                                              allow_small_or_imprecise_dtypes=True), [], [b_io16])
                ts("dve", thr[:], io16[:], 16.0, None, ALU.mult, None, [b_io16], [b_thr])

                def tile_prog(blk, ti, n_):
                    Q = []

                    def q(fn, *a_, **k_):
                        Q.append((fn, a_, k_))
                    sc, scr2, m16, i16, i16f, cs_, cs2 = n_.sc, n_.scr2, n_.m16, n_.i16, n_.i16f, n_.cs_, n_.cs2
                    b16, p16, p16f, af, bf_, E, sel, z8 = n_.b16, n_.p16, n_.p16f, n_.af, n_.bf_, n_.E, n_.sel, n_.z8
                    pb0 = n_.pb0
                    for c in range(16):
                        bk_ = pb0 + c // 4
                        q(mm, bank(bk_, (c % 4) * 128, (c % 4) * 128 + 128), qT3[:, c, ti * 128:(ti + 1) * 128],
                          keyT[:, c * 128:(c + 1) * 128], True, True, [b_qT[c], b_keyT], [pb[bk_]])
                    q(cp, "act", sc[:], PS[:, pb0 * 512:pb0 * 512 + 2048], [pb[pb0 + i_] for i_ in range(4)], [n_.b_sc])
                    for stp in range(5):
                        for g in range(16):
                            sg_ = sc[:, g * 128:(g + 1) * 128]
                            sr_ = scr2[:, g * 128:(g + 1) * 128]
                            if stp == 0:
                                q(S.op, "dve", lambda e, g=g, sg_=sg_: e.max(out=m16[:, g * 16:g * 16 + 8], in_=sg_), [n_.b_sc], [n_.b_m16a[g]])
                            elif stp == 1:
                                q(S.op, "dve", lambda e, g=g, sg_=sg_: e.max_index(out=i16[:, g * 16:g * 16 + 8], in_max=m16[:, g * 16:g * 16 + 8],
                                                                                   in_values=sg_), [n_.b_sc, n_.b_m16a[g]], [n_.b_i16a[g]])
                            elif stp == 2:
                                q(S.op, "dve", lambda e, g=g, sg_=sg_, sr_=sr_: e.match_replace(out=sr_, in_to_replace=m16[:, g * 16:g * 16 + 8],
                                                                                                in_values=sg_, imm_value=-1e30),
                                  [n_.b_sc, n_.b_m16a[g]], [n_.b_scr2g[g]])
                            elif stp == 3:
                                q(S.op, "dve", lambda e, g=g, sr_=sr_: e.max(out=m16[:, g * 16 + 8:g * 16 + 16], in_=sr_), [n_.b_scr2g[g]], [n_.b_m16b[g]])
                            else:
                                q(S.op, "dve", lambda e, g=g, sr_=sr_: e.max_index(out=i16[:, g * 16 + 8:g * 16 + 16],
                                                                                   in_max=m16[:, g * 16 + 8:g * 16 + 16], in_values=sr_),
                                  [n_.b_scr2g[g], n_.b_m16b[g]], [n_.b_i16b[g]])
                    b_m16 = n_.b_m16a + n_.b_m16b
                    b_i16 = n_.b_i16a + n_.b_i16b
                    q(cp, "dve", i16f[:], i16[:], b_i16, [n_.b_i16f])
                    q(tt, "dve", XAP(cs_[:], [[256, 8], [16, 16], [1, 16]]), XAP(m16[:], [[32, 8], [1, 16], [0, 16]]),
                      XAP(m16[:], [[32, 8], [0, 16], [1, 16]], 16), ALU.add, b_m16, [n_.b_cs_])
                    for stp in range(5):
                        for h in range(8):
                            ch = cs_[:, h * 256:(h + 1) * 256]
                            ch2 = cs2[:, h * 256:(h + 1) * 256]
                            if stp == 0:
                                q(S.op, "dve", lambda e, h=h, ch=ch: e.max(out=b16[:, h * 16:h * 16 + 8], in_=ch), [n_.b_cs_], [n_.b_b16a[h]])
                            elif stp == 1:
                                q(S.op, "dve", lambda e, h=h, ch=ch: e.max_index(out=p16[:, h * 16:h * 16 + 8], in_max=b16[:, h * 16:h * 16 + 8],
                                                                                 in_values=ch), [n_.b_cs_, n_.b_b16a[h]], [n_.b_p16a[h]])
                            elif stp == 2:
                                q(S.op, "dve", lambda e, h=h, ch=ch, ch2=ch2: e.match_replace(out=ch2, in_to_replace=b16[:, h * 16:h * 16 + 8],
                                                                                              in_values=ch, imm_value=-1e30),
                                  [n_.b_cs_, n_.b_b16a[h]], [n_.b_cs2h[h]])
                            elif stp == 3:
                                q(S.op, "dve", lambda e, h=h, ch2=ch2: e.max(out=b16[:, h * 16 + 8:h * 16 + 16], in_=ch2), [n_.b_cs2h[h]], [n_.b_b16b[h]])
                            else:
                                q(S.op, "dve", lambda e, h=h, ch2=ch2: e.max_index(out=p16[:, h * 16 + 8:h * 16 + 16],
                                                                                   in_max=b16[:, h * 16 + 8:h * 16 + 16], in_values=ch2),
                                  [n_.b_cs2h[h], n_.b_b16b[h]], [n_.b_p16b[h]])
                    b_b16l = n_.b_b16a + n_.b_b16b
                    b_p16l = n_.b_p16a + n_.b_p16b
                    q(cp, "dve", p16f[:], p16[:], b_p16l, [n_.b_p16f])
                    q(tt, "dve", XAP(E[:], [[15, 128], [1, 15]]), XAP(p16f[:], [[1, 128], [0, 15]]),
                      XAP(thr[:], [[0, 128], [1, 15]], 1), ALU.is_ge, [n_.b_p16f, b_thr], [n_.b_E])
                    q(S.op, "dve", lambda e: e.tensor_reduce(out=af[:], in_=XAP(E[:], [[15, 128], [1, 15]]), axis=AX.X, op=ALU.add),
                      [n_.b_E], [n_.b_af])
                    q(stt, bf_[:], af[:], -16.0, p16f[:], ALU.mult, ALU.add, [n_.b_af, n_.b_p16f], [n_.b_bf])
                    for (src, bsrc, off, dsti) in ((af, n_.b_af, 0, 0), (bf_, n_.b_bf, 16, 1)):
                        q(tt, "dve", XAP(E[:], [[16, 128], [1, 16]]), XAP(src[:], [[1, 128], [0, 16]]),
                          XAP(io16[:], [[0, 128], [1, 16]]), ALU.is_equal, [bsrc, b_io16], [n_.b_E])
                        q(tt, "dve", XAP(E[:], [[256, 8], [16, 16], [1, 16]]), XAP(E[:], [[256, 8], [16, 16], [1, 16]]),
                          XAP(i16f[:], [[32, 8], [0, 16], [1, 16]], off), ALU.mult, [n_.b_E, n_.b_i16f], [n_.b_E])
                        q(S.op, "dve", lambda e, dsti=dsti: e.tensor_reduce(out=sel[:, dsti * 128:(dsti + 1) * 128],
                                                                             in_=XAP(E[:], [[16, 128], [1, 16]]), axis=AX.X, op=ALU.add),
                          [n_.b_E], [n_.b_sel])
                    q(tt, "dve", XAP(E[:], [[16, 8], [1, 16]]), XAP(b16[:], [[16, 8], [1, 16]]), XAP(b16[:], [[16, 8], [0, 16]]),
                      ALU.subtract, b_b16l, [n_.b_E])
                    q(act, E[:, 0:128], E[:, 0:128], AF.Exp, [n_.b_E], [n_.b_E])
                    q(S.op, "dve", lambda e: e.tensor_reduce(out=z8[:, 0:8], in_=XAP(E[:], [[16, 8], [1, 16]]), axis=AX.X, op=ALU.add),
                      [n_.b_E], [n_.b_z8])
                    q(rcp, z8[:, 8:16], z8[:, 0:8], [n_.b_z8], [n_.b_z8])
                    q(tt, "dve", XAP(sel[:], [[16, 8], [1, 16]], 256), XAP(E[:], [[16, 8], [1, 16]]), XAP(z8[:], [[1, 8], [0, 16]], 8),
                      ALU.mult, [n_.b_E, n_.b_z8], [n_.b_sel])
                    if blk == 0 and ti == 0:
                        q(dump, "sel", sel[:], [n_.b_sel])
                    for a3 in range(3):
                        q(tr, bank(pb0, a3 * 128, (a3 + 1) * 128), sel[:, a3 * 128:(a3 + 1) * 128], ident_f[:], [n_.b_sel, b_idf], [pb[pb0]])
                    q(cp, "act", rtb3[:, :, ti * 128:(ti + 1) * 128], bank(pb0, 0, 384).rearrange("p (a n) -> p a n", a=3), [pb[pb0]], [b_rtb])
                    return Q

                nblk = 1 if upto == 3 else 8
                def load_h2(blk_):
                    dma("sp", h2Ts[blk_ % 2][:].rearrange("p (k n) -> p k n", k=8),
                        h2_d.rearrange("k p n -> p k n")[:, :, blk_ * 512:(blk_ + 1) * 512], [], [b_h2Ts[blk_ % 2]])
                load_h2(0)
                for blk in range(nblk):
                    if blk + 1 < nblk:
                        load_h2(blk + 1)
                    h2T3 = h2Ts[blk % 2][:].rearrange("p (k n) -> p k n", k=8)
                    b_h2T = b_h2Ts[blk % 2]
                    for c in range(16):
                        bq = 4 + c % 2
                        for k in range(8):
                            mm(bank(bq), wq3[:, k, c * 128:(c + 1) * 128], h2T3[:, k, :], k == 0, k == 7, [b_wq, b_h2T], [pb[bq]], silent=(k < 7))
                        cp("act", qT3[:, c, :], bank(bq), [pb[bq]], [b_qT[c]])
                    if blk == 0:
                        cast_tables(after=[b_qT[15]])
                    for tp_ in range(2):
                        QA = tile_prog(blk, 2 * tp_, sets[0])
                        QB = tile_prog(blk, 2 * tp_ + 1, sets[1])
                        for i_ in range(max(len(QA), len(QB))):
                            for Q_ in (QA, QB):
                                if i_ < len(Q_):
                                    fn_, a_, k_ = Q_[i_]
                                    fn_(*a_, **k_)
                    dma("sp", rt_d.rearrange("a p n -> p a n")[:, :, blk * 512:(blk + 1) * 512], rtb3, [b_rtb], [b_rt[blk]])
                S.barrier(exclude=S.dsem["pool"])

        if upto >= 4:
            with ExitStack() as es:
                Wds = [sbt(es, "Wd%d" % i, [128, GP * 128], BF16) for i in range(2)]
                b_Wds = [[Buf() for _ in range(GP // 4)] for _ in range(2)]
                NSB = 3
                UTs = [sbt(es, "UTs%d" % i, [128, 2 * 1024], BF16) for i in range(NSB)]; b_UTs = [Buf() for _ in range(NSB)]
                Vcs = [sbt(es, "Vcs%d" % i, [128, 2 * 1024], BF16) for i in range(NSB)]; b_Vcs = [Buf() for _ in range(NSB)]
                h2gs = [sbt(es, "h2g%d" % i, [128, 8 * GP], BF16) for i in range(2)]; b_h2gs = [Buf(), Buf()]
                rtgs = [sbt(es, "rtg%d" % i, [128, 3 * GP], F32) for i in range(2)]; b_rtgs = [Buf(), Buf()]
                iof = sbt(es, "iof", [128, 128], F32); b_iof = Buf()
                iob = sbt(es, "iob", [128, 128], BF16); b_iob = Buf()
                gl = [sbt(es, "gl%d" % i, [128, GP], BF16) for i in range(2)]; b_gl = [Buf(), Buf()]
                At = [sbt(es, "At%d" % i, [128, GP], BF16) for i in range(2)]; b_At = [Buf(), Buf()]
                g2b = sbt(es, "g2b", [128, D], F32); b_g2b = Buf()
                fgb = sbt(es, "fgb", [128, D], F32); b_fgb = Buf()
                xsl = sbt(es, "xsl", [128, D], F32); b_xsl = Buf()
                yt = sbt(es, "yt", [128, D], F32); b_yt = Buf()
                yo = sbt(es, "yo", [128, D], F32); b_yo = Buf()
                junk = sbt(es, "junk3", [128, D], BF16); b_junk = Buf()
                st4 = sbt(es, "st4c", [128, 4], F32); b_st4 = Buf()
                dma("sp", g2b[:], bass.AP(tensor=g2_d.tensor, offset=0, ap=[[0, 128], [1, D]]), [b_g2d], [b_g2b])
                dma("sp", fgb[:], bass.AP(tensor=fg_d.tensor, offset=0, ap=[[0, 128], [1, D]]), [], [b_fgb])
                S.op("pool", lambda e: e.iota(out=iof[:], pattern=[[1, 128]], base=0, channel_multiplier=0,
                                              allow_small_or_imprecise_dtypes=True), [], [b_iof])
                cp("dve", iob[:], iof[:], [b_iof], [b_iob])
                utv = utbf_d.rearrange("(c p) n -> p c n", p=128)
                vtv = vbf_d.rearrange("(c p) n -> p c n", p=128)
                ngrp = 1 if upto == 4 else T // GP
                ocnt = [0]

                def load_grp(grp):
                    t0 = grp * GP
                    gi = grp % 2
                    dma("sp", h2gs[gi][:].rearrange("p (k n) -> p k n", k=8), h2_d.rearrange("k p n -> p k n")[:, :, t0:t0 + GP], [], [b_h2gs[gi]])
                    dma("sp", rtgs[gi][:].rearrange("p (a n) -> p a n", a=3), rt_d.rearrange("a p n -> p a n")[:, :, t0:t0 + GP], [], [b_rtgs[gi]])

                NOH = 8
                oh1 = [sbt(es, "oh1b_%d" % i, [128, 128], BF16) for i in range(NOH)]; b_oh1 = [Buf() for _ in range(NOH)]
                oh2 = [sbt(es, "oh2b_%d" % i, [128, 128], BF16) for i in range(NOH)]; b_oh2 = [Buf() for _ in range(NOH)]

                def wd_oh(grp, t):
                    gi = grp % 2
                    rtg3 = rtgs[gi][:].rearrange("p (a n) -> p a n", a=3)
                    o1 = oh1[t % NOH]; bo1 = b_oh1[t % NOH]
                    o2 = oh2[t % NOH]; bo2 = b_oh2[t % NOH]
                    ts("dve", o1[:], iob[:], rtg3[:, 0, t:t + 1], None, ALU.is_equal, None, [b_iob, b_rtgs[gi]], [bo1])
                    ts("dve", o2[:], iob[:], rtg3[:, 1, t:t + 1], rtg3[:, 2, t:t + 1], ALU.is_equal, ALU.mult, [b_iob, b_rtgs[gi]], [bo2])

                def wd_mm(grp, t):
                    bk = 6 + (t // 4) % 2
                    mm(bank(bk, (t % 4) * 128, (t % 4) * 128 + 128), oh1[t % NOH][:], oh2[t % NOH][:], True, True,
                       [b_oh1[t % NOH], b_oh2[t % NOH]], [pb[bk]])

                def wd_cp(grp, t4):
                    gi = grp % 2
                    bk = 6 + t4 % 2
                    cp("act", Wds[gi][:, t4 * 512:(t4 + 1) * 512], bank(bk), [pb[bk]], [b_Wds[gi][t4]])

                def wd_stage(grp, s):
                    n = GP // 2
                    if 0 <= s < n:
                        wd_oh(grp, 2 * s); wd_oh(grp, 2 * s + 1)
                    if 0 <= s - 1 < n:
                        wd_mm(grp, 2 * (s - 1)); wd_mm(grp, 2 * (s - 1) + 1)
                    if s - 3 >= 0 and (s - 3) % 2 == 1 and (s - 3) < n:
                        wd_cp(grp, (s - 3) // 2)

                def load2(grp_, cq):
                    i3 = (grp_ * 64 + cq) % NSB
                    dma("sp", UTs[i3][:].rearrange("p (c n) -> p c n", c=2), utv[:, cq * 2:(cq + 1) * 2, :], [b_ut[cq // 8]], [b_UTs[i3]])
                    dma("sp", Vcs[i3][:].rearrange("p (c n) -> p c n", c=2), vtv[:, cq * 2:(cq + 1) * 2, :], [b_vt[cq // 8]], [b_Vcs[i3]])

                def emitH(grp_, c2):
                    i3 = (grp_ * 64 + c2 // 2) % NSB
                    cc = c2 % 2
                    bh = 4 + c2 % 2
                    h2g3_ = h2gs[grp_ % 2][:].rearrange("p (k n) -> p k n", k=8)
                    for k in range(8):
                        mm(bank(bh, 0, GP), UTs[i3][:, cc * 1024 + k * 128:cc * 1024 + (k + 1) * 128], h2g3_[:, k, :],
                           k == 0, k == 7, [b_UTs[i3], b_h2gs[grp_ % 2]], [pb[bh]], silent=(k < 7))

                xsls = [xsl, sbt(es, "xsl1", [128, D], F32)]; b_xsls = [b_xsl, Buf()]
                yts = [yt, sbt(es, "yt1", [128, D], F32)]; b_yts = [b_yt, Buf()]
                load_grp(0)
                for s in range(GP // 2 + 4):
                    wd_stage(0, s)
                load2(0, 0)
                load2(0, 1)
                emitH(0, 0)
                for grp in range(ngrp):
                    gi = grp % 2
                    t0 = grp * GP
                    Wd = Wds[gi]; b_Wd = b_Wds[gi]
                    if grp + 1 < ngrp:
                        load_grp(grp + 1)
                    for tl in range(GP // 128):
                        tg = t0 // 128 + tl
                        dma("sp", xsls[tl][:], xs_d[tg * 128:(tg + 1) * 128, :], [], [b_xsls[tl]])
                    for c2 in range(128):
                        cq, cc = c2 // 2, c2 % 2
                        if cc == 0 and cq + 2 < 64:
                            load2(grp, cq + 2)
                        if c2 + 1 < 128:
                            emitH(grp, c2 + 1)
                        i3 = (grp * 64 + cq) % NSB
                        bh = 4 + c2 % 2
                        g_ = gl[c2 % 2]; bg_ = b_gl[c2 % 2]
                        a_ = At[c2 % 2]; ba_ = b_At[c2 % 2]
                        act(g_[:], bank(bh, 0, GP), AF.Gelu, [pb[bh]], [bg_])
                        tt("dve", a_[:], g_[:], XAP(Wd[:], [[128, GP]], c2), ALU.mult, [bg_] + b_Wd, [ba_])
                        for tl in range(GP // 128):
                            for nb in range(2):
                                bo = tl * 2 + nb
                                mm(bank(bo), a_[:, tl * 128:(tl + 1) * 128], Vcs[i3][:, cc * 1024 + nb * 512:cc * 1024 + (nb + 1) * 512],
                                   c2 == 0, c2 == 127, [ba_, b_Vcs[i3]], [pb[bo]], silent=(bo < GP // 64 - 1))
                        if grp + 1 < ngrp:
                            wd_stage(grp + 1, c2)
                    if grp + 1 < ngrp:
                        for s in range(128, GP // 2 + 4):
                            wd_stage(grp + 1, s)
                        load2(grp + 1, 0)
                        load2(grp + 1, 1)
                        emitH(grp + 1, 0)
                    for tl in range(GP // 128):
                        tt("dve", yts[tl][:], PS[:, tl * 1024:(tl + 1) * 1024], g2b[:], ALU.mult, [pb[tl * 2], pb[tl * 2 + 1], b_g2b], [b_yts[tl]])
                    for tl in range(GP // 128):
                        tg = t0 // 128 + tl
                        yt_ = yts[tl]; b_yt_ = b_yts[tl]
                        tt("pool", yt_[:], yt_[:], xsls[tl][:], ALU.add, [b_yt_, b_xsls[tl]], [b_yt_])
                        act(junk[:], yt_[:], AF.Square, [b_yt_], [b_junk, b_st4], accum=st4[:, 0:1])
                        ts("dve", st4[:, 1:2], st4[:, 0:1], 1.0 / D, EPS, ALU.mult, ALU.add, [b_st4], [b_st4])
                        act(st4[:, 2:3], st4[:, 1:2], AF.Sqrt, [b_st4], [b_st4])
                        rcp(st4[:, 3:4], st4[:, 2:3], [b_st4], [b_st4])
                        stt(yo[:], yt_[:], st4[:, 3:4], fgb[:], ALU.mult, ALU.mult, [b_yt_, b_st4, b_fgb], [b_yo])
                        dma("sp", out_d[tg * 128:(tg + 1) * 128, :], yo[:], [b_yo], [])
        S.emit()
    return nc


def host_prep(inputs):
    f = lambda a: np.ascontiguousarray(np.asarray(a, dtype=np.float32))
    col = lambda v: f(np.asarray(v).reshape(8, 128).T)
    shared = {}
    shared["w_mod"] = f(inputs["w_mod"][0])
    shared["b_mod"] = f(inputs["b_mod"][0].reshape(1, -1))
    shared["ncol"] = f(np.concatenate([col(inputs["norm1_g"][0]), col(inputs["norm2_g"][0]), col(inputs["conv_b"][0]),
                                       col(inputs["conv_ln_g"][0]), col(inputs["conv_ln_b"][0])], axis=1))
    shared["w_in"] = f(inputs["w_in"][0])
    shared["qkg"] = f(np.concatenate([inputs["q_norm_g"][0], inputs["k_norm_g"][0]]).reshape(1, 256))
    shared["w_attn_o"] = f(inputs["w_attn_o"][0])
    cdw = np.asarray(inputs["conv_dw"][0])
    shared["cdw"] = f(cdw.T.reshape(8, 128, 31).transpose(1, 0, 2).reshape(128, 8 * 31))
    shared["w_conv_o"] = f(inputs["w_conv_o"][0])
    shared["w_out"] = f(inputs["w_out"][0])
    shared["peer_wq"] = f(inputs["peer_wq"][0])
    keys = np.asarray(inputs["peer_keys"][0])
    shared["keyT"] = f(keys.reshape(16, 128, 128).transpose(2, 0, 1).reshape(128, 16 * 128))
    U = np.asarray(inputs["peer_u"][0])
    shared["ut_l"] = f(U.reshape(128, 128, 8, 128).transpose(1, 3, 2, 0).reshape(16384, 1024))
    V = np.asarray(inputs["peer_v"][0])
    shared["v_l"] = f(V.reshape(128, 128, 1024).transpose(1, 0, 2).reshape(16384, 1024))
    shared["fg"] = f(np.asarray(inputs["final_norm_g"]).reshape(1, -1))
    t = np.arange(T)
    row = (t // 64).astype(np.float32)
    colp = (t % 64).astype(np.float32)
    inv = (np.float32(10000.0) ** (-np.arange(0, 64, 2, dtype=np.float32) / np.float32(64))).astype(np.float32)
    ar = row[:, None] * inv[None, :]
    ac = colp[:, None] * inv[None, :]
    cr, sr, cc, sc = np.cos(ar), np.sin(ar), np.cos(ac), np.sin(ac)
    shared["rope_c"] = f(np.concatenate([cr, cr, cc, cc], axis=1))
    shared["rope_s"] = f(np.concatenate([-sr, sr, -sc, sc], axis=1))
    in_maps = []
    cctx = col(inputs["c_ctx"])
    for b in range(8):
        m = dict(shared)
        m["x"] = f(inputs["x"][b])
        m["ctx"] = f(inputs["ctx"][b])
        m["ccol"] = f(np.concatenate([col(inputs["c"][b]), cctx], axis=1))
        in_maps.append(m)
    return in_maps


def kernel(**inputs):
    in_maps = host_prep(inputs)
    nc = build()
    res = run_bass_kernel_spmd(nc, in_maps, core_ids=list(range(8)))
    return np.stack([np.asarray(r["out"], dtype=np.float32) for r in res.results], axis=0)
```

```python
import numpy as np
from contextlib import ExitStack
import concourse.bass as bass
import concourse.mybir as mybir
from concourse.bass_utils import run_bass_kernel_spmd

F32 = mybir.dt.float32
BF16 = mybir.dt.bfloat16
I32 = mybir.dt.int32
U32 = mybir.dt.uint32
AF = mybir.ActivationFunctionType
ALU = mybir.AluOpType
AX = mybir.AxisListType

T = 4096
NT = 32
CTXN = 256
NKEY = T + CTXN
NKT = NKEY // 128
D = 1024
KC = 8
INW = 5632
EPS = 1e-6
GP = 256
ENGS = ("pe", "act", "dve", "pool", "sp")


class Buf:
    __slots__ = ("name", "lw", "rd")

    def __init__(self, name=""):
        self.name = name
        self.lw = None
        self.rd = {}


class Sched:
    def __init__(self, nc, n_dma_sems=(("sp", 16), ("pool", 40), ("act", 4))):
        self.nc = nc
        self.q = {e: [] for e in ENGS}
        self.sems = {}
        self.cnt = {}
        for e in ENGS:
            self.sems[e] = nc.alloc_semaphore(name="s_" + e)
            self.cnt[e] = 0
        self.dsem = {}
        self.drr = {}
        for e, n in n_dma_sems:
            self.dsem[e] = []
            for i in range(n):
                k = "d_%s_%d" % (e, i)
                self.sems[k] = nc.alloc_semaphore(name=k)
                self.cnt[k] = 0
                self.dsem[e].append(k)
            self.drr[e] = 0
        self.seen = {e: {} for e in ENGS}
        self.nops = 0

    def _deps(self, reads, writes):
        deps = {}

        def need(tok):
            if tok is None:
                return
            k, v = tok
            if deps.get(k, 0) < v:
                deps[k] = v
        for r in reads:
            need(r.lw)
        for w in writes:
            need(w.lw)
            for k, v in w.rd.items():
                need((k, v))
        return deps

    def _push(self, eng, deps, fn, sk, inc, reads, writes, silent=False):
        waits = []
        seen = self.seen[eng]
        for k, v in deps.items():
            if k == eng and v > self.cnt[k]:
                continue
            if seen.get(k, 0) < v:
                seen[k] = v
                waits.append((k, v))
        if silent:
            val = self.cnt[sk] + inc
            self.q[eng].append((waits, fn, None, 0))
        else:
            self.cnt[sk] += inc
            val = self.cnt[sk]
            self.q[eng].append((waits, fn, sk, inc))
        for r in reads:
            if r.rd.get(sk, 0) < val:
                r.rd[sk] = val
        for w in writes:
            w.lw = (sk, val)
            w.rd = {}
        self.nops += 1

    def op(self, eng, fn, reads=(), writes=(), silent=False):
        self._push(eng, self._deps(reads, writes), fn, eng, 1, reads, writes, silent)

    def dma(self, eng, fn, reads=(), writes=()):
        deps = self._deps(reads, writes)
        k = self.dsem[eng][self.drr[eng] % len(self.dsem[eng])]
        self.drr[eng] += 1
        if self.cnt[k] > 0 and deps.get(k, 0) < self.cnt[k]:
            deps[k] = self.cnt[k]
        self._push(eng, deps, fn, k, 16, reads, writes)

    def barrier(self, exclude=()):
        for e in ENGS:
            waits = []
            for k, v in self.cnt.items():
                if k in exclude or v == 0:
                    continue
                if self.seen[e].get(k, 0) < v:
                    self.seen[e][k] = v
                    waits.append((k, v))
            if waits:
                self.q[e].append((waits, None, None, 0))

    def emit(self):
        nc = self.nc
        sems = self.sems
        q = self.q
        cnt = self.cnt
        with nc.Block() as block:
            def mk(ename):
                def body(e):
                    for waits, fn, sk, inc in q[ename]:
                        for k, v in waits:
                            e.wait_ge(sems[k], v)
                        if fn is not None:
                            if sk is None:
                                fn(e)
                            else:
                                fn(e).then_inc(sems[sk], inc)
                    if ename == "sp":
                        for k, v in cnt.items():
                            if v > 0:
                                e.wait_ge(sems[k], v)
                return body
            block.tensor(mk("pe"))
            block.scalar(mk("act"))
            block.vector(mk("dve"))
            block.gpsimd(mk("pool"))
            block.sync(mk("sp"))


def XAP(base, dims, off=0):
    return bass.AP(tensor=base.tensor, offset=base.offset + off,
                   ap=[list(base.ap[0])] + [list(d) for d in dims])


def build(upto=9, dbg=()):
    nc = bass.Bass("TRN2", target_bir_lowering=False)
    dt_in = lambda n, s, d=F32: nc.dram_tensor(n, list(s), d, kind="ExternalInput").ap()
    x_d = dt_in("x", [T, D])
    ctx_d = dt_in("ctx", [CTXN, D])
    ccol_d = dt_in("ccol", [128, 16])
    wmod_d = dt_in("w_mod", [D, 6 * D])
    bmod_d = dt_in("b_mod", [1, 6 * D])
    ncol_d = dt_in("ncol", [128, 40])
    win_d = dt_in("w_in", [D, INW])
    qkg_d = dt_in("qkg", [1, 256])
    wao_d = dt_in("w_attn_o", [D, D])
    cdw_d = dt_in("cdw", [128, 8 * 31])
    wco_d = dt_in("w_conv_o", [D, D])
    wout_d = dt_in("w_out", [D, D])
    wq_d = dt_in("peer_wq", [D, 2048])
    keyT_d = dt_in("keyT", [128, 16 * 128])
    ut_d = dt_in("ut_l", [16384, 1024])
    vl_d = dt_in("v_l", [16384, 1024])
    fg_d = dt_in("fg", [1, D])
    ropec_d = dt_in("rope_c", [T, 128])
    ropes_d = dt_in("rope_s", [T, 128])
    out_d = nc.dram_tensor("out", [T, D], F32, kind="ExternalOutput").ap()
    dbg_d = {}
    for n, s, d in dbg:
        dbg_d[n] = nc.dram_tensor("dbg_" + n, list(s), d, kind="ExternalOutput").ap()

    scr = lambda n, s, d: nc.dram_tensor(n, list(s), d, kind="Internal").ap()
    utbf_d = scr("utbf", [16384, 1024], BF16)
    vbf_d = scr("vbf", [16384, 1024], BF16)
    qt_d = scr("qt_scr", [8, 128, T], BF16)
    gt_d = scr("gt_scr", [8, 128, T + 30], BF16)
    sg_d = scr("sg_scr", [16, 128, T], BF16)
    xs_d = scr("xs_scr", [T, D], F32)
    h2_d = scr("h2_scr", [8, 128, T], BF16)
    rt_d = scr("rt_scr", [3, 128, T], F32)
    g2_d = scr("g2_scr", [1, D], F32)
    waobf_d = scr("wao_bf", [D, D], BF16); b_waobf = Buf()
    wcobf_d = scr("wco_bf", [D, D], BF16); b_wcobf = Buf()
    woutbf_d = scr("wout_bf", [D, D], BF16); b_woutbf = Buf()
    wqbf_d = scr("wq_bf", [D, 2048], BF16); b_wqbf = Buf()
    keyTbf_d = scr("keyT_bf", [128, 2048], BF16); b_keyTbf = Buf()
    b_g2d = Buf()
    b_qt = [Buf() for _ in range(8)]
    b_gt = [Buf() for _ in range(8)]
    b_gtpad = Buf()
    b_sg = [Buf() for _ in range(8)]
    b_xs = [Buf() for _ in range(NT)]
    b_h2 = [Buf() for _ in range(8)]
    b_rt = [Buf() for _ in range(8)]

    S = Sched(nc)

    def mm(out, lhsT, rhs, start, stop, r, w, silent=False):
        S.op("pe", lambda e: e.matmul(out, lhsT=lhsT, rhs=rhs, start=start, stop=stop), r, w, silent=silent)

    def tr(out, in_, ident, r, w):
        S.op("pe", lambda e: e.transpose(out=out, in_=in_, identity=ident), r, w)

    def act(out, in_, func, r, w, bias=None, scale=None, accum=None, eng="act"):
        kw = {}
        if bias is not None:
            kw["bias"] = bias
        if scale is not None:
            kw["scale"] = scale
        if accum is not None:
            kw["accum_out"] = accum
        S.op(eng, lambda e: e.activation(out=out, in_=in_, func=func, **kw), r, w)

    def tt(eng, out, in0, in1, op, r, w):
        S.op(eng, lambda e: e.tensor_tensor(out=out, in0=in0, in1=in1, op=op), r, w)

    def ts(eng, out, in0, s1, s2, op0, op1, r, w):
        if op1 is None:
            S.op(eng, lambda e: e.tensor_scalar(out=out, in0=in0, scalar1=s1, scalar2=None, op0=op0), r, w)
        else:
            S.op(eng, lambda e: e.tensor_scalar(out=out, in0=in0, scalar1=s1, scalar2=s2, op0=op0, op1=op1), r, w)

    def stt(out, in0, scalar, in1, op0, op1, r, w):
        S.op("dve", lambda e: e.scalar_tensor_tensor(out=out, in0=in0, scalar=scalar, in1=in1, op0=op0, op1=op1), r, w)

    def cp(eng, out, in_, r, w):
        if eng == "act":
            S.op("act", lambda e: e.activation(out=out, in_=in_, func=AF.Copy), r, w)
        else:
            S.op(eng, lambda e: e.tensor_copy(out=out, in_=in_), r, w)

    def rcp(out, in_, r, w):
        S.op("dve", lambda e: e.reciprocal(out=out, in_=in_), r, w)

    def dma(q, out, in_, r, w):
        S.dma(q, lambda e: e.dma_start(out=out, in_=in_), r, w)

    def dump(name, ap, r):
        if name in dbg_d:
            dma("sp", dbg_d[name], ap, r, [])

    top = ExitStack()
    with top:
        sbt = lambda es, n, s, d: es.enter_context(nc.sbuf_tensor("sb_" + n, list(s), d))
        PS = top.enter_context(nc.psum_tensor("PS", [128, 4096], F32))
        PSB = PS.bitcast(BF16)
        pb = [Buf("bank%d" % i) for i in range(8)]
        bank = lambda i, a=0, b=512: PS[:, i * 512 + a:i * 512 + b]
        bankb = lambda i, a=0, b=1024: PSB[:, i * 1024 + a:i * 1024 + b]

        ident_f = sbt(top, "ident_f", [128, 128], F32); b_idf = Buf()
        ident_b = sbt(top, "ident_b", [128, 128], BF16); b_idb = Buf()
        ones_f = sbt(top, "ones_f", [128, 128], F32); b_1f = Buf()
        ones_b = sbt(top, "ones_b", [128, 128], BF16); b_1b = Buf()
        modc = sbt(top, "modc", [128, 48], F32); b_modc = Buf()
        ncol = sbt(top, "ncol", [128, 40], F32); b_ncol = Buf()
        mid = ExitStack()
        g1b = sbt(mid, "g1b", [128, D], F32); b_g1b = Buf()
        KT = sbt(mid, "KT", [128, 2 * NKEY], BF16); b_KT = [Buf() for _ in range(NKT)]
        Vs = sbt(mid, "Vs", [128, NKT * 256], BF16); b_V = [Buf() for _ in range(NKT)]

        S.op("pool", lambda e: e.memset(ident_f[:], 0.0), [], [b_idf])
        S.op("pool", lambda e: e.affine_select(out=ident_f[:], in_=ident_f[:], pattern=[[-1, 128]],
                                               compare_op=ALU.not_equal, fill=1.0, base=0, channel_multiplier=1),
             [b_idf], [b_idf])
        cp("dve", ident_b[:], ident_f[:], [b_idf], [b_idb])
        S.op("pool", lambda e: e.memset(ones_f[:], 1.0), [], [b_1f])
        cp("dve", ones_b[:], ones_f[:], [b_1f], [b_1b])
        dma("sp", ncol[:], ncol_d, [], [b_ncol])

        b_ut = [Buf() for _ in range(8)]
        b_vt = [Buf() for _ in range(8)]
        def cast_tables(after=()):
            if upto >= 4:
                for a in range(8):
                    dma("pool", utbf_d[a * 2048:(a + 1) * 2048, :], ut_d[a * 2048:(a + 1) * 2048, :], list(after), [b_ut[a]])
                    dma("pool", vbf_d[a * 2048:(a + 1) * 2048, :], vl_d[a * 2048:(a + 1) * 2048, :], list(after), [b_vt[a]])

        w01 = ExitStack()
        win = sbt(w01, "win", [128, 8 * INW], BF16); b_win = [Buf() for _ in range(11)]
        winv = win_d.rearrange("(k p) n -> p k n", p=128)
        win3 = win[:].rearrange("p (k n) -> p k n", k=8)

        with ExitStack() as es:
            ccol = sbt(es, "ccol", [128, 16], F32); b_ccol = Buf()
            cs = sbt(es, "cs", [128, 16], F32); b_cs = Buf()
            wm = [sbt(es, "wm%d" % i, [128, 8 * 512], F32) for i in range(2)]; b_wm = [Buf(), Buf()]
            bmj = [sbt(es, "bm%d" % i, [1, 512], F32) for i in range(2)]; b_bmj = [Buf(), Buf()]
            mrow = sbt(es, "mrow", [1, 6 * D], F32); b_mrow = Buf()
            mrowc = sbt(es, "mrowc", [1, 2 * D], F32); b_mrowc = Buf()
            tmpc = sbt(es, "tmpc", [128, 48], F32); b_tmpc = Buf()
            dma("sp", ccol[:], ccol_d, [], [b_ccol])
            act(cs[:], ccol[:], AF.Silu, [b_ccol], [b_cs])
            wmv = wmod_d.rearrange("(k p) n -> p k n", p=128)
            for j in range(12):
                w_ = wm[j % 2]
                dma("sp", w_[:].rearrange("p (k n) -> p k n", k=8), wmv[:, :, j * 512:(j + 1) * 512], [], [b_wm[j % 2]])
                bm = bmj[j % 2]; b_bm = b_bmj[j % 2]
                dma("sp", bm[:], bmod_d[0:1, j * 512:(j + 1) * 512], [], [b_bm])
                if upto >= 1 and j >= 1:
                    wi_ = wm[(j - 1) % 2]
                    dma("sp", wi_[:].rearrange("p (k n) -> p k n", k=8), winv[:, :, (j - 1) * 512:j * 512], [], [b_wm[(j - 1) % 2]])
                    cp("act", win3[:, :, (j - 1) * 512:j * 512], wi_[:].rearrange("p (k n) -> p k n", k=8),
                       [b_wm[(j - 1) % 2]], [b_win[j - 1]])
                for k in range(8):
                    mm(bank(0)[0:1, :], cs[:, k:k + 1], w_[:, k * 512:(k + 1) * 512], k == 0, k == 7,
                       [b_cs, b_wm[j % 2]], [pb[0]])
                tt("dve", mrow[0:1, j * 512:(j + 1) * 512], bank(0)[0:1, :], bm[0:1, :], ALU.add,
                   [pb[0], b_bm], [b_mrow])
                if j < 4:
                    for k in range(8):
                        mm(bank(1)[0:1, :], cs[:, 8 + k:9 + k], w_[:, k * 512:(k + 1) * 512], k == 0, k == 7,
                           [b_cs, b_wm[j % 2]], [pb[1]])
                    tt("dve", mrowc[0:1, j * 512:(j + 1) * 512], bank(1)[0:1, :], bm[0:1, :], ALU.add,
                       [pb[1], b_bm], [b_mrowc])
            segs = [(mrow, 0, b_mrow), (mrow, 1, b_mrow), (mrowc, 0, b_mrowc), (mrowc, 1, b_mrowc),
                    (mrow, 3, b_mrow), (mrow, 4, b_mrow)]
            for si, (row, seg, bb) in enumerate(segs):
                for k in range(8):
                    mm(bank(2)[:, si * 8 + k:si * 8 + k + 1], row[0:1, seg * D + k * 128:seg * D + (k + 1) * 128],
                       ones_f[0:1, 0:1], True, True, [bb, b_1f], [pb[2]])
            cp("dve", tmpc[:], bank(2)[:, 0:48], [pb[2]], [b_tmpc])
            for (dst, sc_i, sh_i, ng_off) in ((0, 1, 0, 0), (2, 3, 2, 0), (4, 5, 4, 8)):
                stt(modc[:, dst * 8:dst * 8 + 8], tmpc[:, sc_i * 8:sc_i * 8 + 8], 1.0, ncol[:, ng_off:ng_off + 8],
                    ALU.add, ALU.mult, [b_tmpc, b_ncol], [b_modc])
                cp("dve", modc[:, (dst + 1) * 8:(dst + 1) * 8 + 8], tmpc[:, sh_i * 8:sh_i * 8 + 8], [b_tmpc], [b_modc])
            for (dstt, bdst, seg) in ((g1b, b_g1b, 2),):
                for nb in range(2):
                    mm(bank(3 + nb), ones_f[0:1, :], mrow[0:1, seg * D + nb * 512:seg * D + (nb + 1) * 512], True, True,
                       [b_mrow, b_1f], [pb[3 + nb]])
                    cp("dve", dstt[:, nb * 512:(nb + 1) * 512], bank(3 + nb), [pb[3 + nb]], [bdst])
            dma("sp", g2_d, mrow[0:1, 5 * D:6 * D], [b_mrow], [b_g2d])
            dump("modc", modc[:], [b_modc])
            dump("g1b", g1b[:], [b_g1b])
            S.barrier(exclude=S.dsem["pool"])

        if upto >= 1:
            with ExitStack() as es:
                bw = lambda c0, c1: [b_win[i] for i in range(c0 // 512, (c1 - 1) // 512 + 1)]
                dma("pool", waobf_d, wao_d, [], [b_waobf])
                dma("pool", wcobf_d, wco_d, [], [b_wcobf])
                dma("pool", woutbf_d, wout_d, [], [b_woutbf])
                xt = [sbt(es, "xt%d" % i, [128, D], F32) for i in range(2)]; b_xt = [Buf(), Buf()]
                junk = sbt(es, "junk", [128, D], BF16); b_junk = Buf()
                st4 = sbt(es, "st4", [128, 4], F32); b_st4 = Buf()
                xn = sbt(es, "xn", [128, D], F32); b_xn = Buf()
                hT = sbt(es, "hT", [128, 8 * 512], BF16); b_hT = [Buf() for _ in range(4)]
                hT3 = hT[:].rearrange("p (k n) -> p k n", k=8)
                sq = sbt(es, "sq", [128, 1280], F32); b_sq = Buf()
                qn = sbt(es, "qn", [128, 1280], F32); b_qn = Buf()
                t2 = sbt(es, "t2", [128, 1280], F32); b_t2 = Buf()
                qr = sbt(es, "qr", [128, 1280], BF16); b_qr = Buf()
                s10 = sbt(es, "s10", [128, 32], F32); b_s10 = Buf()
                Gt = sbt(es, "Gt", [128, 1280], F32); b_Gt = Buf()
                gq = sbt(es, "gq", [128, 256], F32); b_gq = Buf()
                rc = [sbt(es, "rc%d" % i, [128, 128], F32) for i in range(2)]; b_rc = [Buf(), Buf()]
                rs = [sbt(es, "rs%d" % i, [128, 128], F32) for i in range(2)]; b_rs = [Buf(), Buf()]
                QTb = sbt(es, "QTb", [128, 8 * 512], BF16); b_QTb = Buf()
                QTb3 = QTb[:].rearrange("p (h n) -> p h n", h=8)
                sgt = sbt(es, "sgt", [128, 512], F32); b_sgt = Buf()
                ob = [sbt(es, "ob%d" % i, [128, 512], BF16) for i in range(2)]; b_ob = [Buf(), Buf()]
                zpad = sbt(es, "zpad", [128, 8 * 15], BF16); b_zpad = Buf()

                dma("sp", gq[:], bass.AP(tensor=qkg_d.tensor, offset=0, ap=[[0, 128], [1, 256]]), [], [b_gq])
                ts("dve", Gt[:, 0:1024].rearrange("p (h d) -> p h d", h=8), XAP(gq[:, 0:128], [[0, 8], [1, 128]]),
                   float(128 ** -0.5), None, ALU.mult, None, [b_gq], [b_Gt])
                cp("dve", Gt[:, 1024:1280].rearrange("p (h d) -> p h d", h=2), XAP(gq[:, 128:256], [[0, 2], [1, 128]]), [b_gq], [b_Gt])
                S.op("pool", lambda e: e.memset(zpad[:], 0.0), [], [b_zpad])
                gtv = gt_d.rearrange("k p n -> p k n")
                dma("sp", gtv[:, :, 0:15], zpad[:].rearrange("p (k n) -> p k n", k=8), [b_zpad], [b_gtpad])
                dma("sp", gtv[:, :, T + 15:T + 30], zpad[:].rearrange("p (k n) -> p k n", k=8), [b_zpad], [b_gtpad])

                t1r = sbt(es, "t1r", [128, 1280], F32); b_t1r = Buf()

                def p1_tile(src_ap, ti_in_blk, key_tile, is_ctx, pos0, cnt):
                    i2 = cnt % 2
                    a_off, b_off = (16, 24) if is_ctx else (0, 8)
                    if is_ctx:
                        c0, nh, banks = 1024, 2, [pb[4]]
                        src_qk = bank(4, 0, 256)
                    else:
                        c0, nh, banks = 0, 10, [pb[2], pb[3], pb[4]]
                        src_qk = PS[:, 2 * 512:2 * 512 + 1280]
                    w_ = nh * 128

                    def front():
                        dma("sp", xt[i2][:], src_ap, [], [b_xt[i2]])
                        if not is_ctx:
                            dma("sp", rc[i2][:], ropec_d[pos0:pos0 + 128, :], [], [b_rc[i2]])
                            dma("sp", rs[i2][:], ropes_d[pos0:pos0 + 128, :], [], [b_rs[i2]])
                        act(junk[:], xt[i2][:], AF.Square, [b_xt[i2]], [b_junk, b_st4], accum=st4[:, 0:1])
                        ts("dve", st4[:, 1:2], st4[:, 0:1], 1.0 / D, EPS, ALU.mult, ALU.add, [b_st4], [b_st4])
                        act(st4[:, 2:3], st4[:, 1:2], AF.Sqrt, [b_st4], [b_st4])
                        rcp(st4[:, 3:4], st4[:, 2:3], [b_st4], [b_st4])
                        ts("dve", xn[:], xt[i2][:], st4[:, 3:4], None, ALU.mult, None, [b_xt[i2], b_st4], [b_xn])
                        for k in range(8):
                            tr(bank(k // 4, (k % 4) * 128, (k % 4) * 128 + 128), xn[:, k * 128:(k + 1) * 128], ident_f[:],
                               [b_xn, b_idf], [pb[k // 4]])
                        for k in range(8):
                            dst = hT3[:, k, ti_in_blk * 128:(ti_in_blk + 1) * 128]
                            src = bank(k // 4, (k % 4) * 128, (k % 4) * 128 + 128)
                            if k % 2 == 0:
                                act(dst, src, AF.Identity, [pb[k // 4], b_modc], [b_hT[ti_in_blk]],
                                    bias=modc[:, b_off + k:b_off + k + 1], scale=modc[:, a_off + k:a_off + k + 1])
                            else:
                                ts("dve", dst, src, modc[:, a_off + k:a_off + k + 1], modc[:, b_off + k:b_off + k + 1],
                                   ALU.mult, ALU.add, [pb[k // 4], b_modc], [b_hT[ti_in_blk]])
                        for cb in ((2,) if is_ctx else (0, 1, 2)):
                            for k in range(8):
                                mm(bank(2 + cb), hT3[:, k, ti_in_blk * 128:(ti_in_blk + 1) * 128],
                                   win3[:, k, cb * 512:(cb + 1) * 512], k == 0, k == 7,
                                   [b_hT[ti_in_blk]] + bw(cb * 512, cb * 512 + 512), [pb[2 + cb]], silent=(k < 7))

                    def backA():
                        cp("act", Vs[:, key_tile * 256:(key_tile + 1) * 256], bank(4, 256, 512), [pb[4]], [b_V[key_tile]])
                        act(sq[:, c0:c0 + w_], src_qk, AF.Square, banks, [b_sq])
                        S.op("dve", lambda e: e.tensor_reduce(out=s10[:, 0:nh], in_=sq[:, c0:c0 + w_].rearrange("p (h d) -> p h d", h=nh),
                                                              axis=AX.X, op=ALU.add), [b_sq], [b_s10])
                        ts("dve", s10[:, 10:10 + nh], s10[:, 0:nh], 1.0 / 128, EPS, ALU.mult, ALU.add, [b_s10], [b_s10])
                        act(s10[:, 20:20 + nh], s10[:, 10:10 + nh], AF.Sqrt, [b_s10], [b_s10])
                        rcp(s10[:, 0:nh], s10[:, 20:20 + nh], [b_s10], [b_s10])
                        tt("dve", qn[:, c0:c0 + w_].rearrange("p (h d) -> p h d", h=nh), src_qk.rearrange("p (h d) -> p h d", h=nh),
                           XAP(s10[:, 0:nh], [[1, nh], [0, 128]]), ALU.mult, banks + [b_s10], [b_qn])

                    def backB():
                        tt("pool", qn[:, c0:c0 + w_], qn[:, c0:c0 + w_], Gt[:, c0:c0 + w_], ALU.mult, [b_qn, b_Gt], [b_qn])
                        if is_ctx:
                            cp("dve", qr[:, 1024:1280], qn[:, 1024:1280], [b_qn], [b_qr])
                        else:
                            tt("pool", t1r[:].rearrange("p (h d) -> p h d", h=10), qn[:].rearrange("p (h d) -> p h d", h=10),
                               XAP(rc[i2][:], [[0, 10], [1, 128]]), ALU.mult, [b_qn, b_rc[i2]], [b_t1r])
                            for hf in range(2):
                                o_off, i_off = hf * 32, (1 - hf) * 32
                                tt("dve", XAP(t2[:], [[128, 10], [64, 2], [1, 32]], o_off),
                                   XAP(qn[:], [[128, 10], [64, 2], [1, 32]], i_off),
                                   XAP(rs[i2][:], [[0, 10], [64, 2], [1, 32]], o_off), ALU.mult, [b_qn, b_rs[i2]], [b_t2])
                            tt("dve", qr[:], t1r[:], t2[:], ALU.add, [b_t1r, b_t2], [b_qr])
                        if not is_ctx:
                            for h in range(8):
                                tr(bankb(5, h * 128, (h + 1) * 128), qr[:, h * 128:(h + 1) * 128], ident_b[:], [b_qr, b_idb], [pb[5]])
                            cp("act", QTb3[:, :, ti_in_blk * 128:(ti_in_blk + 1) * 128],
                               bankb(5).rearrange("p (h n) -> p h n", h=8), [pb[5]], [b_QTb])
                        for h in range(2):
                            tr(bankb(6, h * 128, (h + 1) * 128), qr[:, 1024 + h * 128:1024 + (h + 1) * 128], ident_b[:],
                               [b_qr, b_idb], [pb[6]])
                        cp("dve", XAP(KT[:, key_tile * 128:(key_tile + 1) * 128], [[NKEY, 2], [1, 128]]),
                           bankb(6, 0, 256).rearrange("p (h n) -> p h n", h=2), [pb[6]], [b_KT[key_tile]])
                    return front, backA, backB

                def run_tiles(tiles, mid_fn=None):
                    n_ = len(tiles)
                    tiles[0][0]()
                    for i_ in range(n_):
                        tiles[i_][1]()
                        if i_ + 1 < n_:
                            tiles[i_ + 1][0]()
                        elif mid_fn is not None:
                            mid_fn()
                        tiles[i_][2]()

                cnt = 0
                ctl = []
                for ci in range(2):
                    ctl.append(p1_tile(ctx_d[ci * 128:(ci + 1) * 128, :], ci, NT + ci, True, 0, cnt))
                    cnt += 1
                run_tiles(ctl)
                for blk in range(8):
                    tl_ = []
                    for ti in range(4):
                        tg = blk * 4 + ti
                        tl_.append(p1_tile(x_d[tg * 128:(tg + 1) * 128, :], ti, tg, False, tg * 128, cnt))
                        cnt += 1
                    obi = [0]

                    def glu_part():
                        for j in range(8):
                            ca = 1536 + j * 128
                            cb_ = 1536 + 1024 + j * 128
                            ba, bb_ = (0, 1) if j % 2 == 0 else (2, 3)
                            for k in range(8):
                                mm(bank(ba), win3[:, k, ca:ca + 128], hT3[:, k, :], k == 0, k == 7, b_hT + bw(ca, ca + 128), [pb[ba]], silent=(k < 7))
                            for k in range(8):
                                mm(bank(bb_), win3[:, k, cb_:cb_ + 128], hT3[:, k, :], k == 0, k == 7, b_hT + bw(cb_, cb_ + 128), [pb[bb_]], silent=(k < 7))
                            act(sgt[:], bank(bb_), AF.Sigmoid, [pb[bb_]], [b_sgt])
                            o = ob[obi[0] % 2]; bo = b_ob[obi[0] % 2]; obi[0] += 1
                            tt("dve", o[:], bank(ba), sgt[:], ALU.mult, [pb[ba], b_sgt], [bo])
                            dma("sp", gt_d[j, :, 15 + blk * 512:15 + (blk + 1) * 512], o[:], [bo], [b_gt[blk]])
                    run_tiles(tl_, glu_part)
                    dma("sp", qt_d.rearrange("h p n -> p h n")[:, :, blk * 512:(blk + 1) * 512], QTb3, [b_QTb], [b_qt[blk]])
                    for j in range(16):
                        cg = 3584 + j * 128
                        bg = 4 + (j % 2)
                        for k in range(8):
                            mm(bank(bg), win3[:, k, cg:cg + 128], hT3[:, k, :], k == 0, k == 7, b_hT + bw(cg, cg + 128), [pb[bg]], silent=(k < 7))
                        o = ob[obi[0] % 2]; bo = b_ob[obi[0] % 2]; obi[0] += 1
                        act(o[:], bank(bg), AF.Sigmoid, [pb[bg]], [bo])
                        dma("sp", sg_d[j, :, blk * 512:(blk + 1) * 512], o[:], [bo], [b_sg[blk]])
                    if upto == 1 and blk == 0:
                        break
                dump("KT", KT[:], b_KT)
                dump("Vs", Vs[:], b_V)
                dump("QTb", QTb[:], [b_QTb])
                S.barrier(exclude=S.dsem["pool"])
                dump("gt", gt_d.rearrange("k p n -> p k n")[:, :, 0:542], [])
                dump("sg", sg_d.rearrange("k p n -> p k n")[:, :, 0:512], [])

        if upto >= 1:
            S.barrier(exclude=S.dsem["pool"])
        w01.close()

        if upto >= 2:
            with ExitStack() as es:
                wao = sbt(es, "wao", [128, 8 * D], BF16); b_wao = Buf()
                wco = sbt(es, "wco", [128, 8 * D], BF16); b_wco = Buf()
                wout = sbt(es, "wout", [128, 8 * D], BF16); b_wout = Buf()
                wao3 = wao[:].rearrange("p (k n) -> p k n", k=8)
                wco3 = wco[:].rearrange("p (k n) -> p k n", k=8)
                wout3 = wout[:].rearrange("p (k n) -> p k n", k=8)
                dma("sp", wao3, waobf_d.rearrange("(k p) n -> p k n", p=128), [b_waobf], [b_wao])
                dma("sp", wco3, wcobf_d.rearrange("(k p) n -> p k n", p=128), [b_wcobf], [b_wco])
                dma("sp", wout3, woutbf_d.rearrange("(k p) n -> p k n", p=128), [b_woutbf], [b_wout])
                dma("pool", wqbf_d, wq_d, [], [b_wqbf])
                dma("pool", keyTbf_d, keyT_d, [], [b_keyTbf])
                cdw = sbt(es, "cdw", [128, 248], F32); b_cdw = Buf()
                dma("sp", cdw[:], cdw_d, [], [b_cdw])
                QTb = sbt(es, "QTb2", [128, 8 * 512], BF16); b_QTb = Buf()
                QTb3 = QTb[:].rearrange("p (h n) -> p h n", h=8)
                gth = sbt(es, "gth", [128, 8 * 542], BF16); b_gth = Buf()
                gth3 = gth[:].rearrange("p (k n) -> p k n", k=8)
                sgl = [sbt(es, "sgl%d" % i, [128, 512], BF16) for i in range(4)]; b_sgl = [Buf() for _ in range(4)]
                psb = [sbt(es, "psb%d" % i, [128, 1024], BF16) for i in range(3)]; b_psb = [Buf() for _ in range(3)]
                rz = sbt(es, "rz", [128, 512], F32); b_rz = Buf()
                OTb = sbt(es, "OTb", [128, 8 * 512], BF16); b_OT = [Buf() for _ in range(8)]
                OTb3 = OTb[:].rearrange("p (h n) -> p h n", h=8)
                ycv = sbt(es, "ycv", [128, 8 * 512], F32); b_ycv = [Buf() for _ in range(8)]
                ycv3 = ycv[:].rearrange("p (k n) -> p k n", k=8)
                sqy = sbt(es, "sqy", [128, 512], F32); b_sqy = Buf()
                cacc = [sbt(es, "cacc%d" % i, [128, 512], F32) for i in range(3)]; b_cacc = [Buf() for _ in range(3)]
                mean = sbt(es, "mean", [128, 512], F32); b_mean = Buf()
                msq = sbt(es, "msq", [128, 512], F32); b_msq = Buf()
                var = sbt(es, "var", [128, 512], F32); b_var = Buf()
                rstd = sbt(es, "rstd", [128, 512], F32); b_rstd = Buf()
                yns = [sbt(es, "yn%d" % i, [128, 512], F32) for i in range(2)]; b_yns = [Buf(), Buf()]
                zT = sbt(es, "zT", [128, 8 * 512], BF16); b_zT = [Buf() for _ in range(8)]
                zT3 = zT[:].rearrange("p (k n) -> p k n", k=8)
                m2 = sbt(es, "m2", [128, 512], F32); b_m2 = Buf()
                tm2 = sbt(es, "tm2", [128, 512], F32); b_tm2 = Buf()
                mgT = sbt(es, "mgT", [128, 8 * 512], BF16); b_mg = [Buf() for _ in range(8)]
                mgT3 = mgT[:].rearrange("p (k n) -> p k n", k=8)
                xt = [sbt(es, "xt2_%d" % i, [128, D], F32) for i in range(2)]; b_xt = [Buf(), Buf()]
                t1 = sbt(es, "t1", [128, D], F32); b_t1 = Buf()
                xs = sbt(es, "xs", [128, D], F32); b_xs_ = Buf()
                junk = sbt(es, "junk2", [128, D], BF16); b_junk = Buf()
                xn = sbt(es, "xn2", [128, D], F32); b_xn = Buf()
                st4 = sbt(es, "st4b", [128, 4], F32); b_st4 = Buf()

                STG = 9
                pcnt = 0
                sgcnt = 0
                xcnt = 0
                nblk = 1 if upto == 2 else 8
                for blk in range(nblk):
                    dma("sp", QTb3, qt_d.rearrange("h p n -> p h n")[:, :, blk * 512:(blk + 1) * 512], [], [b_QTb])
                    dma("sp", gth3, gt_d.rearrange("k p n -> p k n")[:, :, blk * 512:blk * 512 + 542], [], [b_gth])
                    NK_ = NKT
                    steps = [(h_, kt_) for h_ in range(8) for kt_ in range(NK_)]
                    SB = [0, 1, 6, 7]

                    def emitS(si):
                        h_, kt_ = steps[si]
                        kvh_ = h_ // 4
                        bk_ = SB[si % 4]
                        mm(bank(bk_), KT[:, kvh_ * NKEY + kt_ * 128:kvh_ * NKEY + (kt_ + 1) * 128], QTb3[:, h_, :], True, True,
                           [b_QTb], [pb[bk_]])

                    def conv_tap(j, tp):
                        ch = tp % 4
                        acc = ycv3[:, j, :] if ch == 0 else cacc[ch - 1][:]
                        bacc = b_ycv[j] if ch == 0 else b_cacc[ch - 1]
                        if tp == 0:
                            ts("dve", acc, gth3[:, j, 0:512], cdw[:, j * 31:j * 31 + 1], ncol[:, 16 + j:17 + j],
                               ALU.mult, ALU.add, [b_gth, b_cdw, b_ncol], [bacc])
                        elif tp < 4:
                            ts("dve", acc, gth3[:, j, tp:tp + 512], cdw[:, j * 31 + tp:j * 31 + tp + 1], None,
                               ALU.mult, None, [b_gth, b_cdw], [bacc])
                        else:
                            stt(acc, gth3[:, j, tp:tp + 512], cdw[:, j * 31 + tp:j * 31 + tp + 1], acc,
                                ALU.mult, ALU.add, [b_gth, b_cdw, bacc], [bacc])
                    emitS(0)
                    emitS(1)
                    for h in range(8):
                        kvh = h // 4
                        bo, bz = (2, 3) if h % 2 == 0 else (4, 5)
                        j = h
                        for kp in range(NK_ // 2):
                            si = h * NK_ + 2 * kp
                            for d_ in (2, 3):
                                if si + d_ < len(steps):
                                    emitS(si + d_)
                            for tp in (2 * kp, 2 * kp + 1):
                                if tp < 31:
                                    conv_tap(j, tp)
                            b0 = SB[si % 4]
                            p_ = psb[pcnt % 3]; bp = b_psb[pcnt % 3]; pcnt += 1
                            act(p_[:], PS[:, b0 * 512:(b0 + 2) * 512], AF.Exp, [pb[b0], pb[b0 + 1]], [bp])
                            for hf in range(2):
                                kt = 2 * kp + hf
                                mm(bank(bo), Vs[:, kt * 256 + kvh * 128:kt * 256 + (kvh + 1) * 128], p_[:, hf * 512:(hf + 1) * 512],
                                   kt == 0, kt == NK_ - 1, [bp], [pb[bo]])
                                mm(bank(bz), ones_b[:], p_[:, hf * 512:(hf + 1) * 512], kt == 0, kt == NK_ - 1, [bp], [pb[bz]])
                        rcp(rz[:], bank(bz), [pb[bz]], [b_rz])
                        tt("dve", OTb3[:, h, :], bank(bo), rz[:], ALU.mult, [pb[bo], b_rz], [b_OT[h]])
                        tt("pool", cacc[1][:], cacc[1][:], cacc[2][:], ALU.add, [b_cacc[1], b_cacc[2]], [b_cacc[1]])
                        tt("pool", ycv3[:, j, :], ycv3[:, j, :], cacc[0][:], ALU.add, [b_ycv[j], b_cacc[0]], [b_ycv[j]])
                        tt("pool", ycv3[:, j, :], ycv3[:, j, :], cacc[1][:], ALU.add, [b_ycv[j], b_cacc[1]], [b_ycv[j]])
                    for j in range(8):
                        sga = sgl[sgcnt % 4]; bsga = b_sgl[sgcnt % 4]; sgcnt += 1
                        dma("sp", sga[:], sg_d[j, :, blk * 512:(blk + 1) * 512], [], [bsga])
                        ba_ = 2 + j % 4
                        for k in range(8):
                            mm(bank(ba_), wao3[:, k, j * 128:(j + 1) * 128], OTb3[:, k, :], k == 0, k == 7, [b_wao, b_OT[k]], [pb[ba_]], silent=(k < 7))
                        tt("dve", mgT3[:, j, :], bank(ba_), sga[:], ALU.mult, [pb[ba_], bsga], [b_mg[j]])
                    for j in range(8):
                        act(sqy[:], ycv3[:, j, :], AF.Square, [b_ycv[j]], [b_sqy])
                        mm(bank(6), ones_f[:], ycv3[:, j, :], j == 0, j == 7, [b_ycv[j], b_1f], [pb[6]])
                        mm(bank(7), ones_f[:], sqy[:], j == 0, j == 7, [b_sqy, b_1f], [pb[7]])
                    if STG < 3:
                        continue
                    ts("dve", mean[:], bank(6), 1.0 / D, None, ALU.mult, None, [pb[6]], [b_mean])
                    tt("dve", msq[:], mean[:], mean[:], ALU.mult, [b_mean], [b_msq])
                    stt(var[:], bank(7), 1.0 / D, msq[:], ALU.mult, ALU.subtract, [pb[7], b_msq], [b_var])
                    ts("dve", var[:], var[:], EPS, None, ALU.add, None, [b_var], [b_var])
                    act(var[:], var[:], AF.Sqrt, [b_var], [b_var])
                    rcp(rstd[:], var[:], [b_var], [b_rstd])
                    for j in range(8):
                        yn = yns[j % 2]; b_yn = b_yns[j % 2]
                        tt("dve", yn[:], ycv3[:, j, :], mean[:], ALU.subtract, [b_ycv[j], b_mean], [b_yn])
                        tt("dve", yn[:], yn[:], rstd[:], ALU.mult, [b_yn, b_rstd], [b_yn])
                        act(zT3[:, j, :], yn[:], AF.Silu, [b_yn, b_ncol], [b_zT[j]],
                            bias=ncol[:, 32 + j:33 + j], scale=ncol[:, 24 + j:25 + j])
                    if STG < 4:
                        continue
                    for j in range(8):
                        sgc = sgl[sgcnt % 4]; bsgc = b_sgl[sgcnt % 4]; sgcnt += 1
                        dma("sp", sgc[:], sg_d[8 + j, :, blk * 512:(blk + 1) * 512], [], [bsgc])
                        bc_ = (6, 7, 0, 1)[j % 4]
                        for k in range(8):
                            mm(bank(bc_), wco3[:, k, j * 128:(j + 1) * 128], zT3[:, k, :], k == 0, k == 7, [b_wco, b_zT[k]], [pb[bc_]], silent=(k < 7))
                        tt("dve", m2[:], bank(bc_), sgc[:], ALU.mult, [pb[bc_], bsgc], [b_m2])
                        tt("dve", mgT3[:, j, :], mgT3[:, j, :], m2[:], ALU.add, [b_mg[j], b_m2], [b_mg[j]])
                    if blk == 0:
                        dump("mgT", mgT[:], b_mg)
                        dump("OTb", OTb[:], b_OT)
                        dump("zT", zT[:], b_zT)
                    if STG < 5:
                        continue
                    xbufs = []
                    for ti in range(4):
                        tg = blk * 4 + ti
                        x_ = xt[xcnt % 2]; bx = b_xt[xcnt % 2]; xcnt += 1
                        xbufs.append((x_, bx))

                    def t_mm(ti):
                        tg = blk * 4 + ti
                        x_, bx = xbufs[ti]
                        dma("sp", x_[:], x_d[tg * 128:(tg + 1) * 128, :], [], [bx])
                        ob_ = 6 if ti % 2 == 0 else 2
                        for nb in range(2):
                            for k in range(8):
                                mm(bank(ob_ + nb), mgT3[:, k, ti * 128:(ti + 1) * 128], wout3[:, k, nb * 512:(nb + 1) * 512],
                                   k == 0, k == 7, [b_mg[k], b_wout], [pb[ob_ + nb]], silent=(k < 7))

                    def t_chain(ti):
                        tg = blk * 4 + ti
                        x_, bx = xbufs[ti]
                        ob_ = 6 if ti % 2 == 0 else 2
                        tt("dve", t1[:], PS[:, ob_ * 512:(ob_ + 2) * 512], g1b[:], ALU.mult, [pb[ob_], pb[ob_ + 1], b_g1b], [b_t1])
                        tt("dve", xs[:], t1[:], x_[:], ALU.add, [b_t1, bx], [b_xs_])
                        dma("sp", xs_d[tg * 128:(tg + 1) * 128, :], xs[:], [b_xs_], [b_xs[tg]])
                        act(junk[:], xs[:], AF.Square, [b_xs_], [b_junk, b_st4], accum=st4[:, 0:1])
                        ts("dve", st4[:, 1:2], st4[:, 0:1], 1.0 / D, EPS, ALU.mult, ALU.add, [b_st4], [b_st4])
                        act(st4[:, 2:3], st4[:, 1:2], AF.Sqrt, [b_st4], [b_st4])
                        rcp(st4[:, 3:4], st4[:, 2:3], [b_st4], [b_st4])
                        ts("dve", xn[:], xs[:], st4[:, 3:4], None, ALU.mult, None, [b_xs_, b_st4], [b_xn])

                    def t_trev(ti):
                        tb_ = 0 if ti % 2 == 0 else 4
                        for k in range(8):
                            tr(bank(tb_ + k // 4, (k % 4) * 128, (k % 4) * 128 + 128), xn[:, k * 128:(k + 1) * 128], ident_f[:],
                               [b_xn, b_idf], [pb[tb_ + k // 4]])
                        for k in range(8):
                            dst = zT3[:, k, ti * 128:(ti + 1) * 128]
                            src = bank(tb_ + k // 4, (k % 4) * 128, (k % 4) * 128 + 128)
                            ts("dve", dst, src, modc[:, 32 + k:33 + k], modc[:, 40 + k:41 + k],
                               ALU.mult, ALU.add, [pb[tb_ + k // 4], b_modc], [b_zT[k]])
                    t_mm(0)
                    t_mm(1)
                    for ti in range(4):
                        t_chain(ti)
                        if ti + 2 < 4:
                            t_mm(ti + 2)
                        t_trev(ti)
                    if STG >= 8:
                        dma("sp", h2_d.rearrange("k p n -> p k n")[:, :, blk * 512:(blk + 1) * 512], zT3, b_zT, [b_h2[blk]])
                S.barrier(exclude=S.dsem["pool"])
                dump("xs0", xs_d[0:512, :], [])
                dump("h2", h2_d.rearrange("k p n -> p k n")[:, :, 0:512], [])

        mid.close()

        if upto >= 3:
            with ExitStack() as es:
                wq = sbt(es, "wq", [128, 8 * 2048], BF16); b_wq = Buf()
                wq3 = wq[:].rearrange("p (k n) -> p k n", k=8)
                dma("sp", wq3, wqbf_d.rearrange("(k p) n -> p k n", p=128), [b_wqbf], [b_wq])
                keyT = sbt(es, "keyT", [128, 2048], BF16); b_keyT = Buf()
                dma("sp", keyT[:], keyTbf_d, [b_keyTbf], [b_keyT])
                h2Ts = [sbt(es, "h2T%d" % i, [128, 8 * 512], BF16) for i in range(2)]; b_h2Ts = [Buf(), Buf()]
                qT = sbt(es, "qT", [128, 16 * 512], BF16); b_qT = [Buf() for _ in range(16)]
                qT3 = qT[:].rearrange("p (c n) -> p c n", c=16)
                thr = sbt(es, "thr", [128, 16], F32); b_thr = Buf()
                io16 = sbt(es, "io16", [128, 16], F32); b_io16 = Buf()
                rtb = sbt(es, "rtb", [128, 3 * 512], F32); b_rtb = Buf()
                rtb3 = rtb[:].rearrange("p (a n) -> p a n", a=3)

                class _NS:
                    pass
                sets = []
                for s_ in range(2):
                    n_ = _NS()
                    n_.sc = sbt(es, "sc%d" % s_, [128, 2048], F32); n_.b_sc = Buf()
                    n_.scr2 = sbt(es, "scr2%d" % s_, [128, 2048], F32)
                    n_.m16 = sbt(es, "m16%d" % s_, [128, 256], F32)
                    n_.b_m16a = [Buf() for _ in range(16)]; n_.b_m16b = [Buf() for _ in range(16)]
                    n_.b_i16a = [Buf() for _ in range(16)]; n_.b_i16b = [Buf() for _ in range(16)]
                    n_.b_scr2g = [Buf() for _ in range(16)]
                    n_.b_b16a = [Buf() for _ in range(8)]; n_.b_b16b = [Buf() for _ in range(8)]
                    n_.b_p16a = [Buf() for _ in range(8)]; n_.b_p16b = [Buf() for _ in range(8)]
                    n_.b_cs2h = [Buf() for _ in range(8)]
                    n_.i16 = sbt(es, "i16%d" % s_, [128, 256], U32)
                    n_.i16f = sbt(es, "i16f%d" % s_, [128, 256], F32); n_.b_i16f = Buf()
                    n_.cs_ = sbt(es, "cs_%d" % s_, [128, 2048], F32); n_.b_cs_ = Buf()
                    n_.cs2 = sbt(es, "cs2%d" % s_, [128, 2048], F32)
                    n_.b16 = sbt(es, "b16%d" % s_, [128, 128], F32)
                    n_.p16 = sbt(es, "p16%d" % s_, [128, 128], U32)
                    n_.p16f = sbt(es, "p16f%d" % s_, [128, 128], F32); n_.b_p16f = Buf()
                    n_.af = sbt(es, "af%d" % s_, [128, 128], F32); n_.b_af = Buf()
                    n_.bf_ = sbt(es, "bf_%d" % s_, [128, 128], F32); n_.b_bf = Buf()
                    n_.E = sbt(es, "E%d" % s_, [128, 2048], F32); n_.b_E = Buf()
                    n_.sel = sbt(es, "sel%d" % s_, [128, 3 * 128], F32); n_.b_sel = Buf()
                    n_.z8 = sbt(es, "z8%d" % s_, [128, 16], F32); n_.b_z8 = Buf()
                    n_.pb0 = 4 * s_
                    sets.append(n_)
                S.op("pool", lambda e: e.iota(out=io16[:], pattern=[[1, 16]], base=0, channel_multiplier=0,
                                              allow_small_or_imprecise_dtypes=True), [], [b_io16])
                ts("dve", thr[:], io16[:], 16.0, None, ALU.mult, None, [b_io16], [b_thr])

                def tile_prog(blk, ti, n_):
                    Q = []

                    def q(fn, *a_, **k_):
                        Q.append((fn, a_, k_))
                    sc, scr2, m16, i16, i16f, cs_, cs2 = n_.sc, n_.scr2, n_.m16, n_.i16, n_.i16f, n_.cs_, n_.cs2
                    b16, p16, p16f, af, bf_, E, sel, z8 = n_.b16, n_.p16, n_.p16f, n_.af, n_.bf_, n_.E, n_.sel, n_.z8
                    pb0 = n_.pb0
                    for c in range(16):
                        bk_ = pb0 + c // 4
                        q(mm, bank(bk_, (c % 4) * 128, (c % 4) * 128 + 128), qT3[:, c, ti * 128:(ti + 1) * 128],
                          keyT[:, c * 128:(c + 1) * 128], True, True, [b_qT[c], b_keyT], [pb[bk_]])
                    q(cp, "act", sc[:], PS[:, pb0 * 512:pb0 * 512 + 2048], [pb[pb0 + i_] for i_ in range(4)], [n_.b_sc])
                    for stp in range(5):
                        for g in range(16):
                            sg_ = sc[:, g * 128:(g + 1) * 128]
                            sr_ = scr2[:, g * 128:(g + 1) * 128]
                            if stp == 0:
                                q(S.op, "dve", lambda e, g=g, sg_=sg_: e.max(out=m16[:, g * 16:g * 16 + 8], in_=sg_), [n_.b_sc], [n_.b_m16a[g]])
                            elif stp == 1:
                                q(S.op, "dve", lambda e, g=g, sg_=sg_: e.max_index(out=i16[:, g * 16:g * 16 + 8], in_max=m16[:, g * 16:g * 16 + 8],
                                                                                   in_values=sg_), [n_.b_sc, n_.b_m16a[g]], [n_.b_i16a[g]])
                            elif stp == 2:
                                q(S.op, "dve", lambda e, g=g, sg_=sg_, sr_=sr_: e.match_replace(out=sr_, in_to_replace=m16[:, g * 16:g * 16 + 8],
                                                                                                in_values=sg_, imm_value=-1e30),
                                  [n_.b_sc, n_.b_m16a[g]], [n_.b_scr2g[g]])
                            elif stp == 3:
                                q(S.op, "dve", lambda e, g=g, sr_=sr_: e.max(out=m16[:, g * 16 + 8:g * 16 + 16], in_=sr_), [n_.b_scr2g[g]], [n_.b_m16b[g]])
                            else:
                                q(S.op, "dve", lambda e, g=g, sr_=sr_: e.max_index(out=i16[:, g * 16 + 8:g * 16 + 16],
                                                                                   in_max=m16[:, g * 16 + 8:g * 16 + 16], in_values=sr_),
                                  [n_.b_scr2g[g], n_.b_m16b[g]], [n_.b_i16b[g]])
                    b_m16 = n_.b_m16a + n_.b_m16b
                    b_i16 = n_.b_i16a + n_.b_i16b
                    q(cp, "dve", i16f[:], i16[:], b_i16, [n_.b_i16f])
                    q(tt, "dve", XAP(cs_[:], [[256, 8], [16, 16], [1, 16]]), XAP(m16[:], [[32, 8], [1, 16], [0, 16]]),
                      XAP(m16[:], [[32, 8], [0, 16], [1, 16]], 16), ALU.add, b_m16, [n_.b_cs_])
                    for stp in range(5):
                        for h in range(8):
                            ch = cs_[:, h * 256:(h + 1) * 256]
                            ch2 = cs2[:, h * 256:(h + 1) * 256]
                            if stp == 0:
                                q(S.op, "dve", lambda e, h=h, ch=ch: e.max(out=b16[:, h * 16:h * 16 + 8], in_=ch), [n_.b_cs_], [n_.b_b16a[h]])
                            elif stp == 1:
                                q(S.op, "dve", lambda e, h=h, ch=ch: e.max_index(out=p16[:, h * 16:h * 16 + 8], in_max=b16[:, h * 16:h * 16 + 8],
                                                                                 in_values=ch), [n_.b_cs_, n_.b_b16a[h]], [n_.b_p16a[h]])
                            elif stp == 2:
                                q(S.op, "dve", lambda e, h=h, ch=ch, ch2=ch2: e.match_replace(out=ch2, in_to_replace=b16[:, h * 16:h * 16 + 8],
                                                                                              in_values=ch, imm_value=-1e30),
                                  [n_.b_cs_, n_.b_b16a[h]], [n_.b_cs2h[h]])
                            elif stp == 3:
                                q(S.op, "dve", lambda e, h=h, ch2=ch2: e.max(out=b16[:, h * 16 + 8:h * 16 + 16], in_=ch2), [n_.b_cs2h[h]], [n_.b_b16b[h]])
                            else:
                                q(S.op, "dve", lambda e, h=h, ch2=ch2: e.max_index(out=p16[:, h * 16 + 8:h * 16 + 16],
                                                                                   in_max=b16[:, h * 16 + 8:h * 16 + 16], in_values=ch2),
                                  [n_.b_cs2h[h], n_.b_b16b[h]], [n_.b_p16b[h]])
                    b_b16l = n_.b_b16a + n_.b_b16b
                    b_p16l = n_.b_p16a + n_.b_p16b
                    q(cp, "dve", p16f[:], p16[:], b_p16l, [n_.b_p16f])
                    q(tt, "dve", XAP(E[:], [[15, 128], [1, 15]]), XAP(p16f[:], [[1, 128], [0, 15]]),
                      XAP(thr[:], [[0, 128], [1, 15]], 1), ALU.is_ge, [n_.b_p16f, b_thr], [n_.b_E])
                    q(S.op, "dve", lambda e: e.tensor_reduce(out=af[:], in_=XAP(E[:], [[15, 128], [1, 15]]), axis=AX.X, op=ALU.add),
                      [n_.b_E], [n_.b_af])
                    q(stt, bf_[:], af[:], -16.0, p16f[:], ALU.mult, ALU.add, [n_.b_af, n_.b_p16f], [n_.b_bf])
                    for (src, bsrc, off, dsti) in ((af, n_.b_af, 0, 0), (bf_, n_.b_bf, 16, 1)):
                        q(tt, "dve", XAP(E[:], [[16, 128], [1, 16]]), XAP(src[:], [[1, 128], [0, 16]]),
                          XAP(io16[:], [[0, 128], [1, 16]]), ALU.is_equal, [bsrc, b_io16], [n_.b_E])
                        q(tt, "dve", XAP(E[:], [[256, 8], [16, 16], [1, 16]]), XAP(E[:], [[256, 8], [16, 16], [1, 16]]),
                          XAP(i16f[:], [[32, 8], [0, 16], [1, 16]], off), ALU.mult, [n_.b_E, n_.b_i16f], [n_.b_E])
                        q(S.op, "dve", lambda e, dsti=dsti: e.tensor_reduce(out=sel[:, dsti * 128:(dsti + 1) * 128],
                                                                             in_=XAP(E[:], [[16, 128], [1, 16]]), axis=AX.X, op=ALU.add),
                          [n_.b_E], [n_.b_sel])
                    q(tt, "dve", XAP(E[:], [[16, 8], [1, 16]]), XAP(b16[:], [[16, 8], [1, 16]]), XAP(b16[:], [[16, 8], [0, 16]]),
                      ALU.subtract, b_b16l, [n_.b_E])
                    q(act, E[:, 0:128], E[:, 0:128], AF.Exp, [n_.b_E], [n_.b_E])
                    q(S.op, "dve", lambda e: e.tensor_reduce(out=z8[:, 0:8], in_=XAP(E[:], [[16, 8], [1, 16]]), axis=AX.X, op=ALU.add),
                      [n_.b_E], [n_.b_z8])
                    q(rcp, z8[:, 8:16], z8[:, 0:8], [n_.b_z8], [n_.b_z8])
                    q(tt, "dve", XAP(sel[:], [[16, 8], [1, 16]], 256), XAP(E[:], [[16, 8], [1, 16]]), XAP(z8[:], [[1, 8], [0, 16]], 8),
                      ALU.mult, [n_.b_E, n_.b_z8], [n_.b_sel])
                    if blk == 0 and ti == 0:
                        q(dump, "sel", sel[:], [n_.b_sel])
                    for a3 in range(3):
                        q(tr, bank(pb0, a3 * 128, (a3 + 1) * 128), sel[:, a3 * 128:(a3 + 1) * 128], ident_f[:], [n_.b_sel, b_idf], [pb[pb0]])
                    q(cp, "act", rtb3[:, :, ti * 128:(ti + 1) * 128], bank(pb0, 0, 384).rearrange("p (a n) -> p a n", a=3), [pb[pb0]], [b_rtb])
                    return Q

                nblk = 1 if upto == 3 else 8
                def load_h2(blk_):
                    dma("sp", h2Ts[blk_ % 2][:].rearrange("p (k n) -> p k n", k=8),
                        h2_d.rearrange("k p n -> p k n")[:, :, blk_ * 512:(blk_ + 1) * 512], [], [b_h2Ts[blk_ % 2]])
                load_h2(0)
                for blk in range(nblk):
                    if blk + 1 < nblk:
                        load_h2(blk + 1)
                    h2T3 = h2Ts[blk % 2][:].rearrange("p (k n) -> p k n", k=8)
                    b_h2T = b_h2Ts[blk % 2]
                    for c in range(16):
                        bq = 4 + c % 2
                        for k in range(8):
                            mm(bank(bq), wq3[:, k, c * 128:(c + 1) * 128], h2T3[:, k, :], k == 0, k == 7, [b_wq, b_h2T], [pb[bq]], silent=(k < 7))
                        cp("act", qT3[:, c, :], bank(bq), [pb[bq]], [b_qT[c]])
                    if blk == 0:
                        cast_tables(after=[b_qT[15]])
                    for tp_ in range(2):
                        QA = tile_prog(blk, 2 * tp_, sets[0])
                        QB = tile_prog(blk, 2 * tp_ + 1, sets[1])
                        for i_ in range(max(len(QA), len(QB))):
                            for Q_ in (QA, QB):
                                if i_ < len(Q_):
                                    fn_, a_, k_ = Q_[i_]
                                    fn_(*a_, **k_)
                    dma("sp", rt_d.rearrange("a p n -> p a n")[:, :, blk * 512:(blk + 1) * 512], rtb3, [b_rtb], [b_rt[blk]])
                S.barrier(exclude=S.dsem["pool"])

        if upto >= 4:
            with ExitStack() as es:
                Wds = [sbt(es, "Wd%d" % i, [128, GP * 128], BF16) for i in range(2)]
                b_Wds = [[Buf() for _ in range(GP // 4)] for _ in range(2)]
                NSB = 3
                UTs = [sbt(es, "UTs%d" % i, [128, 2 * 1024], BF16) for i in range(NSB)]; b_UTs = [Buf() for _ in range(NSB)]
                Vcs = [sbt(es, "Vcs%d" % i, [128, 2 * 1024], BF16) for i in range(NSB)]; b_Vcs = [Buf() for _ in range(NSB)]
                h2gs = [sbt(es, "h2g%d" % i, [128, 8 * GP], BF16) for i in range(2)]; b_h2gs = [Buf(), Buf()]
                rtgs = [sbt(es, "rtg%d" % i, [128, 3 * GP], F32) for i in range(2)]; b_rtgs = [Buf(), Buf()]
                iof = sbt(es, "iof", [128, 128], F32); b_iof = Buf()
                iob = sbt(es, "iob", [128, 128], BF16); b_iob = Buf()
                gl = [sbt(es, "gl%d" % i, [128, GP], BF16) for i in range(2)]; b_gl = [Buf(), Buf()]
                At = [sbt(es, "At%d" % i, [128, GP], BF16) for i in range(2)]; b_At = [Buf(), Buf()]
                g2b = sbt(es, "g2b", [128, D], F32); b_g2b = Buf()
                fgb = sbt(es, "fgb", [128, D], F32); b_fgb = Buf()
                xsl = sbt(es, "xsl", [128, D], F32); b_xsl = Buf()
                yt = sbt(es, "yt", [128, D], F32); b_yt = Buf()
                yo = sbt(es, "yo", [128, D], F32); b_yo = Buf()
                junk = sbt(es, "junk3", [128, D], BF16); b_junk = Buf()
                st4 = sbt(es, "st4c", [128, 4], F32); b_st4 = Buf()
                dma("sp", g2b[:], bass.AP(tensor=g2_d.tensor, offset=0, ap=[[0, 128], [1, D]]), [b_g2d], [b_g2b])
                dma("sp", fgb[:], bass.AP(tensor=fg_d.tensor, offset=0, ap=[[0, 128], [1, D]]), [], [b_fgb])
                S.op("pool", lambda e: e.iota(out=iof[:], pattern=[[1, 128]], base=0, channel_multiplier=0,
                                              allow_small_or_imprecise_dtypes=True), [], [b_iof])
                cp("dve", iob[:], iof[:], [b_iof], [b_iob])
                utv = utbf_d.rearrange("(c p) n -> p c n", p=128)
                vtv = vbf_d.rearrange("(c p) n -> p c n", p=128)
                ngrp = 1 if upto == 4 else T // GP
                ocnt = [0]

                def load_grp(grp):
                    t0 = grp * GP
                    gi = grp % 2
                    dma("sp", h2gs[gi][:].rearrange("p (k n) -> p k n", k=8), h2_d.rearrange("k p n -> p k n")[:, :, t0:t0 + GP], [], [b_h2gs[gi]])
                    dma("sp", rtgs[gi][:].rearrange("p (a n) -> p a n", a=3), rt_d.rearrange("a p n -> p a n")[:, :, t0:t0 + GP], [], [b_rtgs[gi]])

                NOH = 8
                oh1 = [sbt(es, "oh1b_%d" % i, [128, 128], BF16) for i in range(NOH)]; b_oh1 = [Buf() for _ in range(NOH)]
                oh2 = [sbt(es, "oh2b_%d" % i, [128, 128], BF16) for i in range(NOH)]; b_oh2 = [Buf() for _ in range(NOH)]

                def wd_oh(grp, t):
                    gi = grp % 2
                    rtg3 = rtgs[gi][:].rearrange("p (a n) -> p a n", a=3)
                    o1 = oh1[t % NOH]; bo1 = b_oh1[t % NOH]
                    o2 = oh2[t % NOH]; bo2 = b_oh2[t % NOH]
                    ts("dve", o1[:], iob[:], rtg3[:, 0, t:t + 1], None, ALU.is_equal, None, [b_iob, b_rtgs[gi]], [bo1])
                    ts("dve", o2[:], iob[:], rtg3[:, 1, t:t + 1], rtg3[:, 2, t:t + 1], ALU.is_equal, ALU.mult, [b_iob, b_rtgs[gi]], [bo2])

                def wd_mm(grp, t):
                    bk = 6 + (t // 4) % 2
                    mm(bank(bk, (t % 4) * 128, (t % 4) * 128 + 128), oh1[t % NOH][:], oh2[t % NOH][:], True, True,
                       [b_oh1[t % NOH], b_oh2[t % NOH]], [pb[bk]])

                def wd_cp(grp, t4):
                    gi = grp % 2
                    bk = 6 + t4 % 2
                    cp("act", Wds[gi][:, t4 * 512:(t4 + 1) * 512], bank(bk), [pb[bk]], [b_Wds[gi][t4]])

                def wd_stage(grp, s):
                    n = GP // 2
                    if 0 <= s < n:
                        wd_oh(grp, 2 * s); wd_oh(grp, 2 * s + 1)
                    if 0 <= s - 1 < n:
                        wd_mm(grp, 2 * (s - 1)); wd_mm(grp, 2 * (s - 1) + 1)
                    if s - 3 >= 0 and (s - 3) % 2 == 1 and (s - 3) < n:
                        wd_cp(grp, (s - 3) // 2)

                def load2(grp_, cq):
                    i3 = (grp_ * 64 + cq) % NSB
                    dma("sp", UTs[i3][:].rearrange("p (c n) -> p c n", c=2), utv[:, cq * 2:(cq + 1) * 2, :], [b_ut[cq // 8]], [b_UTs[i3]])
                    dma("sp", Vcs[i3][:].rearrange("p (c n) -> p c n", c=2), vtv[:, cq * 2:(cq + 1) * 2, :], [b_vt[cq // 8]], [b_Vcs[i3]])

                def emitH(grp_, c2):
                    i3 = (grp_ * 64 + c2 // 2) % NSB
                    cc = c2 % 2
                    bh = 4 + c2 % 2
                    h2g3_ = h2gs[grp_ % 2][:].rearrange("p (k n) -> p k n", k=8)
                    for k in range(8):
                        mm(bank(bh, 0, GP), UTs[i3][:, cc * 1024 + k * 128:cc * 1024 + (k + 1) * 128], h2g3_[:, k, :],
                           k == 0, k == 7, [b_UTs[i3], b_h2gs[grp_ % 2]], [pb[bh]], silent=(k < 7))

                xsls = [xsl, sbt(es, "xsl1", [128, D], F32)]; b_xsls = [b_xsl, Buf()]
                yts = [yt, sbt(es, "yt1", [128, D], F32)]; b_yts = [b_yt, Buf()]
                load_grp(0)
                for s in range(GP // 2 + 4):
                    wd_stage(0, s)
                load2(0, 0)
                load2(0, 1)
                emitH(0, 0)
                for grp in range(ngrp):
                    gi = grp % 2
                    t0 = grp * GP
                    Wd = Wds[gi]; b_Wd = b_Wds[gi]
                    if grp + 1 < ngrp:
                        load_grp(grp + 1)
                    for tl in range(GP // 128):
                        tg = t0 // 128 + tl
                        dma("sp", xsls[tl][:], xs_d[tg * 128:(tg + 1) * 128, :], [], [b_xsls[tl]])
                    for c2 in range(128):
                        cq, cc = c2 // 2, c2 % 2
                        if cc == 0 and cq + 2 < 64:
                            load2(grp, cq + 2)
                        if c2 + 1 < 128:
                            emitH(grp, c2 + 1)
                        i3 = (grp * 64 + cq) % NSB
                        bh = 4 + c2 % 2
                        g_ = gl[c2 % 2]; bg_ = b_gl[c2 % 2]
                        a_ = At[c2 % 2]; ba_ = b_At[c2 % 2]
                        act(g_[:], bank(bh, 0, GP), AF.Gelu, [pb[bh]], [bg_])
                        tt("dve", a_[:], g_[:], XAP(Wd[:], [[128, GP]], c2), ALU.mult, [bg_] + b_Wd, [ba_])
                        for tl in range(GP // 128):
                            for nb in range(2):
                                bo = tl * 2 + nb
                                mm(bank(bo), a_[:, tl * 128:(tl + 1) * 128], Vcs[i3][:, cc * 1024 + nb * 512:cc * 1024 + (nb + 1) * 512],
                                   c2 == 0, c2 == 127, [ba_, b_Vcs[i3]], [pb[bo]], silent=(bo < GP // 64 - 1))
                        if grp + 1 < ngrp:
                            wd_stage(grp + 1, c2)
                    if grp + 1 < ngrp:
                        for s in range(128, GP // 2 + 4):
                            wd_stage(grp + 1, s)
                        load2(grp + 1, 0)
                        load2(grp + 1, 1)
                        emitH(grp + 1, 0)
                    for tl in range(GP // 128):
                        tt("dve", yts[tl][:], PS[:, tl * 1024:(tl + 1) * 1024], g2b[:], ALU.mult, [pb[tl * 2], pb[tl * 2 + 1], b_g2b], [b_yts[tl]])
                    for tl in range(GP // 128):
                        tg = t0 // 128 + tl
                        yt_ = yts[tl]; b_yt_ = b_yts[tl]
                        tt("pool", yt_[:], yt_[:], xsls[tl][:], ALU.add, [b_yt_, b_xsls[tl]], [b_yt_])
                        act(junk[:], yt_[:], AF.Square, [b_yt_], [b_junk, b_st4], accum=st4[:, 0:1])
                        ts("dve", st4[:, 1:2], st4[:, 0:1], 1.0 / D, EPS, ALU.mult, ALU.add, [b_st4], [b_st4])
                        act(st4[:, 2:3], st4[:, 1:2], AF.Sqrt, [b_st4], [b_st4])
                        rcp(st4[:, 3:4], st4[:, 2:3], [b_st4], [b_st4])
                        stt(yo[:], yt_[:], st4[:, 3:4], fgb[:], ALU.mult, ALU.mult, [b_yt_, b_st4, b_fgb], [b_yo])
                        dma("sp", out_d[tg * 128:(tg + 1) * 128, :], yo[:], [b_yo], [])
        S.emit()
    return nc


def host_prep(inputs):
    f = lambda a: np.ascontiguousarray(np.asarray(a, dtype=np.float32))
    col = lambda v: f(np.asarray(v).reshape(8, 128).T)
    shared = {}
    shared["w_mod"] = f(inputs["w_mod"][0])
    shared["b_mod"] = f(inputs["b_mod"][0].reshape(1, -1))
    shared["ncol"] = f(np.concatenate([col(inputs["norm1_g"][0]), col(inputs["norm2_g"][0]), col(inputs["conv_b"][0]),
                                       col(inputs["conv_ln_g"][0]), col(inputs["conv_ln_b"][0])], axis=1))
    shared["w_in"] = f(inputs["w_in"][0])
    shared["qkg"] = f(np.concatenate([inputs["q_norm_g"][0], inputs["k_norm_g"][0]]).reshape(1, 256))
    shared["w_attn_o"] = f(inputs["w_attn_o"][0])
    cdw = np.asarray(inputs["conv_dw"][0])
    shared["cdw"] = f(cdw.T.reshape(8, 128, 31).transpose(1, 0, 2).reshape(128, 8 * 31))
    shared["w_conv_o"] = f(inputs["w_conv_o"][0])
    shared["w_out"] = f(inputs["w_out"][0])
    shared["peer_wq"] = f(inputs["peer_wq"][0])
    keys = np.asarray(inputs["peer_keys"][0])
    shared["keyT"] = f(keys.reshape(16, 128, 128).transpose(2, 0, 1).reshape(128, 16 * 128))
    U = np.asarray(inputs["peer_u"][0])
    shared["ut_l"] = f(U.reshape(128, 128, 8, 128).transpose(1, 3, 2, 0).reshape(16384, 1024))
    V = np.asarray(inputs["peer_v"][0])
    shared["v_l"] = f(V.reshape(128, 128, 1024).transpose(1, 0, 2).reshape(16384, 1024))
    shared["fg"] = f(np.asarray(inputs["final_norm_g"]).reshape(1, -1))
    t = np.arange(T)
    row = (t // 64).astype(np.float32)
    colp = (t % 64).astype(np.float32)
    inv = (np.float32(10000.0) ** (-np.arange(0, 64, 2, dtype=np.float32) / np.float32(64))).astype(np.float32)
    ar = row[:, None] * inv[None, :]
    ac = colp[:, None] * inv[None, :]
    cr, sr, cc, sc = np.cos(ar), np.sin(ar), np.cos(ac), np.sin(ac)
    shared["rope_c"] = f(np.concatenate([cr, cr, cc, cc], axis=1))
    shared["rope_s"] = f(np.concatenate([-sr, sr, -sc, sc], axis=1))
    in_maps = []
    cctx = col(inputs["c_ctx"])
    for b in range(8):
        m = dict(shared)
        m["x"] = f(inputs["x"][b])
        m["ctx"] = f(inputs["ctx"][b])
        m["ccol"] = f(np.concatenate([col(inputs["c"][b]), cctx], axis=1))
        in_maps.append(m)
    return in_maps


def kernel(**inputs):
    in_maps = host_prep(inputs)
    nc = build()
    res = run_bass_kernel_spmd(nc, in_maps, core_ids=list(range(8)))
    return np.stack([np.asarray(r["out"], dtype=np.float32) for r in res.results], axis=0)
```

```python
import numpy as np
from contextlib import ExitStack
import concourse.bass as bass
import concourse.mybir as mybir
from concourse.bass_utils import run_bass_kernel_spmd

F32 = mybir.dt.float32
BF16 = mybir.dt.bfloat16
I32 = mybir.dt.int32
U32 = mybir.dt.uint32
AF = mybir.ActivationFunctionType
ALU = mybir.AluOpType
AX = mybir.AxisListType

T = 4096
NT = 32
CTXN = 256
NKEY = T + CTXN
NKT = NKEY // 128
D = 1024
KC = 8
INW = 5632
EPS = 1e-6
GP = 256
ENGS = ("pe", "act", "dve", "pool", "sp")


class Buf:
    __slots__ = ("name", "lw", "rd")

    def __init__(self, name=""):
        self.name = name
        self.lw = None
        self.rd = {}


class Sched:
    def __init__(self, nc, n_dma_sems=(("sp", 16), ("pool", 40), ("act", 4))):
        self.nc = nc
        self.q = {e: [] for e in ENGS}
        self.sems = {}
        self.cnt = {}
        for e in ENGS:
            self.sems[e] = nc.alloc_semaphore(name="s_" + e)
            self.cnt[e] = 0
        self.dsem = {}
        self.drr = {}
        for e, n in n_dma_sems:
            self.dsem[e] = []
            for i in range(n):
                k = "d_%s_%d" % (e, i)
                self.sems[k] = nc.alloc_semaphore(name=k)
                self.cnt[k] = 0
                self.dsem[e].append(k)
            self.drr[e] = 0
        self.seen = {e: {} for e in ENGS}
        self.nops = 0

    def _deps(self, reads, writes):
        deps = {}

        def need(tok):
            if tok is None:
                return
            k, v = tok
            if deps.get(k, 0) < v:
                deps[k] = v
        for r in reads:
            need(r.lw)
        for w in writes:
            need(w.lw)
            for k, v in w.rd.items():
                need((k, v))
        return deps

    def _push(self, eng, deps, fn, sk, inc, reads, writes, silent=False):
        waits = []
        seen = self.seen[eng]
        for k, v in deps.items():
            if k == eng and v > self.cnt[k]:
                continue
            if seen.get(k, 0) < v:
                seen[k] = v
                waits.append((k, v))
        if silent:
            val = self.cnt[sk] + inc
            self.q[eng].append((waits, fn, None, 0))
        else:
            self.cnt[sk] += inc
            val = self.cnt[sk]
            self.q[eng].append((waits, fn, sk, inc))
        for r in reads:
            if r.rd.get(sk, 0) < val:
                r.rd[sk] = val
        for w in writes:
            w.lw = (sk, val)
            w.rd = {}
        self.nops += 1

    def op(self, eng, fn, reads=(), writes=(), silent=False):
        self._push(eng, self._deps(reads, writes), fn, eng, 1, reads, writes, silent)

    def dma(self, eng, fn, reads=(), writes=()):
        deps = self._deps(reads, writes)
        k = self.dsem[eng][self.drr[eng] % len(self.dsem[eng])]
        self.drr[eng] += 1
        if self.cnt[k] > 0 and deps.get(k, 0) < self.cnt[k]:
            deps[k] = self.cnt[k]
        self._push(eng, deps, fn, k, 16, reads, writes)

    def barrier(self, exclude=()):
        for e in ENGS:
            waits = []
            for k, v in self.cnt.items():
                if k in exclude or v == 0:
                    continue
                if self.seen[e].get(k, 0) < v:
                    self.seen[e][k] = v
                    waits.append((k, v))
            if waits:
                self.q[e].append((waits, None, None, 0))

    def emit(self):
        nc = self.nc
        sems = self.sems
        q = self.q
        cnt = self.cnt
        with nc.Block() as block:
            def mk(ename):
                def body(e):
                    for waits, fn, sk, inc in q[ename]:
                        for k, v in waits:
                            e.wait_ge(sems[k], v)
                        if fn is not None:
                            if sk is None:
                                fn(e)
                            else:
                                fn(e).then_inc(sems[sk], inc)
                    if ename == "sp":
                        for k, v in cnt.items():
                            if v > 0:
                                e.wait_ge(sems[k], v)
                return body
            block.tensor(mk("pe"))
            block.scalar(mk("act"))
            block.vector(mk("dve"))
            block.gpsimd(mk("pool"))
            block.sync(mk("sp"))


def XAP(base, dims, off=0):
    return bass.AP(tensor=base.tensor, offset=base.offset + off,
                   ap=[list(base.ap[0])] + [list(d) for d in dims])


def build(upto=9, dbg=()):
    nc = bass.Bass("TRN2", target_bir_lowering=False)
    dt_in = lambda n, s, d=F32: nc.dram_tensor(n, list(s), d, kind="ExternalInput").ap()
    x_d = dt_in("x", [T, D])
    ctx_d = dt_in("ctx", [CTXN, D])
    ccol_d = dt_in("ccol", [128, 16])
    wmod_d = dt_in("w_mod", [D, 6 * D])
    bmod_d = dt_in("b_mod", [1, 6 * D])
    ncol_d = dt_in("ncol", [128, 40])
    win_d = dt_in("w_in", [D, INW])
    qkg_d = dt_in("qkg", [1, 256])
    wao_d = dt_in("w_attn_o", [D, D])
    cdw_d = dt_in("cdw", [128, 8 * 31])
    wco_d = dt_in("w_conv_o", [D, D])
    wout_d = dt_in("w_out", [D, D])
    wq_d = dt_in("peer_wq", [D, 2048])
    keyT_d = dt_in("keyT", [128, 16 * 128])
    ut_d = dt_in("ut_l", [16384, 1024])
    vl_d = dt_in("v_l", [16384, 1024])
    fg_d = dt_in("fg", [1, D])
    ropec_d = dt_in("rope_c", [T, 128])
    ropes_d = dt_in("rope_s", [T, 128])
    out_d = nc.dram_tensor("out", [T, D], F32, kind="ExternalOutput").ap()
    dbg_d = {}
    for n, s, d in dbg:
        dbg_d[n] = nc.dram_tensor("dbg_" + n, list(s), d, kind="ExternalOutput").ap()

    scr = lambda n, s, d: nc.dram_tensor(n, list(s), d, kind="Internal").ap()
    utbf_d = scr("utbf", [16384, 1024], BF16)
    vbf_d = scr("vbf", [16384, 1024], BF16)
    qt_d = scr("qt_scr", [8, 128, T], BF16)
    gt_d = scr("gt_scr", [8, 128, T + 30], BF16)
    sg_d = scr("sg_scr", [16, 128, T], BF16)
    xs_d = scr("xs_scr", [T, D], F32)
    h2_d = scr("h2_scr", [8, 128, T], BF16)
    rt_d = scr("rt_scr", [3, 128, T], F32)
    g2_d = scr("g2_scr", [1, D], F32)
    waobf_d = scr("wao_bf", [D, D], BF16); b_waobf = Buf()
    wcobf_d = scr("wco_bf", [D, D], BF16); b_wcobf = Buf()
    woutbf_d = scr("wout_bf", [D, D], BF16); b_woutbf = Buf()
    wqbf_d = scr("wq_bf", [D, 2048], BF16); b_wqbf = Buf()
    keyTbf_d = scr("keyT_bf", [128, 2048], BF16); b_keyTbf = Buf()
    b_g2d = Buf()
    b_qt = [Buf() for _ in range(8)]
    b_gt = [Buf() for _ in range(8)]
    b_gtpad = Buf()
    b_sg = [Buf() for _ in range(8)]
    b_xs = [Buf() for _ in range(NT)]
    b_h2 = [Buf() for _ in range(8)]
    b_rt = [Buf() for _ in range(8)]

    S = Sched(nc)

    def mm(out, lhsT, rhs, start, stop, r, w, silent=False):
        S.op("pe", lambda e: e.matmul(out, lhsT=lhsT, rhs=rhs, start=start, stop=stop), r, w, silent=silent)

    def tr(out, in_, ident, r, w):
        S.op("pe", lambda e: e.transpose(out=out, in_=in_, identity=ident), r, w)

    def act(out, in_, func, r, w, bias=None, scale=None, accum=None, eng="act"):
        kw = {}
        if bias is not None:
            kw["bias"] = bias
        if scale is not None:
            kw["scale"] = scale
        if accum is not None:
            kw["accum_out"] = accum
        S.op(eng, lambda e: e.activation(out=out, in_=in_, func=func, **kw), r, w)

    def tt(eng, out, in0, in1, op, r, w):
        S.op(eng, lambda e: e.tensor_tensor(out=out, in0=in0, in1=in1, op=op), r, w)

    def ts(eng, out, in0, s1, s2, op0, op1, r, w):
        if op1 is None:
            S.op(eng, lambda e: e.tensor_scalar(out=out, in0=in0, scalar1=s1, scalar2=None, op0=op0), r, w)
        else:
            S.op(eng, lambda e: e.tensor_scalar(out=out, in0=in0, scalar1=s1, scalar2=s2, op0=op0, op1=op1), r, w)

    def stt(out, in0, scalar, in1, op0, op1, r, w):
        S.op("dve", lambda e: e.scalar_tensor_tensor(out=out, in0=in0, scalar=scalar, in1=in1, op0=op0, op1=op1), r, w)

    def cp(eng, out, in_, r, w):
        if eng == "act":
            S.op("act", lambda e: e.activation(out=out, in_=in_, func=AF.Copy), r, w)
        else:
            S.op(eng, lambda e: e.tensor_copy(out=out, in_=in_), r, w)

    def rcp(out, in_, r, w):
        S.op("dve", lambda e: e.reciprocal(out=out, in_=in_), r, w)

    def dma(q, out, in_, r, w):
        S.dma(q, lambda e: e.dma_start(out=out, in_=in_), r, w)

    def dump(name, ap, r):
        if name in dbg_d:
            dma("sp", dbg_d[name], ap, r, [])

    top = ExitStack()
    with top:
        sbt = lambda es, n, s, d: es.enter_context(nc.sbuf_tensor("sb_" + n, list(s), d))
        PS = top.enter_context(nc.psum_tensor("PS", [128, 4096], F32))
        PSB = PS.bitcast(BF16)
        pb = [Buf("bank%d" % i) for i in range(8)]
        bank = lambda i, a=0, b=512: PS[:, i * 512 + a:i * 512 + b]
        bankb = lambda i, a=0, b=1024: PSB[:, i * 1024 + a:i * 1024 + b]

        ident_f = sbt(top, "ident_f", [128, 128], F32); b_idf = Buf()
        ident_b = sbt(top, "ident_b", [128, 128], BF16); b_idb = Buf()
        ones_f = sbt(top, "ones_f", [128, 128], F32); b_1f = Buf()
        ones_b = sbt(top, "ones_b", [128, 128], BF16); b_1b = Buf()
        modc = sbt(top, "modc", [128, 48], F32); b_modc = Buf()
        ncol = sbt(top, "ncol", [128, 40], F32); b_ncol = Buf()
        mid = ExitStack()
        g1b = sbt(mid, "g1b", [128, D], F32); b_g1b = Buf()
        KT = sbt(mid, "KT", [128, 2 * NKEY], BF16); b_KT = [Buf() for _ in range(NKT)]
        Vs = sbt(mid, "Vs", [128, NKT * 256], BF16); b_V = [Buf() for _ in range(NKT)]

        S.op("pool", lambda e: e.memset(ident_f[:], 0.0), [], [b_idf])
        S.op("pool", lambda e: e.affine_select(out=ident_f[:], in_=ident_f[:], pattern=[[-1, 128]],
                                               compare_op=ALU.not_equal, fill=1.0, base=0, channel_multiplier=1),
             [b_idf], [b_idf])
        cp("dve", ident_b[:], ident_f[:], [b_idf], [b_idb])
        S.op("pool", lambda e: e.memset(ones_f[:], 1.0), [], [b_1f])
        cp("dve", ones_b[:], ones_f[:], [b_1f], [b_1b])
        dma("sp", ncol[:], ncol_d, [], [b_ncol])

        b_ut = [Buf() for _ in range(8)]
        b_vt = [Buf() for _ in range(8)]
        def cast_tables(after=()):
            if upto >= 4:
                for a in range(8):
                    dma("pool", utbf_d[a * 2048:(a + 1) * 2048, :], ut_d[a * 2048:(a + 1) * 2048, :], list(after), [b_ut[a]])
                    dma("pool", vbf_d[a * 2048:(a + 1) * 2048, :], vl_d[a * 2048:(a + 1) * 2048, :], list(after), [b_vt[a]])

        w01 = ExitStack()
        win = sbt(w01, "win", [128, 8 * INW], BF16); b_win = [Buf() for _ in range(11)]
        winv = win_d.rearrange("(k p) n -> p k n", p=128)
        win3 = win[:].rearrange("p (k n) -> p k n", k=8)

        with ExitStack() as es:
            ccol = sbt(es, "ccol", [128, 16], F32); b_ccol = Buf()
            cs = sbt(es, "cs", [128, 16], F32); b_cs = Buf()
            wm = [sbt(es, "wm%d" % i, [128, 8 * 512], F32) for i in range(2)]; b_wm = [Buf(), Buf()]
            bmj = [sbt(es, "bm%d" % i, [1, 512], F32) for i in range(2)]; b_bmj = [Buf(), Buf()]
            mrow = sbt(es, "mrow", [1, 6 * D], F32); b_mrow = Buf()
            mrowc = sbt(es, "mrowc", [1, 2 * D], F32); b_mrowc = Buf()
            tmpc = sbt(es, "tmpc", [128, 48], F32); b_tmpc = Buf()
            dma("sp", ccol[:], ccol_d, [], [b_ccol])
            act(cs[:], ccol[:], AF.Silu, [b_ccol], [b_cs])
            wmv = wmod_d.rearrange("(k p) n -> p k n", p=128)
            for j in range(12):
                w_ = wm[j % 2]
                dma("sp", w_[:].rearrange("p (k n) -> p k n", k=8), wmv[:, :, j * 512:(j + 1) * 512], [], [b_wm[j % 2]])
                bm = bmj[j % 2]; b_bm = b_bmj[j % 2]
                dma("sp", bm[:], bmod_d[0:1, j * 512:(j + 1) * 512], [], [b_bm])
                if upto >= 1 and j >= 1:
                    wi_ = wm[(j - 1) % 2]
                    dma("sp", wi_[:].rearrange("p (k n) -> p k n", k=8), winv[:, :, (j - 1) * 512:j * 512], [], [b_wm[(j - 1) % 2]])
                    cp("act", win3[:, :, (j - 1) * 512:j * 512], wi_[:].rearrange("p (k n) -> p k n", k=8),
                       [b_wm[(j - 1) % 2]], [b_win[j - 1]])
                for k in range(8):
                    mm(bank(0)[0:1, :], cs[:, k:k + 1], w_[:, k * 512:(k + 1) * 512], k == 0, k == 7,
                       [b_cs, b_wm[j % 2]], [pb[0]])
                tt("dve", mrow[0:1, j * 512:(j + 1) * 512], bank(0)[0:1, :], bm[0:1, :], ALU.add,
                   [pb[0], b_bm], [b_mrow])
                if j < 4:
                    for k in range(8):
                        mm(bank(1)[0:1, :], cs[:, 8 + k:9 + k], w_[:, k * 512:(k + 1) * 512], k == 0, k == 7,
                           [b_cs, b_wm[j % 2]], [pb[1]])
                    tt("dve", mrowc[0:1, j * 512:(j + 1) * 512], bank(1)[0:1, :], bm[0:1, :], ALU.add,
                       [pb[1], b_bm], [b_mrowc])
            segs = [(mrow, 0, b_mrow), (mrow, 1, b_mrow), (mrowc, 0, b_mrowc), (mrowc, 1, b_mrowc),
                    (mrow, 3, b_mrow), (mrow, 4, b_mrow)]
            for si, (row, seg, bb) in enumerate(segs):
                for k in range(8):
                    mm(bank(2)[:, si * 8 + k:si * 8 + k + 1], row[0:1, seg * D + k * 128:seg * D + (k + 1) * 128],
                       ones_f[0:1, 0:1], True, True, [bb, b_1f], [pb[2]])
            cp("dve", tmpc[:], bank(2)[:, 0:48], [pb[2]], [b_tmpc])
            for (dst, sc_i, sh_i, ng_off) in ((0, 1, 0, 0), (2, 3, 2, 0), (4, 5, 4, 8)):
                stt(modc[:, dst * 8:dst * 8 + 8], tmpc[:, sc_i * 8:sc_i * 8 + 8], 1.0, ncol[:, ng_off:ng_off + 8],
                    ALU.add, ALU.mult, [b_tmpc, b_ncol], [b_modc])
                cp("dve", modc[:, (dst + 1) * 8:(dst + 1) * 8 + 8], tmpc[:, sh_i * 8:sh_i * 8 + 8], [b_tmpc], [b_modc])
            for (dstt, bdst, seg) in ((g1b, b_g1b, 2),):
                for nb in range(2):
                    mm(bank(3 + nb), ones_f[0:1, :], mrow[0:1, seg * D + nb * 512:seg * D + (nb + 1) * 512], True, True,
                       [b_mrow, b_1f], [pb[3 + nb]])
                    cp("dve", dstt[:, nb * 512:(nb + 1) * 512], bank(3 + nb), [pb[3 + nb]], [bdst])
            dma("sp", g2_d, mrow[0:1, 5 * D:6 * D], [b_mrow], [b_g2d])
            dump("modc", modc[:], [b_modc])
            dump("g1b", g1b[:], [b_g1b])
            S.barrier(exclude=S.dsem["pool"])

        if upto >= 1:
            with ExitStack() as es:
                bw = lambda c0, c1: [b_win[i] for i in range(c0 // 512, (c1 - 1) // 512 + 1)]
                dma("pool", waobf_d, wao_d, [], [b_waobf])
                dma("pool", wcobf_d, wco_d, [], [b_wcobf])
                dma("pool", woutbf_d, wout_d, [], [b_woutbf])
                xt = [sbt(es, "xt%d" % i, [128, D], F32) for i in range(2)]; b_xt = [Buf(), Buf()]
                junk = sbt(es, "junk", [128, D], BF16); b_junk = Buf()
                st4 = sbt(es, "st4", [128, 4], F32); b_st4 = Buf()
                xn = sbt(es, "xn", [128, D], F32); b_xn = Buf()
                hT = sbt(es, "hT", [128, 8 * 512], BF16); b_hT = [Buf() for _ in range(4)]
                hT3 = hT[:].rearrange("p (k n) -> p k n", k=8)
                sq = sbt(es, "sq", [128, 1280], F32); b_sq = Buf()
                qn = sbt(es, "qn", [128, 1280], F32); b_qn = Buf()
                t2 = sbt(es, "t2", [128, 1280], F32); b_t2 = Buf()
                qr = sbt(es, "qr", [128, 1280], BF16); b_qr = Buf()
                s10 = sbt(es, "s10", [128, 32], F32); b_s10 = Buf()
                Gt = sbt(es, "Gt", [128, 1280], F32); b_Gt = Buf()
                gq = sbt(es, "gq", [128, 256], F32); b_gq = Buf()
                rc = [sbt(es, "rc%d" % i, [128, 128], F32) for i in range(2)]; b_rc = [Buf(), Buf()]
                rs = [sbt(es, "rs%d" % i, [128, 128], F32) for i in range(2)]; b_rs = [Buf(), Buf()]
                QTb = sbt(es, "QTb", [128, 8 * 512], BF16); b_QTb = Buf()
                QTb3 = QTb[:].rearrange("p (h n) -> p h n", h=8)
                sgt = sbt(es, "sgt", [128, 512], F32); b_sgt = Buf()
                ob = [sbt(es, "ob%d" % i, [128, 512], BF16) for i in range(2)]; b_ob = [Buf(), Buf()]
                zpad = sbt(es, "zpad", [128, 8 * 15], BF16); b_zpad = Buf()

                dma("sp", gq[:], bass.AP(tensor=qkg_d.tensor, offset=0, ap=[[0, 128], [1, 256]]), [], [b_gq])
                ts("dve", Gt[:, 0:1024].rearrange("p (h d) -> p h d", h=8), XAP(gq[:, 0:128], [[0, 8], [1, 128]]),
                   float(128 ** -0.5), None, ALU.mult, None, [b_gq], [b_Gt])
                cp("dve", Gt[:, 1024:1280].rearrange("p (h d) -> p h d", h=2), XAP(gq[:, 128:256], [[0, 2], [1, 128]]), [b_gq], [b_Gt])
                S.op("pool", lambda e: e.memset(zpad[:], 0.0), [], [b_zpad])
                gtv = gt_d.rearrange("k p n -> p k n")
                dma("sp", gtv[:, :, 0:15], zpad[:].rearrange("p (k n) -> p k n", k=8), [b_zpad], [b_gtpad])
                dma("sp", gtv[:, :, T + 15:T + 30], zpad[:].rearrange("p (k n) -> p k n", k=8), [b_zpad], [b_gtpad])

                t1r = sbt(es, "t1r", [128, 1280], F32); b_t1r = Buf()

                def p1_tile(src_ap, ti_in_blk, key_tile, is_ctx, pos0, cnt):
                    i2 = cnt % 2
                    a_off, b_off = (16, 24) if is_ctx else (0, 8)
                    if is_ctx:
                        c0, nh, banks = 1024, 2, [pb[4]]
                        src_qk = bank(4, 0, 256)
                    else:
                        c0, nh, banks = 0, 10, [pb[2], pb[3], pb[4]]
                        src_qk = PS[:, 2 * 512:2 * 512 + 1280]
                    w_ = nh * 128

                    def front():
                        dma("sp", xt[i2][:], src_ap, [], [b_xt[i2]])
                        if not is_ctx:
                            dma("sp", rc[i2][:], ropec_d[pos0:pos0 + 128, :], [], [b_rc[i2]])
                            dma("sp", rs[i2][:], ropes_d[pos0:pos0 + 128, :], [], [b_rs[i2]])
                        act(junk[:], xt[i2][:], AF.Square, [b_xt[i2]], [b_junk, b_st4], accum=st4[:, 0:1])
                        ts("dve", st4[:, 1:2], st4[:, 0:1], 1.0 / D, EPS, ALU.mult, ALU.add, [b_st4], [b_st4])
                        act(st4[:, 2:3], st4[:, 1:2], AF.Sqrt, [b_st4], [b_st4])
                        rcp(st4[:, 3:4], st4[:, 2:3], [b_st4], [b_st4])
                        ts("dve", xn[:], xt[i2][:], st4[:, 3:4], None, ALU.mult, None, [b_xt[i2], b_st4], [b_xn])
                        for k in range(8):
                            tr(bank(k // 4, (k % 4) * 128, (k % 4) * 128 + 128), xn[:, k * 128:(k + 1) * 128], ident_f[:],
                               [b_xn, b_idf], [pb[k // 4]])
                        for k in range(8):
                            dst = hT3[:, k, ti_in_blk * 128:(ti_in_blk + 1) * 128]
                            src = bank(k // 4, (k % 4) * 128, (k % 4) * 128 + 128)
                            if k % 2 == 0:
                                act(dst, src, AF.Identity, [pb[k // 4], b_modc], [b_hT[ti_in_blk]],
                                    bias=modc[:, b_off + k:b_off + k + 1], scale=modc[:, a_off + k:a_off + k + 1])
                            else:
                                ts("dve", dst, src, modc[:, a_off + k:a_off + k + 1], modc[:, b_off + k:b_off + k + 1],
                                   ALU.mult, ALU.add, [pb[k // 4], b_modc], [b_hT[ti_in_blk]])
                        for cb in ((2,) if is_ctx else (0, 1, 2)):
                            for k in range(8):
                                mm(bank(2 + cb), hT3[:, k, ti_in_blk * 128:(ti_in_blk + 1) * 128],
                                   win3[:, k, cb * 512:(cb + 1) * 512], k == 0, k == 7,
                                   [b_hT[ti_in_blk]] + bw(cb * 512, cb * 512 + 512), [pb[2 + cb]], silent=(k < 7))

                    def backA():
                        cp("act", Vs[:, key_tile * 256:(key_tile + 1) * 256], bank(4, 256, 512), [pb[4]], [b_V[key_tile]])
                        act(sq[:, c0:c0 + w_], src_qk, AF.Square, banks, [b_sq])
                        S.op("dve", lambda e: e.tensor_reduce(out=s10[:, 0:nh], in_=sq[:, c0:c0 + w_].rearrange("p (h d) -> p h d", h=nh),
                                                              axis=AX.X, op=ALU.add), [b_sq], [b_s10])
                        ts("dve", s10[:, 10:10 + nh], s10[:, 0:nh], 1.0 / 128, EPS, ALU.mult, ALU.add, [b_s10], [b_s10])
                        act(s10[:, 20:20 + nh], s10[:, 10:10 + nh], AF.Sqrt, [b_s10], [b_s10])
                        rcp(s10[:, 0:nh], s10[:, 20:20 + nh], [b_s10], [b_s10])
                        tt("dve", qn[:, c0:c0 + w_].rearrange("p (h d) -> p h d", h=nh), src_qk.rearrange("p (h d) -> p h d", h=nh),
                           XAP(s10[:, 0:nh], [[1, nh], [0, 128]]), ALU.mult, banks + [b_s10], [b_qn])

                    def backB():
                        tt("pool", qn[:, c0:c0 + w_], qn[:, c0:c0 + w_], Gt[:, c0:c0 + w_], ALU.mult, [b_qn, b_Gt], [b_qn])
                        if is_ctx:
                            cp("dve", qr[:, 1024:1280], qn[:, 1024:1280], [b_qn], [b_qr])
                        else:
                            tt("pool", t1r[:].rearrange("p (h d) -> p h d", h=10), qn[:].rearrange("p (h d) -> p h d", h=10),
                               XAP(rc[i2][:], [[0, 10], [1, 128]]), ALU.mult, [b_qn, b_rc[i2]], [b_t1r])
                            for hf in range(2):
                                o_off, i_off = hf * 32, (1 - hf) * 32
                                tt("dve", XAP(t2[:], [[128, 10], [64, 2], [1, 32]], o_off),
                                   XAP(qn[:], [[128, 10], [64, 2], [1, 32]], i_off),
                                   XAP(rs[i2][:], [[0, 10], [64, 2], [1, 32]], o_off), ALU.mult, [b_qn, b_rs[i2]], [b_t2])
                            tt("dve", qr[:], t1r[:], t2[:], ALU.add, [b_t1r, b_t2], [b_qr])
                        if not is_ctx:
                            for h in range(8):
                                tr(bankb(5, h * 128, (h + 1) * 128), qr[:, h * 128:(h + 1) * 128], ident_b[:], [b_qr, b_idb], [pb[5]])
                            cp("act", QTb3[:, :, ti_in_blk * 128:(ti_in_blk + 1) * 128],
                               bankb(5).rearrange("p (h n) -> p h n", h=8), [pb[5]], [b_QTb])
                        for h in range(2):
                            tr(bankb(6, h * 128, (h + 1) * 128), qr[:, 1024 + h * 128:1024 + (h + 1) * 128], ident_b[:],
                               [b_qr, b_idb], [pb[6]])
                        cp("dve", XAP(KT[:, key_tile * 128:(key_tile + 1) * 128], [[NKEY, 2], [1, 128]]),
                           bankb(6, 0, 256).rearrange("p (h n) -> p h n", h=2), [pb[6]], [b_KT[key_tile]])
                    return front, backA, backB

                def run_tiles(tiles, mid_fn=None):
                    n_ = len(tiles)
                    tiles[0][0]()
                    for i_ in range(n_):
                        tiles[i_][1]()
                        if i_ + 1 < n_:
                            tiles[i_ + 1][0]()
                        elif mid_fn is not None:
                            mid_fn()
                        tiles[i_][2]()

                cnt = 0
                ctl = []
                for ci in range(2):
                    ctl.append(p1_tile(ctx_d[ci * 128:(ci + 1) * 128, :], ci, NT + ci, True, 0, cnt))
                    cnt += 1
                run_tiles(ctl)
                for blk in range(8):
                    tl_ = []
                    for ti in range(4):
                        tg = blk * 4 + ti
                        tl_.append(p1_tile(x_d[tg * 128:(tg + 1) * 128, :], ti, tg, False, tg * 128, cnt))
                        cnt += 1
                    obi = [0]

                    def glu_part():
                        for j in range(8):
                            ca = 1536 + j * 128
                            cb_ = 1536 + 1024 + j * 128
                            ba, bb_ = (0, 1) if j % 2 == 0 else (2, 3)
                            for k in range(8):
                                mm(bank(ba), win3[:, k, ca:ca + 128], hT3[:, k, :], k == 0, k == 7, b_hT + bw(ca, ca + 128), [pb[ba]], silent=(k < 7))
                            for k in range(8):
                                mm(bank(bb_), win3[:, k, cb_:cb_ + 128], hT3[:, k, :], k == 0, k == 7, b_hT + bw(cb_, cb_ + 128), [pb[bb_]], silent=(k < 7))
                            act(sgt[:], bank(bb_), AF.Sigmoid, [pb[bb_]], [b_sgt])
                            o = ob[obi[0] % 2]; bo = b_ob[obi[0] % 2]; obi[0] += 1
                            tt("dve", o[:], bank(ba), sgt[:], ALU.mult, [pb[ba], b_sgt], [bo])
                            dma("sp", gt_d[j, :, 15 + blk * 512:15 + (blk + 1) * 512], o[:], [bo], [b_gt[blk]])
                    run_tiles(tl_, glu_part)
                    dma("sp", qt_d.rearrange("h p n -> p h n")[:, :, blk * 512:(blk + 1) * 512], QTb3, [b_QTb], [b_qt[blk]])
                    for j in range(16):
                        cg = 3584 + j * 128
                        bg = 4 + (j % 2)
                        for k in range(8):
                            mm(bank(bg), win3[:, k, cg:cg + 128], hT3[:, k, :], k == 0, k == 7, b_hT + bw(cg, cg + 128), [pb[bg]], silent=(k < 7))
                        o = ob[obi[0] % 2]; bo = b_ob[obi[0] % 2]; obi[0] += 1
                        act(o[:], bank(bg), AF.Sigmoid, [pb[bg]], [bo])
                        dma("sp", sg_d[j, :, blk * 512:(blk + 1) * 512], o[:], [bo], [b_sg[blk]])
                    if upto == 1 and blk == 0:
                        break
                dump("KT", KT[:], b_KT)
                dump("Vs", Vs[:], b_V)
                dump("QTb", QTb[:], [b_QTb])
                S.barrier(exclude=S.dsem["pool"])
                dump("gt", gt_d.rearrange("k p n -> p k n")[:, :, 0:542], [])
                dump("sg", sg_d.rearrange("k p n -> p k n")[:, :, 0:512], [])

        if upto >= 1:
            S.barrier(exclude=S.dsem["pool"])
        w01.close()

        if upto >= 2:
            with ExitStack() as es:
                wao = sbt(es, "wao", [128, 8 * D], BF16); b_wao = Buf()
                wco = sbt(es, "wco", [128, 8 * D], BF16); b_wco = Buf()
                wout = sbt(es, "wout", [128, 8 * D], BF16); b_wout = Buf()
                wao3 = wao[:].rearrange("p (k n) -> p k n", k=8)
                wco3 = wco[:].rearrange("p (k n) -> p k n", k=8)
                wout3 = wout[:].rearrange("p (k n) -> p k n", k=8)
                dma("sp", wao3, waobf_d.rearrange("(k p) n -> p k n", p=128), [b_waobf], [b_wao])
                dma("sp", wco3, wcobf_d.rearrange("(k p) n -> p k n", p=128), [b_wcobf], [b_wco])
                dma("sp", wout3, woutbf_d.rearrange("(k p) n -> p k n", p=128), [b_woutbf], [b_wout])
                dma("pool", wqbf_d, wq_d, [], [b_wqbf])
                dma("pool", keyTbf_d, keyT_d, [], [b_keyTbf])
                cdw = sbt(es, "cdw", [128, 248], F32); b_cdw = Buf()
                dma("sp", cdw[:], cdw_d, [], [b_cdw])
                QTb = sbt(es, "QTb2", [128, 8 * 512], BF16); b_QTb = Buf()
                QTb3 = QTb[:].rearrange("p (h n) -> p h n", h=8)
                gth = sbt(es, "gth", [128, 8 * 542], BF16); b_gth = Buf()
                gth3 = gth[:].rearrange("p (k n) -> p k n", k=8)
                sgl = [sbt(es, "sgl%d" % i, [128, 512], BF16) for i in range(4)]; b_sgl = [Buf() for _ in range(4)]
                psb = [sbt(es, "psb%d" % i, [128, 1024], BF16) for i in range(3)]; b_psb = [Buf() for _ in range(3)]
                rz = sbt(es, "rz", [128, 512], F32); b_rz = Buf()
                OTb = sbt(es, "OTb", [128, 8 * 512], BF16); b_OT = [Buf() for _ in range(8)]
                OTb3 = OTb[:].rearrange("p (h n) -> p h n", h=8)
                ycv = sbt(es, "ycv", [128, 8 * 512], F32); b_ycv = [Buf() for _ in range(8)]
                ycv3 = ycv[:].rearrange("p (k n) -> p k n", k=8)
                sqy = sbt(es, "sqy", [128, 512], F32); b_sqy = Buf()
                cacc = [sbt(es, "cacc%d" % i, [128, 512], F32) for i in range(3)]; b_cacc = [Buf() for _ in range(3)]
                mean = sbt(es, "mean", [128, 512], F32); b_mean = Buf()
                msq = sbt(es, "msq", [128, 512], F32); b_msq = Buf()
                var = sbt(es, "var", [128, 512], F32); b_var = Buf()
                rstd = sbt(es, "rstd", [128, 512], F32); b_rstd = Buf()
                yns = [sbt(es, "yn%d" % i, [128, 512], F32) for i in range(2)]; b_yns = [Buf(), Buf()]
                zT = sbt(es, "zT", [128, 8 * 512], BF16); b_zT = [Buf() for _ in range(8)]
                zT3 = zT[:].rearrange("p (k n) -> p k n", k=8)
                m2 = sbt(es, "m2", [128, 512], F32); b_m2 = Buf()
                tm2 = sbt(es, "tm2", [128, 512], F32); b_tm2 = Buf()
                mgT = sbt(es, "mgT", [128, 8 * 512], BF16); b_mg = [Buf() for _ in range(8)]
                mgT3 = mgT[:].rearrange("p (k n) -> p k n", k=8)
                xt = [sbt(es, "xt2_%d" % i, [128, D], F32) for i in range(2)]; b_xt = [Buf(), Buf()]
                t1 = sbt(es, "t1", [128, D], F32); b_t1 = Buf()
                xs = sbt(es, "xs", [128, D], F32); b_xs_ = Buf()
                junk = sbt(es, "junk2", [128, D], BF16); b_junk = Buf()
                xn = sbt(es, "xn2", [128, D], F32); b_xn = Buf()
                st4 = sbt(es, "st4b", [128, 4], F32); b_st4 = Buf()

                STG = 9
                pcnt = 0
                sgcnt = 0
                xcnt = 0
                nblk = 1 if upto == 2 else 8
                for blk in range(nblk):
                    dma("sp", QTb3, qt_d.rearrange("h p n -> p h n")[:, :, blk * 512:(blk + 1) * 512], [], [b_QTb])
                    dma("sp", gth3, gt_d.rearrange("k p n -> p k n")[:, :, blk * 512:blk * 512 + 542], [], [b_gth])
                    NK_ = NKT
                    steps = [(h_, kt_) for h_ in range(8) for kt_ in range(NK_)]
                    SB = [0, 1, 6, 7]

                    def emitS(si):
                        h_, kt_ = steps[si]
                        kvh_ = h_ // 4
                        bk_ = SB[si % 4]
                        mm(bank(bk_), KT[:, kvh_ * NKEY + kt_ * 128:kvh_ * NKEY + (kt_ + 1) * 128], QTb3[:, h_, :], True, True,
                           [b_QTb], [pb[bk_]])

                    def conv_tap(j, tp):
                        ch = tp % 4
                        acc = ycv3[:, j, :] if ch == 0 else cacc[ch - 1][:]
                        bacc = b_ycv[j] if ch == 0 else b_cacc[ch - 1]
                        if tp == 0:
                            ts("dve", acc, gth3[:, j, 0:512], cdw[:, j * 31:j * 31 + 1], ncol[:, 16 + j:17 + j],
                               ALU.mult, ALU.add, [b_gth, b_cdw, b_ncol], [bacc])
                        elif tp < 4:
                            ts("dve", acc, gth3[:, j, tp:tp + 512], cdw[:, j * 31 + tp:j * 31 + tp + 1], None,
                               ALU.mult, None, [b_gth, b_cdw], [bacc])
                        else:
                            stt(acc, gth3[:, j, tp:tp + 512], cdw[:, j * 31 + tp:j * 31 + tp + 1], acc,
                                ALU.mult, ALU.add, [b_gth, b_cdw, bacc], [bacc])
                    emitS(0)
                    emitS(1)
                    for h in range(8):
                        kvh = h // 4
                        bo, bz = (2, 3) if h % 2 == 0 else (4, 5)
                        j = h
                        for kp in range(NK_ // 2):
                            si = h * NK_ + 2 * kp
                            for d_ in (2, 3):
                                if si + d_ < len(steps):
                                    emitS(si + d_)
                            for tp in (2 * kp, 2 * kp + 1):
                                if tp < 31:
                                    conv_tap(j, tp)
                            b0 = SB[si % 4]
                            p_ = psb[pcnt % 3]; bp = b_psb[pcnt % 3]; pcnt += 1
                            act(p_[:], PS[:, b0 * 512:(b0 + 2) * 512], AF.Exp, [pb[b0], pb[b0 + 1]], [bp])
                            for hf in range(2):
                                kt = 2 * kp + hf
                                mm(bank(bo), Vs[:, kt * 256 + kvh * 128:kt * 256 + (kvh + 1) * 128], p_[:, hf * 512:(hf + 1) * 512],
                                   kt == 0, kt == NK_ - 1, [bp], [pb[bo]])
                                mm(bank(bz), ones_b[:], p_[:, hf * 512:(hf + 1) * 512], kt == 0, kt == NK_ - 1, [bp], [pb[bz]])
                        rcp(rz[:], bank(bz), [pb[bz]], [b_rz])
                        tt("dve", OTb3[:, h, :], bank(bo), rz[:], ALU.mult, [pb[bo], b_rz], [b_OT[h]])
                        tt("pool", cacc[1][:], cacc[1][:], cacc[2][:], ALU.add, [b_cacc[1], b_cacc[2]], [b_cacc[1]])
                        tt("pool", ycv3[:, j, :], ycv3[:, j, :], cacc[0][:], ALU.add, [b_ycv[j], b_cacc[0]], [b_ycv[j]])
                        tt("pool", ycv3[:, j, :], ycv3[:, j, :], cacc[1][:], ALU.add, [b_ycv[j], b_cacc[1]], [b_ycv[j]])
                    for j in range(8):
                        act(sqy[:], ycv3[:, j, :], AF.Square, [b_ycv[j]], [b_sqy])
                        mm(bank(6), ones_f[:], ycv3[:, j, :], j == 0, j == 7, [b_ycv[j], b_1f], [pb[6]])
                        mm(bank(7), ones_f[:], sqy[:], j == 0, j == 7, [b_sqy, b_1f], [pb[7]])
                    if STG < 3:
                        continue
                    ts("dve", mean[:], bank(6), 1.0 / D, None, ALU.mult, None, [pb[6]], [b_mean])
                    tt("dve", msq[:], mean[:], mean[:], ALU.mult, [b_mean], [b_msq])
                    stt(var[:], bank(7), 1.0 / D, msq[:], ALU.mult, ALU.subtract, [pb[7], b_msq], [b_var])
                    ts("dve", var[:], var[:], EPS, None, ALU.add, None, [b_var], [b_var])
                    act(var[:], var[:], AF.Sqrt, [b_var], [b_var])
                    rcp(rstd[:], var[:], [b_var], [b_rstd])
                    for j in range(8):
                        yn = yns[j % 2]; b_yn = b_yns[j % 2]
                        tt("dve", yn[:], ycv3[:, j, :], mean[:], ALU.subtract, [b_ycv[j], b_mean], [b_yn])
                        tt("dve", yn[:], yn[:], rstd[:], ALU.mult, [b_yn, b_rstd], [b_yn])
                        act(zT3[:, j, :], yn[:], AF.Silu, [b_yn, b_ncol], [b_zT[j]],
                            bias=ncol[:, 32 + j:33 + j], scale=ncol[:, 24 + j:25 + j])
                    if STG < 4:
                        continue
                    for j in range(8):
                        sgc = sgl[sgcnt % 4]; bsgc = b_sgl[sgcnt % 4]; sgcnt += 1
                        dma("sp", sgc[:], sg_d[8 + j, :, blk * 512:(blk + 1) * 512], [], [bsgc])
                        sga = sgl[sgcnt % 4]; bsga = b_sgl[sgcnt % 4]; sgcnt += 1
                        dma("sp", sga[:], sg_d[j, :, blk * 512:(blk + 1) * 512], [], [bsga])
                        bc_, ba_ = (6, 7) if j % 2 == 0 else (0, 1)
                        for k in range(8):
                            mm(bank(bc_), wco3[:, k, j * 128:(j + 1) * 128], zT3[:, k, :], k == 0, k == 7, [b_wco, b_zT[k]], [pb[bc_]], silent=(k < 7))
                        tt("dve", m2[:], bank(bc_), sgc[:], ALU.mult, [pb[bc_], bsgc], [b_m2])
                        for k in range(8):
                            mm(bank(ba_), wao3[:, k, j * 128:(j + 1) * 128], OTb3[:, k, :], k == 0, k == 7, [b_wao, b_OT[k]], [pb[ba_]], silent=(k < 7))
                        tt("dve", tm2[:], bank(ba_), sga[:], ALU.mult, [pb[ba_], bsga], [b_tm2])
                        tt("dve", mgT3[:, j, :], tm2[:], m2[:], ALU.add, [b_tm2, b_m2], [b_mg[j]])
                    if blk == 0:
                        dump("mgT", mgT[:], b_mg)
                        dump("OTb", OTb[:], b_OT)
                        dump("zT", zT[:], b_zT)
                    if STG < 5:
                        continue
                    xbufs = []
                    for ti in range(4):
                        tg = blk * 4 + ti
                        x_ = xt[xcnt % 2]; bx = b_xt[xcnt % 2]; xcnt += 1
                        xbufs.append((x_, bx))

                    def t_mm(ti):
                        tg = blk * 4 + ti
                        x_, bx = xbufs[ti]
                        dma("sp", x_[:], x_d[tg * 128:(tg + 1) * 128, :], [], [bx])
                        ob_ = 6 if ti % 2 == 0 else 2
                        for nb in range(2):
                            for k in range(8):
                                mm(bank(ob_ + nb), mgT3[:, k, ti * 128:(ti + 1) * 128], wout3[:, k, nb * 512:(nb + 1) * 512],
                                   k == 0, k == 7, [b_mg[k], b_wout], [pb[ob_ + nb]], silent=(k < 7))

                    def t_chain(ti):
                        tg = blk * 4 + ti
                        x_, bx = xbufs[ti]
                        ob_ = 6 if ti % 2 == 0 else 2
                        tt("dve", t1[:], PS[:, ob_ * 512:(ob_ + 2) * 512], g1b[:], ALU.mult, [pb[ob_], pb[ob_ + 1], b_g1b], [b_t1])
                        tt("dve", xs[:], t1[:], x_[:], ALU.add, [b_t1, bx], [b_xs_])
                        dma("sp", xs_d[tg * 128:(tg + 1) * 128, :], xs[:], [b_xs_], [b_xs[tg]])
                        act(junk[:], xs[:], AF.Square, [b_xs_], [b_junk, b_st4], accum=st4[:, 0:1])
                        ts("dve", st4[:, 1:2], st4[:, 0:1], 1.0 / D, EPS, ALU.mult, ALU.add, [b_st4], [b_st4])
                        act(st4[:, 2:3], st4[:, 1:2], AF.Sqrt, [b_st4], [b_st4])
                        rcp(st4[:, 3:4], st4[:, 2:3], [b_st4], [b_st4])
                        ts("dve", xn[:], xs[:], st4[:, 3:4], None, ALU.mult, None, [b_xs_, b_st4], [b_xn])

                    def t_trev(ti):
                        tb_ = 0 if ti % 2 == 0 else 4
                        for k in range(8):
                            tr(bank(tb_ + k // 4, (k % 4) * 128, (k % 4) * 128 + 128), xn[:, k * 128:(k + 1) * 128], ident_f[:],
                               [b_xn, b_idf], [pb[tb_ + k // 4]])
                        for k in range(8):
                            dst = zT3[:, k, ti * 128:(ti + 1) * 128]
                            src = bank(tb_ + k // 4, (k % 4) * 128, (k % 4) * 128 + 128)
                            ts("dve", dst, src, modc[:, 32 + k:33 + k], modc[:, 40 + k:41 + k],
                               ALU.mult, ALU.add, [pb[tb_ + k // 4], b_modc], [b_zT[k]])
                    t_mm(0)
                    t_mm(1)
                    for ti in range(4):
                        t_chain(ti)
                        if ti + 2 < 4:
                            t_mm(ti + 2)
                        t_trev(ti)
                    if STG >= 8:
                        dma("sp", h2_d.rearrange("k p n -> p k n")[:, :, blk * 512:(blk + 1) * 512], zT3, b_zT, [b_h2[blk]])
                S.barrier(exclude=S.dsem["pool"])
                dump("xs0", xs_d[0:512, :], [])
                dump("h2", h2_d.rearrange("k p n -> p k n")[:, :, 0:512], [])

        mid.close()

        if upto >= 3:
            with ExitStack() as es:
                wq = sbt(es, "wq", [128, 8 * 2048], BF16); b_wq = Buf()
                wq3 = wq[:].rearrange("p (k n) -> p k n", k=8)
                dma("sp", wq3, wqbf_d.rearrange("(k p) n -> p k n", p=128), [b_wqbf], [b_wq])
                keyT = sbt(es, "keyT", [128, 2048], BF16); b_keyT = Buf()
                dma("sp", keyT[:], keyTbf_d, [b_keyTbf], [b_keyT])
                h2Ts = [sbt(es, "h2T%d" % i, [128, 8 * 512], BF16) for i in range(2)]; b_h2Ts = [Buf(), Buf()]
                qT = sbt(es, "qT", [128, 16 * 512], BF16); b_qT = [Buf() for _ in range(16)]
                qT3 = qT[:].rearrange("p (c n) -> p c n", c=16)
                thr = sbt(es, "thr", [128, 16], F32); b_thr = Buf()
                io16 = sbt(es, "io16", [128, 16], F32); b_io16 = Buf()
                rtb = sbt(es, "rtb", [128, 3 * 512], F32); b_rtb = Buf()
                rtb3 = rtb[:].rearrange("p (a n) -> p a n", a=3)

                class _NS:
                    pass
                sets = []
                for s_ in range(2):
                    n_ = _NS()
                    n_.sc = sbt(es, "sc%d" % s_, [128, 2048], F32); n_.b_sc = Buf()
                    n_.scr2 = sbt(es, "scr2%d" % s_, [128, 2048], F32)
                    n_.m16 = sbt(es, "m16%d" % s_, [128, 256], F32)
                    n_.b_m16a = [Buf() for _ in range(16)]; n_.b_m16b = [Buf() for _ in range(16)]
                    n_.b_i16a = [Buf() for _ in range(16)]; n_.b_i16b = [Buf() for _ in range(16)]
                    n_.b_scr2g = [Buf() for _ in range(16)]
                    n_.b_b16a = [Buf() for _ in range(8)]; n_.b_b16b = [Buf() for _ in range(8)]
                    n_.b_p16a = [Buf() for _ in range(8)]; n_.b_p16b = [Buf() for _ in range(8)]
                    n_.b_cs2h = [Buf() for _ in range(8)]
                    n_.i16 = sbt(es, "i16%d" % s_, [128, 256], U32)
                    n_.i16f = sbt(es, "i16f%d" % s_, [128, 256], F32); n_.b_i16f = Buf()
                    n_.cs_ = sbt(es, "cs_%d" % s_, [128, 2048], F32); n_.b_cs_ = Buf()
                    n_.cs2 = sbt(es, "cs2%d" % s_, [128, 2048], F32)
                    n_.b16 = sbt(es, "b16%d" % s_, [128, 128], F32)
                    n_.p16 = sbt(es, "p16%d" % s_, [128, 128], U32)
                    n_.p16f = sbt(es, "p16f%d" % s_, [128, 128], F32); n_.b_p16f = Buf()
                    n_.af = sbt(es, "af%d" % s_, [128, 128], F32); n_.b_af = Buf()
                    n_.bf_ = sbt(es, "bf_%d" % s_, [128, 128], F32); n_.b_bf = Buf()
                    n_.E = sbt(es, "E%d" % s_, [128, 2048], F32); n_.b_E = Buf()
                    n_.sel = sbt(es, "sel%d" % s_, [128, 3 * 128], F32); n_.b_sel = Buf()
                    n_.z8 = sbt(es, "z8%d" % s_, [128, 16], F32); n_.b_z8 = Buf()
                    n_.pb0 = 4 * s_
                    sets.append(n_)
                S.op("pool", lambda e: e.iota(out=io16[:], pattern=[[1, 16]], base=0, channel_multiplier=0,
                                              allow_small_or_imprecise_dtypes=True), [], [b_io16])
                ts("dve", thr[:], io16[:], 16.0, None, ALU.mult, None, [b_io16], [b_thr])

                def tile_prog(blk, ti, n_):
                    Q = []

                    def q(fn, *a_, **k_):
                        Q.append((fn, a_, k_))
                    sc, scr2, m16, i16, i16f, cs_, cs2 = n_.sc, n_.scr2, n_.m16, n_.i16, n_.i16f, n_.cs_, n_.cs2
                    b16, p16, p16f, af, bf_, E, sel, z8 = n_.b16, n_.p16, n_.p16f, n_.af, n_.bf_, n_.E, n_.sel, n_.z8
                    pb0 = n_.pb0
                    for c in range(16):
                        bk_ = pb0 + c // 4
                        q(mm, bank(bk_, (c % 4) * 128, (c % 4) * 128 + 128), qT3[:, c, ti * 128:(ti + 1) * 128],
                          keyT[:, c * 128:(c + 1) * 128], True, True, [b_qT[c], b_keyT], [pb[bk_]])
                    q(cp, "act", sc[:], PS[:, pb0 * 512:pb0 * 512 + 2048], [pb[pb0 + i_] for i_ in range(4)], [n_.b_sc])
                    for stp in range(5):
                        for g in range(16):
                            sg_ = sc[:, g * 128:(g + 1) * 128]
                            sr_ = scr2[:, g * 128:(g + 1) * 128]
                            if stp == 0:
                                q(S.op, "dve", lambda e, g=g, sg_=sg_: e.max(out=m16[:, g * 16:g * 16 + 8], in_=sg_), [n_.b_sc], [n_.b_m16a[g]])
                            elif stp == 1:
                                q(S.op, "dve", lambda e, g=g, sg_=sg_: e.max_index(out=i16[:, g * 16:g * 16 + 8], in_max=m16[:, g * 16:g * 16 + 8],
                                                                                   in_values=sg_), [n_.b_sc, n_.b_m16a[g]], [n_.b_i16a[g]])
                            elif stp == 2:
                                q(S.op, "dve", lambda e, g=g, sg_=sg_, sr_=sr_: e.match_replace(out=sr_, in_to_replace=m16[:, g * 16:g * 16 + 8],
                                                                                                in_values=sg_, imm_value=-1e30),
                                  [n_.b_sc, n_.b_m16a[g]], [n_.b_scr2g[g]])
                            elif stp == 3:
                                q(S.op, "dve", lambda e, g=g, sr_=sr_: e.max(out=m16[:, g * 16 + 8:g * 16 + 16], in_=sr_), [n_.b_scr2g[g]], [n_.b_m16b[g]])
                            else:
                                q(S.op, "dve", lambda e, g=g, sr_=sr_: e.max_index(out=i16[:, g * 16 + 8:g * 16 + 16],
                                                                                   in_max=m16[:, g * 16 + 8:g * 16 + 16], in_values=sr_),
                                  [n_.b_scr2g[g], n_.b_m16b[g]], [n_.b_i16b[g]])
                    b_m16 = n_.b_m16a + n_.b_m16b
                    b_i16 = n_.b_i16a + n_.b_i16b
                    q(cp, "dve", i16f[:], i16[:], b_i16, [n_.b_i16f])
                    q(tt, "dve", XAP(cs_[:], [[256, 8], [16, 16], [1, 16]]), XAP(m16[:], [[32, 8], [1, 16], [0, 16]]),
                      XAP(m16[:], [[32, 8], [0, 16], [1, 16]], 16), ALU.add, b_m16, [n_.b_cs_])
                    for stp in range(5):
                        for h in range(8):
                            ch = cs_[:, h * 256:(h + 1) * 256]
                            ch2 = cs2[:, h * 256:(h + 1) * 256]
                            if stp == 0:
                                q(S.op, "dve", lambda e, h=h, ch=ch: e.max(out=b16[:, h * 16:h * 16 + 8], in_=ch), [n_.b_cs_], [n_.b_b16a[h]])
                            elif stp == 1:
                                q(S.op, "dve", lambda e, h=h, ch=ch: e.max_index(out=p16[:, h * 16:h * 16 + 8], in_max=b16[:, h * 16:h * 16 + 8],
                                                                                 in_values=ch), [n_.b_cs_, n_.b_b16a[h]], [n_.b_p16a[h]])
                            elif stp == 2:
                                q(S.op, "dve", lambda e, h=h, ch=ch, ch2=ch2: e.match_replace(out=ch2, in_to_replace=b16[:, h * 16:h * 16 + 8],
                                                                                              in_values=ch, imm_value=-1e30),
                                  [n_.b_cs_, n_.b_b16a[h]], [n_.b_cs2h[h]])
                            elif stp == 3:
                                q(S.op, "dve", lambda e, h=h, ch2=ch2: e.max(out=b16[:, h * 16 + 8:h * 16 + 16], in_=ch2), [n_.b_cs2h[h]], [n_.b_b16b[h]])
                            else:
                                q(S.op, "dve", lambda e, h=h, ch2=ch2: e.max_index(out=p16[:, h * 16 + 8:h * 16 + 16],
                                                                                   in_max=b16[:, h * 16 + 8:h * 16 + 16], in_values=ch2),
                                  [n_.b_cs2h[h], n_.b_b16b[h]], [n_.b_p16b[h]])
                    b_b16l = n_.b_b16a + n_.b_b16b
                    b_p16l = n_.b_p16a + n_.b_p16b
                    q(cp, "dve", p16f[:], p16[:], b_p16l, [n_.b_p16f])
                    q(tt, "dve", XAP(E[:], [[15, 128], [1, 15]]), XAP(p16f[:], [[1, 128], [0, 15]]),
                      XAP(thr[:], [[0, 128], [1, 15]], 1), ALU.is_ge, [n_.b_p16f, b_thr], [n_.b_E])
                    q(S.op, "dve", lambda e: e.tensor_reduce(out=af[:], in_=XAP(E[:], [[15, 128], [1, 15]]), axis=AX.X, op=ALU.add),
                      [n_.b_E], [n_.b_af])
                    q(stt, bf_[:], af[:], -16.0, p16f[:], ALU.mult, ALU.add, [n_.b_af, n_.b_p16f], [n_.b_bf])
                    for (src, bsrc, off, dsti) in ((af, n_.b_af, 0, 0), (bf_, n_.b_bf, 16, 1)):
                        q(tt, "dve", XAP(E[:], [[16, 128], [1, 16]]), XAP(src[:], [[1, 128], [0, 16]]),
                          XAP(io16[:], [[0, 128], [1, 16]]), ALU.is_equal, [bsrc, b_io16], [n_.b_E])
                        q(tt, "dve", XAP(E[:], [[256, 8], [16, 16], [1, 16]]), XAP(E[:], [[256, 8], [16, 16], [1, 16]]),
                          XAP(i16f[:], [[32, 8], [0, 16], [1, 16]], off), ALU.mult, [n_.b_E, n_.b_i16f], [n_.b_E])
                        q(S.op, "dve", lambda e, dsti=dsti: e.tensor_reduce(out=sel[:, dsti * 128:(dsti + 1) * 128],
                                                                             in_=XAP(E[:], [[16, 128], [1, 16]]), axis=AX.X, op=ALU.add),
                          [n_.b_E], [n_.b_sel])
                    q(tt, "dve", XAP(E[:], [[16, 8], [1, 16]]), XAP(b16[:], [[16, 8], [1, 16]]), XAP(b16[:], [[16, 8], [0, 16]]),
                      ALU.subtract, b_b16l, [n_.b_E])
                    q(act, E[:, 0:128], E[:, 0:128], AF.Exp, [n_.b_E], [n_.b_E])
                    q(S.op, "dve", lambda e: e.tensor_reduce(out=z8[:, 0:8], in_=XAP(E[:], [[16, 8], [1, 16]]), axis=AX.X, op=ALU.add),
                      [n_.b_E], [n_.b_z8])
                    q(rcp, z8[:, 8:16], z8[:, 0:8], [n_.b_z8], [n_.b_z8])
                    q(tt, "dve", XAP(sel[:], [[16, 8], [1, 16]], 256), XAP(E[:], [[16, 8], [1, 16]]), XAP(z8[:], [[1, 8], [0, 16]], 8),
                      ALU.mult, [n_.b_E, n_.b_z8], [n_.b_sel])
                    if blk == 0 and ti == 0:
                        q(dump, "sel", sel[:], [n_.b_sel])
                    for a3 in range(3):
                        q(tr, bank(pb0, a3 * 128, (a3 + 1) * 128), sel[:, a3 * 128:(a3 + 1) * 128], ident_f[:], [n_.b_sel, b_idf], [pb[pb0]])
                    q(cp, "act", rtb3[:, :, ti * 128:(ti + 1) * 128], bank(pb0, 0, 384).rearrange("p (a n) -> p a n", a=3), [pb[pb0]], [b_rtb])
                    return Q

                nblk = 1 if upto == 3 else 8
                def load_h2(blk_):
                    dma("sp", h2Ts[blk_ % 2][:].rearrange("p (k n) -> p k n", k=8),
                        h2_d.rearrange("k p n -> p k n")[:, :, blk_ * 512:(blk_ + 1) * 512], [], [b_h2Ts[blk_ % 2]])
                load_h2(0)
                for blk in range(nblk):
                    if blk + 1 < nblk:
                        load_h2(blk + 1)
                    h2T3 = h2Ts[blk % 2][:].rearrange("p (k n) -> p k n", k=8)
                    b_h2T = b_h2Ts[blk % 2]
                    for c in range(16):
                        bq = 4 + c % 2
                        for k in range(8):
                            mm(bank(bq), wq3[:, k, c * 128:(c + 1) * 128], h2T3[:, k, :], k == 0, k == 7, [b_wq, b_h2T], [pb[bq]], silent=(k < 7))
                        cp("act", qT3[:, c, :], bank(bq), [pb[bq]], [b_qT[c]])
                    if blk == 0:
                        cast_tables(after=[b_qT[15]])
                    for tp_ in range(2):
                        QA = tile_prog(blk, 2 * tp_, sets[0])
                        QB = tile_prog(blk, 2 * tp_ + 1, sets[1])
                        for i_ in range(max(len(QA), len(QB))):
                            for Q_ in (QA, QB):
                                if i_ < len(Q_):
                                    fn_, a_, k_ = Q_[i_]
                                    fn_(*a_, **k_)
                    dma("sp", rt_d.rearrange("a p n -> p a n")[:, :, blk * 512:(blk + 1) * 512], rtb3, [b_rtb], [b_rt[blk]])
                S.barrier(exclude=S.dsem["pool"])

        if upto >= 4:
            with ExitStack() as es:
                Wds = [sbt(es, "Wd%d" % i, [128, GP * 128], BF16) for i in range(2)]
                b_Wds = [[Buf() for _ in range(GP // 4)] for _ in range(2)]
                NSB = 3
                UTs = [sbt(es, "UTs%d" % i, [128, 2 * 1024], BF16) for i in range(NSB)]; b_UTs = [Buf() for _ in range(NSB)]
                Vcs = [sbt(es, "Vcs%d" % i, [128, 2 * 1024], BF16) for i in range(NSB)]; b_Vcs = [Buf() for _ in range(NSB)]
                h2gs = [sbt(es, "h2g%d" % i, [128, 8 * GP], BF16) for i in range(2)]; b_h2gs = [Buf(), Buf()]
                rtgs = [sbt(es, "rtg%d" % i, [128, 3 * GP], F32) for i in range(2)]; b_rtgs = [Buf(), Buf()]
                iof = sbt(es, "iof", [128, 128], F32); b_iof = Buf()
                iob = sbt(es, "iob", [128, 128], BF16); b_iob = Buf()
                gl = [sbt(es, "gl%d" % i, [128, GP], BF16) for i in range(2)]; b_gl = [Buf(), Buf()]
                At = [sbt(es, "At%d" % i, [128, GP], BF16) for i in range(2)]; b_At = [Buf(), Buf()]
                g2b = sbt(es, "g2b", [128, D], F32); b_g2b = Buf()
                fgb = sbt(es, "fgb", [128, D], F32); b_fgb = Buf()
                xsl = sbt(es, "xsl", [128, D], F32); b_xsl = Buf()
                yt = sbt(es, "yt", [128, D], F32); b_yt = Buf()
                yo = sbt(es, "yo", [128, D], F32); b_yo = Buf()
                junk = sbt(es, "junk3", [128, D], BF16); b_junk = Buf()
                st4 = sbt(es, "st4c", [128, 4], F32); b_st4 = Buf()
                dma("sp", g2b[:], bass.AP(tensor=g2_d.tensor, offset=0, ap=[[0, 128], [1, D]]), [b_g2d], [b_g2b])
                dma("sp", fgb[:], bass.AP(tensor=fg_d.tensor, offset=0, ap=[[0, 128], [1, D]]), [], [b_fgb])
                S.op("pool", lambda e: e.iota(out=iof[:], pattern=[[1, 128]], base=0, channel_multiplier=0,
                                              allow_small_or_imprecise_dtypes=True), [], [b_iof])
                cp("dve", iob[:], iof[:], [b_iof], [b_iob])
                utv = utbf_d.rearrange("(c p) n -> p c n", p=128)
                vtv = vbf_d.rearrange("(c p) n -> p c n", p=128)
                ngrp = 1 if upto == 4 else T // GP
                ocnt = [0]

                def load_grp(grp):
                    t0 = grp * GP
                    gi = grp % 2
                    dma("sp", h2gs[gi][:].rearrange("p (k n) -> p k n", k=8), h2_d.rearrange("k p n -> p k n")[:, :, t0:t0 + GP], [], [b_h2gs[gi]])
                    dma("sp", rtgs[gi][:].rearrange("p (a n) -> p a n", a=3), rt_d.rearrange("a p n -> p a n")[:, :, t0:t0 + GP], [], [b_rtgs[gi]])

                NOH = 8
                oh1 = [sbt(es, "oh1b_%d" % i, [128, 128], BF16) for i in range(NOH)]; b_oh1 = [Buf() for _ in range(NOH)]
                oh2 = [sbt(es, "oh2b_%d" % i, [128, 128], BF16) for i in range(NOH)]; b_oh2 = [Buf() for _ in range(NOH)]

                def wd_oh(grp, t):
                    gi = grp % 2
                    rtg3 = rtgs[gi][:].rearrange("p (a n) -> p a n", a=3)
                    o1 = oh1[t % NOH]; bo1 = b_oh1[t % NOH]
                    o2 = oh2[t % NOH]; bo2 = b_oh2[t % NOH]
                    ts("dve", o1[:], iob[:], rtg3[:, 0, t:t + 1], None, ALU.is_equal, None, [b_iob, b_rtgs[gi]], [bo1])
                    ts("dve", o2[:], iob[:], rtg3[:, 1, t:t + 1], rtg3[:, 2, t:t + 1], ALU.is_equal, ALU.mult, [b_iob, b_rtgs[gi]], [bo2])

                def wd_mm(grp, t):
                    bk = 6 + (t // 4) % 2
                    mm(bank(bk, (t % 4) * 128, (t % 4) * 128 + 128), oh1[t % NOH][:], oh2[t % NOH][:], True, True,
                       [b_oh1[t % NOH], b_oh2[t % NOH]], [pb[bk]])

                def wd_cp(grp, t4):
                    gi = grp % 2
                    bk = 6 + t4 % 2
                    cp("act", Wds[gi][:, t4 * 512:(t4 + 1) * 512], bank(bk), [pb[bk]], [b_Wds[gi][t4]])

                def wd_stage(grp, s, part=None):
                    n = GP // 2
                    if part in (None, "pre") and 0 <= s - 1 < n:
                        wd_mm(grp, 2 * (s - 1))
                    if part == "pre":
                        return
                    if 0 <= s < n:
                        wd_oh(grp, 2 * s); wd_oh(grp, 2 * s + 1)
                    if 0 <= s - 1 < n:
                        wd_mm(grp, 2 * (s - 1) + 1)
                    if s - 3 >= 0 and (s - 3) % 2 == 1 and (s - 3) < n:
                        wd_cp(grp, (s - 3) // 2)

                def load2(grp_, cq):
                    i3 = (grp_ * 64 + cq) % NSB
                    dma("sp", UTs[i3][:].rearrange("p (c n) -> p c n", c=2), utv[:, cq * 2:(cq + 1) * 2, :], [b_ut[cq // 8]], [b_UTs[i3]])
                    dma("sp", Vcs[i3][:].rearrange("p (c n) -> p c n", c=2), vtv[:, cq * 2:(cq + 1) * 2, :], [b_vt[cq // 8]], [b_Vcs[i3]])

                def emitH(grp_, c2):
                    i3 = (grp_ * 64 + c2 // 2) % NSB
                    cc = c2 % 2
                    bh = 4 + c2 % 2
                    h2g3_ = h2gs[grp_ % 2][:].rearrange("p (k n) -> p k n", k=8)
                    for k in range(8):
                        mm(bank(bh, 0, GP), UTs[i3][:, cc * 1024 + k * 128:cc * 1024 + (k + 1) * 128], h2g3_[:, k, :],
                           k == 0, k == 7, [b_UTs[i3], b_h2gs[grp_ % 2]], [pb[bh]], silent=(k < 7))

                xsls = [xsl, sbt(es, "xsl1", [128, D], F32)]; b_xsls = [b_xsl, Buf()]
                yts = [yt, sbt(es, "yt1", [128, D], F32)]; b_yts = [b_yt, Buf()]
                load_grp(0)
                for s in range(GP // 2 + 4):
                    wd_stage(0, s)
                load2(0, 0)
                load2(0, 1)
                emitH(0, 0)
                for grp in range(ngrp):
                    gi = grp % 2
                    t0 = grp * GP
                    Wd = Wds[gi]; b_Wd = b_Wds[gi]
                    if grp + 1 < ngrp:
                        load_grp(grp + 1)
                    for tl in range(GP // 128):
                        tg = t0 // 128 + tl
                        dma("sp", xsls[tl][:], xs_d[tg * 128:(tg + 1) * 128, :], [], [b_xsls[tl]])
                    for c2 in range(128):
                        cq, cc = c2 // 2, c2 % 2
                        if cc == 0 and cq + 2 < 64:
                            load2(grp, cq + 2)
                        if c2 + 1 < 128:
                            emitH(grp, c2 + 1)
                        if grp + 1 < ngrp:
                            wd_stage(grp + 1, c2, "pre")
                        i3 = (grp * 64 + cq) % NSB
                        bh = 4 + c2 % 2
                        g_ = gl[c2 % 2]; bg_ = b_gl[c2 % 2]
                        a_ = At[c2 % 2]; ba_ = b_At[c2 % 2]
                        act(g_[:], bank(bh, 0, GP), AF.Gelu, [pb[bh]], [bg_])
                        tt("dve", a_[:], g_[:], XAP(Wd[:], [[128, GP]], c2), ALU.mult, [bg_] + b_Wd, [ba_])
                        for tl in range(GP // 128):
                            for nb in range(2):
                                bo = tl * 2 + nb
                                mm(bank(bo), a_[:, tl * 128:(tl + 1) * 128], Vcs[i3][:, cc * 1024 + nb * 512:cc * 1024 + (nb + 1) * 512],
                                   c2 == 0, c2 == 127, [ba_, b_Vcs[i3]], [pb[bo]], silent=(bo < GP // 64 - 1))
                        if grp + 1 < ngrp:
                            wd_stage(grp + 1, c2, "post")
                    if grp + 1 < ngrp:
                        for s in range(128, GP // 2 + 4):
                            wd_stage(grp + 1, s)
                        load2(grp + 1, 0)
                        load2(grp + 1, 1)
                        emitH(grp + 1, 0)
                    for tl in range(GP // 128):
                        tt("dve", yts[tl][:], PS[:, tl * 1024:(tl + 1) * 1024], g2b[:], ALU.mult, [pb[tl * 2], pb[tl * 2 + 1], b_g2b], [b_yts[tl]])
                    for tl in range(GP // 128):
                        tg = t0 // 128 + tl
                        yt_ = yts[tl]; b_yt_ = b_yts[tl]
                        tt("pool", yt_[:], yt_[:], xsls[tl][:], ALU.add, [b_yt_, b_xsls[tl]], [b_yt_])
                        act(junk[:], yt_[:], AF.Square, [b_yt_], [b_junk, b_st4], accum=st4[:, 0:1])
                        ts("dve", st4[:, 1:2], st4[:, 0:1], 1.0 / D, EPS, ALU.mult, ALU.add, [b_st4], [b_st4])
                        act(st4[:, 2:3], st4[:, 1:2], AF.Sqrt, [b_st4], [b_st4])
                        rcp(st4[:, 3:4], st4[:, 2:3], [b_st4], [b_st4])
                        stt(yo[:], yt_[:], st4[:, 3:4], fgb[:], ALU.mult, ALU.mult, [b_yt_, b_st4, b_fgb], [b_yo])
                        dma("sp", out_d[tg * 128:(tg + 1) * 128, :], yo[:], [b_yo], [])
        S.emit()
    return nc


def host_prep(inputs):
    f = lambda a: np.ascontiguousarray(np.asarray(a, dtype=np.float32))
    col = lambda v: f(np.asarray(v).reshape(8, 128).T)
    shared = {}
    shared["w_mod"] = f(inputs["w_mod"][0])
    shared["b_mod"] = f(inputs["b_mod"][0].reshape(1, -1))
    shared["ncol"] = f(np.concatenate([col(inputs["norm1_g"][0]), col(inputs["norm2_g"][0]), col(inputs["conv_b"][0]),
                                       col(inputs["conv_ln_g"][0]), col(inputs["conv_ln_b"][0])], axis=1))
    shared["w_in"] = f(inputs["w_in"][0])
    shared["qkg"] = f(np.concatenate([inputs["q_norm_g"][0], inputs["k_norm_g"][0]]).reshape(1, 256))
    shared["w_attn_o"] = f(inputs["w_attn_o"][0])
    cdw = np.asarray(inputs["conv_dw"][0])
    shared["cdw"] = f(cdw.T.reshape(8, 128, 31).transpose(1, 0, 2).reshape(128, 8 * 31))
    shared["w_conv_o"] = f(inputs["w_conv_o"][0])
    shared["w_out"] = f(inputs["w_out"][0])
    shared["peer_wq"] = f(inputs["peer_wq"][0])
    keys = np.asarray(inputs["peer_keys"][0])
    shared["keyT"] = f(keys.reshape(16, 128, 128).transpose(2, 0, 1).reshape(128, 16 * 128))
    U = np.asarray(inputs["peer_u"][0])
    shared["ut_l"] = f(U.reshape(128, 128, 8, 128).transpose(1, 3, 2, 0).reshape(16384, 1024))
    V = np.asarray(inputs["peer_v"][0])
    shared["v_l"] = f(V.reshape(128, 128, 1024).transpose(1, 0, 2).reshape(16384, 1024))
    shared["fg"] = f(np.asarray(inputs["final_norm_g"]).reshape(1, -1))
    t = np.arange(T)
    row = (t // 64).astype(np.float32)
    colp = (t % 64).astype(np.float32)
    inv = (np.float32(10000.0) ** (-np.arange(0, 64, 2, dtype=np.float32) / np.float32(64))).astype(np.float32)
    ar = row[:, None] * inv[None, :]
    ac = colp[:, None] * inv[None, :]
    cr, sr, cc, sc = np.cos(ar), np.sin(ar), np.cos(ac), np.sin(ac)
    shared["rope_c"] = f(np.concatenate([cr, cr, cc, cc], axis=1))
    shared["rope_s"] = f(np.concatenate([-sr, sr, -sc, sc], axis=1))
    in_maps = []
    cctx = col(inputs["c_ctx"])
    for b in range(8):
        m = dict(shared)
        m["x"] = f(inputs["x"][b])
        m["ctx"] = f(inputs["ctx"][b])
        m["ccol"] = f(np.concatenate([col(inputs["c"][b]), cctx], axis=1))
        in_maps.append(m)
    return in_maps


def kernel(**inputs):
    in_maps = host_prep(inputs)
    nc = build()
    res = run_bass_kernel_spmd(nc, in_maps, core_ids=list(range(8)))
    return np.stack([np.asarray(r["out"], dtype=np.float32) for r in res.results], axis=0)
```
